# Optimizing a Trainium2 kernel written in Bass

```python
import math
import jax, jax.numpy as jnp
from jax import lax
import numpy as np

D_MODEL = 1024
BATCH = 2
SEQ = 8192
DEPTH = 2

N_MIXERS = 2

DN_QK_HEADS = 8
DN_V_HEADS = 16
DN_HEAD_DIM = 128
DN_CONV = 4
DN_CHUNK = 64
DN_KEY_DIM = DN_QK_HEADS * DN_HEAD_DIM
DN_VAL_DIM = DN_V_HEADS * DN_HEAD_DIM
DN_CONV_DIM = 2 * DN_KEY_DIM + DN_VAL_DIM
DN_PROJ = DN_CONV_DIM + DN_VAL_DIM + 2 * DN_V_HEADS

CF_KERNEL = 31
CF_INNER = D_MODEL

N_EXPERTS = 32
TOP_K = 4
D_FF = D_MODEL
SWIGLU_LIMIT = 7.0
SWIGLU_ALPHA = 1.702
EXPERT_BLOCK = 128

LN_EPS = 1e-5
RMS_EPS = 1e-6
L2_EPS = 1e-6
DEEPNORM_ALPHA = (2 * DEPTH) ** 0.25
DEEPNORM_BETA = (8 * DEPTH) ** -0.25

kernel_name = 'hybrid_deltanet_conformer_moe_deepnorm_adaln'


def layer_norm(x, g, b):
    xf = x.astype(jnp.float32)
    mu = jnp.mean(xf, -1, keepdims=True)
    var = jnp.mean(jnp.square(xf - mu), -1, keepdims=True)
    return ((xf - mu) * lax.rsqrt(var + LN_EPS) * g + b).astype(x.dtype)


def modulate(x, shift, scale):
    return x * (1.0 + scale[:, None, :]) + shift[:, None, :]


def causal_depthwise_conv(x, w):
    K, C = w.shape
    return lax.conv_general_dilated(
        x, w[:, None, :].astype(x.dtype), window_strides=(1,), padding=[(K - 1, 0)],
        dimension_numbers=('NWC', 'WIO', 'NWC'), feature_group_count=C)


def gated_delta_rule(q, k, v, g, beta):
    f32 = jnp.float32
    B, S, H, DK = q.shape
    DV = v.shape[-1]
    C = DN_CHUNK
    N = S // C
    q, k, v, g, beta = (t.astype(f32) for t in (q, k, v, g, beta))
    q = q * lax.rsqrt(jnp.sum(q * q, -1, keepdims=True) + L2_EPS) * (DK ** -0.5)
    k = k * lax.rsqrt(jnp.sum(k * k, -1, keepdims=True) + L2_EPS)

    def chunks(t):
        t = t.reshape((B, N, C, H) + t.shape[3:])
        return jnp.moveaxis(t, 3, 1)

    q, k, v, beta = chunks(q), chunks(k), chunks(v), chunks(beta)
    g = jnp.cumsum(chunks(g), axis=-1)
    causal = jnp.tril(jnp.ones((C, C), dtype=bool))
    strict = jnp.tril(jnp.ones((C, C), dtype=bool), -1)
    decay = jnp.exp(jnp.where(causal, g[..., :, None] - g[..., None, :], -jnp.inf))
    kb = k * beta[..., None]
    a = jnp.where(strict, jnp.einsum('bhnid,bhnjd->bhnij', kb, k) * decay, 0.0)
    lower = a + jnp.eye(C, dtype=f32)
    rhs = jnp.concatenate([v * beta[..., None], kb * jnp.exp(g)[..., None]], axis=-1)
    sol = lax.linalg.triangular_solve(lower, rhs, left_side=True, lower=True, unit_diagonal=True)
    u, w = sol[..., :DV], sol[..., DV:]
    qk = jnp.where(causal, jnp.einsum('bhnid,bhnjd->bhnij', q, k) * decay, 0.0)
    qg = q * jnp.exp(g)[..., None]
    kd = k * jnp.exp(g[..., -1:] - g)[..., None]
    g_last = jnp.exp(g[..., -1])

    def step(state, xs):
        qg_i, qk_i, u_i, w_i, kd_i, gl_i = xs
        v_new = u_i - jnp.einsum('bhck,bhkv->bhcv', w_i, state)
        o = jnp.einsum('bhck,bhkv->bhcv', qg_i, state) + jnp.einsum('bhij,bhjv->bhiv', qk_i, v_new)
        state = state * gl_i[..., None, None] + jnp.einsum('bhck,bhcv->bhkv', kd_i, v_new)
        return state, o

    xs = tuple(jnp.moveaxis(t, 2, 0) for t in (qg, qk, u, w, kd, g_last))
    _, o = lax.scan(step, jnp.zeros((B, H, DK, DV), f32), xs)
    return jnp.transpose(o, (1, 0, 3, 2, 4)).reshape(B, S, H, DV)


def gated_deltanet(h, in_w, conv_w, A_log, dt_bias, onorm_w, out_w):
    B, S, _ = h.shape
    proj = h @ in_w
    qkv, z, b, a = jnp.split(proj, [DN_CONV_DIM, DN_CONV_DIM + DN_VAL_DIM,
                                    DN_CONV_DIM + DN_VAL_DIM + DN_V_HEADS], axis=-1)
    qkv = jax.nn.silu(causal_depthwise_conv(qkv, conv_w))
    q, k, v = jnp.split(qkv, [DN_KEY_DIM, 2 * DN_KEY_DIM], axis=-1)
    rep = DN_V_HEADS // DN_QK_HEADS
    q = jnp.repeat(q.reshape(B, S, DN_QK_HEADS, DN_HEAD_DIM), rep, axis=2)
    k = jnp.repeat(k.reshape(B, S, DN_QK_HEADS, DN_HEAD_DIM), rep, axis=2)
    v = v.reshape(B, S, DN_V_HEADS, DN_HEAD_DIM)
    beta = jax.nn.sigmoid(b.astype(jnp.float32))
    g = -jnp.exp(A_log.astype(jnp.float32)) * jax.nn.softplus(a.astype(jnp.float32) + dt_bias)
    o = gated_delta_rule(q, k, v, g, beta)
    o = o * lax.rsqrt(jnp.mean(o * o, -1, keepdims=True) + RMS_EPS) * onorm_w
    o = o * jax.nn.silu(z.astype(jnp.float32).reshape(B, S, DN_V_HEADS, DN_HEAD_DIM))
    return o.reshape(B, S, DN_VAL_DIM).astype(h.dtype) @ out_w


def conformer_conv(h, pw1_w, pw1_b, dw_w, dw_b, ln_g, ln_b, pw2_w, pw2_b):
    p = h @ pw1_w + pw1_b
    u = p[..., :CF_INNER] * jax.nn.sigmoid(p[..., CF_INNER:])
    u = causal_depthwise_conv(u, dw_w) + dw_b
    u = jax.nn.silu(layer_norm(u, ln_g, ln_b))
    return u @ pw2_w + pw2_b


def _padded_rows(n_assign):
    bound = n_assign + N_EXPERTS * (EXPERT_BLOCK - 1)
    return -(-bound // EXPERT_BLOCK) * EXPERT_BLOCK


def moe(h, router_w, router_b, w1, b1, w2, b2):
    B, S, D = h.shape
    T = B * S
    ht = h.reshape(T, D)
    logits = (ht @ router_w + router_b).astype(jnp.float32)
    top_val, top_idx = lax.top_k(logits, TOP_K)
    gates = jax.nn.softmax(top_val, axis=-1)
    A = T * TOP_K
    e_flat = top_idx.reshape(A)
    tok_flat = jnp.arange(A, dtype=jnp.int32) // TOP_K
    order = jnp.argsort(e_flat)
    e_sorted = e_flat[order]
    counts = jnp.zeros((N_EXPERTS,), jnp.int32).at[e_flat].add(1)
    padded = (counts + EXPERT_BLOCK - 1) // EXPERT_BLOCK * EXPERT_BLOCK
    starts = jnp.cumsum(counts) - counts
    pends = jnp.cumsum(padded)
    pstarts = pends - padded
    dest = pstarts[e_sorted] + jnp.arange(A, dtype=jnp.int32) - starts[e_sorted]
    P = _padded_rows(A)
    nb = P // EXPERT_BLOCK
    row_tok = jnp.zeros((P,), jnp.int32).at[dest].set(tok_flat[order])
    row_w = jnp.zeros((P,), jnp.float32).at[dest].set(gates.reshape(A)[order])
    block_e = jnp.minimum(
        jnp.searchsorted(pends, jnp.arange(nb, dtype=jnp.int32) * EXPERT_BLOCK, side='right'),
        N_EXPERTS - 1)
    xb = ht[row_tok].reshape(nb, EXPERT_BLOCK, D)

    def expert_block(args):
        xg, e = args
        hh = xg @ w1[e] + b1[e]
        x_glu, x_lin = hh[..., :D_FF], hh[..., D_FF:]
        x_glu = jnp.minimum(x_glu, SWIGLU_LIMIT)
        x_lin = jnp.clip(x_lin, -SWIGLU_LIMIT, SWIGLU_LIMIT)
        act = x_glu * jax.nn.sigmoid(SWIGLU_ALPHA * x_glu) * (x_lin + 1.0)
        return act @ w2[e] + b2[e]

    yb = lax.map(expert_block, (xb, block_e)).reshape(P, D)
    y = jax.ops.segment_sum(yb * row_w[:, None], row_tok, num_segments=T)
    return y.reshape(B, S, D).astype(h.dtype)


def setup_inputs(seed: int = 0) -> dict:
    key = jax.random.key(seed)
    ks = iter(jax.random.split(key, 32))
    n_dn = (DEPTH + N_MIXERS - 1) // N_MIXERS
    n_cf = DEPTH // N_MIXERS

    def nrm(shape, scale):
        return jax.random.normal(next(ks), shape, jnp.float32) * scale

    def gain(shape):
        return 1.0 + nrm(shape, 0.02)

    d = D_MODEL
    inputs = {}
    inputs['x'] = nrm((BATCH, SEQ, d), 1.0)
    inputs['c'] = nrm((BATCH, d), 1.0)
    inputs['ada_w'] = nrm((DEPTH, d, 6 * d), 0.5 * d ** -0.5)
    inputs['ada_b'] = nrm((DEPTH, 6 * d), 0.02)
    inputs['dn_in_w'] = nrm((n_dn, d, DN_PROJ), d ** -0.5)
    inputs['dn_conv_w'] = nrm((n_dn, DN_CONV, DN_CONV_DIM), DN_CONV ** -0.5)
    inputs['dn_A_log'] = jnp.log(jax.random.uniform(next(ks), (n_dn, DN_V_HEADS), jnp.float32, 1.0, 16.0))
    dt = jnp.exp(jax.random.uniform(next(ks), (n_dn, DN_V_HEADS), jnp.float32,
                                    math.log(1e-3), math.log(1e-1)))
    inputs['dn_dt_bias'] = dt + jnp.log(-jnp.expm1(-dt))
    inputs['dn_onorm_w'] = gain((n_dn, DN_HEAD_DIM))
    inputs['dn_out_w'] = nrm((n_dn, DN_VAL_DIM, d), DN_VAL_DIM ** -0.5 * DEEPNORM_BETA)
    inputs['cf_pw1_w'] = nrm((n_cf, d, 2 * CF_INNER), d ** -0.5)
    inputs['cf_pw1_b'] = nrm((n_cf, 2 * CF_INNER), 0.02)
    inputs['cf_dw_w'] = nrm((n_cf, CF_KERNEL, CF_INNER), CF_KERNEL ** -0.5)
    inputs['cf_dw_b'] = nrm((n_cf, CF_INNER), 0.02)
    inputs['cf_ln_g'] = gain((n_cf, CF_INNER))
    inputs['cf_ln_b'] = nrm((n_cf, CF_INNER), 0.02)
    inputs['cf_pw2_w'] = nrm((n_cf, CF_INNER, d), CF_INNER ** -0.5 * DEEPNORM_BETA)
    inputs['cf_pw2_b'] = nrm((n_cf, d), 0.02)
    inputs['ln1_g'] = gain((DEPTH, d))
    inputs['ln1_b'] = nrm((DEPTH, d), 0.02)
    inputs['router_w'] = nrm((DEPTH, d, N_EXPERTS), d ** -0.5)
    inputs['router_b'] = nrm((DEPTH, N_EXPERTS), 0.01)
    inputs['e_w1'] = nrm((DEPTH, N_EXPERTS, d, 2 * D_FF), d ** -0.5)
    inputs['e_b1'] = nrm((DEPTH, N_EXPERTS, 2 * D_FF), 0.02)
    inputs['e_w2'] = nrm((DEPTH, N_EXPERTS, D_FF, d), D_FF ** -0.5 * DEEPNORM_BETA)
    inputs['e_b2'] = nrm((DEPTH, N_EXPERTS, d), 0.02)
    inputs['ln2_g'] = gain((DEPTH, d))
    inputs['ln2_b'] = nrm((DEPTH, d), 0.02)
    return inputs


def reference(x, c, ada_w, ada_b, dn_in_w, dn_conv_w, dn_A_log, dn_dt_bias, dn_onorm_w, dn_out_w,
              cf_pw1_w, cf_pw1_b, cf_dw_w, cf_dw_b, cf_ln_g, cf_ln_b, cf_pw2_w, cf_pw2_b,
              ln1_g, ln1_b, router_w, router_b, e_w1, e_b1, e_w2, e_b2, ln2_g, ln2_b):
    cond = jax.nn.silu(c)
    for i in range(DEPTH):
        mod = cond @ ada_w[i] + ada_b[i]
        sh1, sc1, gt1, sh2, sc2, gt2 = jnp.split(mod, 6, axis=-1)
        h = modulate(x, sh1, sc1)
        j = i // N_MIXERS
        if i % N_MIXERS == 0:
            y = gated_deltanet(h, dn_in_w[j], dn_conv_w[j], dn_A_log[j], dn_dt_bias[j],
                               dn_onorm_w[j], dn_out_w[j])
        else:
            y = conformer_conv(h, cf_pw1_w[j], cf_pw1_b[j], cf_dw_w[j], cf_dw_b[j],
                               cf_ln_g[j], cf_ln_b[j], cf_pw2_w[j], cf_pw2_b[j])
        x = layer_norm(DEEPNORM_ALPHA * x + (1.0 + gt1)[:, None, :] * y, ln1_g[i], ln1_b[i])
        h = modulate(x, sh2, sc2)
        y = moe(h, router_w[i], router_b[i], e_w1[i], e_b1[i], e_w2[i], e_b2[i])
        x = layer_norm(DEEPNORM_ALPHA * x + (1.0 + gt2)[:, None, :] * y, ln2_g[i], ln2_b[i])
    return x
```

```python
import contextlib
import numpy as np
import concourse.bass as bass
import concourse.mybir as mybir
from concourse.bass_utils import run_bass_kernel_spmd

F32 = mybir.dt.float32
BF16 = mybir.dt.bfloat16
ALU = mybir.AluOpType
AF = mybir.ActivationFunctionType

ENGS = ("pe", "dve", "act", "pool", "sp")
N_DMA_SEMS = 12

D = 1024
SEQ = 8192
DEPTH = 2
NEXP = 32
ALPHA = (2 * DEPTH) ** 0.25
LN_EPS = 1e-5
RMS_EPS = 1e-6
L2_EPS = 1e-6
LIMIT = 7.0
SW_ALPHA = 1.702


class V:
    def __init__(self, ap, key):
        self.ap, self.key = ap, key


class Buf:
    def __init__(self, nc, name, shape, dtype, psum=False):
        if psum:
            self.t = nc.alloc_psum_tensor(name, shape, dtype)
        else:
            self.t = nc.alloc_sbuf_tensor("sb_" + name, shape, dtype)
        self.key = name

    def __getitem__(self, idx):
        return V(self.t[idx], self.key)

    def v(self, idx, sub):
        return V(self.t[idx], (self.key, sub))


class Sched:
    def __init__(self, nc):
        self.nc = nc
        self.ops = []
        self.flushed = 0
        self.cnt = {e: 0 for e in ENGS}
        self.last_w = {}
        self.readers = {}
        self.dma_tot = [0] * (N_DMA_SEMS * 3)
        self.dma_last = [None] * (N_DMA_SEMS * 3)
        self.dma_rr = {"sp": 0, "act": 0, "pool": 0}
        self.opinfo = []
        self.esem = {e: nc.alloc_semaphore("s_" + e) for e in ENGS}
        self.dsem = [nc.alloc_semaphore("d%d" % i) for i in range(N_DMA_SEMS * 3)]
        self.known = {e: {} for e in ENGS}
        self.last_on = {e: None for e in ENGS}
        import os
        self.maxops = int(os.environ.get("KSTOP", "0")) or None
        self.marks = {}

    def mark(self, label):
        self.marks.setdefault(label, len(self.ops))

    def _deps(self, reads, writes):
        d = set()
        for k in reads:
            w = self.last_w.get(k)
            if w is not None:
                d.add(w)
        for k in writes:
            w = self.last_w.get(k)
            if w is not None:
                d.add(w)
            for r in self.readers.get(k, ()):
                d.add(r)
        return d

    def _commit(self, oid, reads, writes):
        for k in reads:
            self.readers.setdefault(k, []).append(oid)
        for k in writes:
            self.last_w[k] = oid
            self.readers[k] = []

    def op(self, eng, fn, reads=(), writes=()):
        if self.maxops is not None and len(self.ops) >= self.maxops:
            return -1
        reads = tuple(reads)
        writes = tuple(writes)
        deps = self._deps(reads, writes)
        oid = len(self.ops)
        self.cnt[eng] += 1
        self.opinfo.append(("c", eng, self.cnt[eng]))
        self.ops.append((eng, fn, deps, None))
        self._commit(oid, reads, writes)
        self.last_on[eng] = oid
        return oid

    def dma(self, fn, reads=(), writes=(), q="sp"):
        if self.maxops is not None and len(self.ops) >= self.maxops:
            return -1
        reads = tuple(reads)
        writes = tuple(writes)
        deps = self._deps(reads, writes)
        base = {"sp": 0, "act": N_DMA_SEMS, "pool": 2 * N_DMA_SEMS}[q]
        slot = base + self.dma_rr[q]
        self.dma_rr[q] = (self.dma_rr[q] + 1) % N_DMA_SEMS
        if self.dma_last[slot] is not None:
            deps.add(self.dma_last[slot])
        oid = len(self.ops)
        self.dma_tot[slot] += 16
        self.opinfo.append(("d", slot, self.dma_tot[slot]))
        self.dma_last[slot] = oid
        self.ops.append((q, fn, deps, slot))
        self._commit(oid, reads, writes)
        return oid

    def barrier(self):
        if self.maxops is not None and len(self.ops) >= self.maxops:
            return
        deps = set()
        for e in ENGS:
            if self.last_on[e] is not None:
                deps.add(self.last_on[e])
        for o in self.dma_last:
            if o is not None:
                deps.add(o)
        for e in ENGS:
            self.ops.append((e, None, set(deps), "bar"))
            self.opinfo.append(("n", None, 0))
        self.last_w = {}
        self.readers = {}

    def flush(self, final=False):
        nc = self.nc
        lo, hi = self.flushed, len(self.ops)
        self.flushed = hi
        per_eng = {e: [] for e in ENGS}
        for oid in range(lo, hi):
            per_eng[self.ops[oid][0]].append(oid)
        final_deps = []
        if final:
            for o in self.dma_last:
                if o is not None:
                    final_deps.append(o)

        def run(engname, engobj):
            known = self.known[engname]

            def wait_for(dlist, is_pe_compute):
                need = {}
                for d in dlist:
                    kind, a, v = self.opinfo[d]
                    if kind == "n":
                        continue
                    if kind == "c":
                        if a == "pe" and is_pe_compute:
                            continue
                        key = ("c", a)
                    else:
                        key = ("d", a)
                    if v > need.get(key, 0):
                        need[key] = v
                for key, v in need.items():
                    if known.get(key, 0) >= v:
                        continue
                    known[key] = v
                    sem = self.esem[key[1]] if key[0] == "c" else self.dsem[key[1]]
                    engobj.wait_ge(sem, v)

            for oid in per_eng[engname]:
                eng, fn, deps, slot = self.ops[oid]
                if slot == "bar":
                    wait_for(deps, False)
                    continue
                wait_for(deps, engname == "pe" and slot is None)
                ins = fn(engobj)
                if slot is None:
                    ins.then_inc(self.esem[engname], 1)
                else:
                    ins.then_inc(self.dsem[slot], 16)
            if final and engname == "sp":
                wait_for(final_deps, False)

        with nc.Block() as block:
            block.tensor(lambda e: run("pe", e))
            block.vector(lambda e: run("dve", e))
            block.scalar(lambda e: run("act", e))
            block.gpsimd(lambda e: run("pool", e))
            block.sync(lambda e: run("sp", e))

    @staticmethod
    def _k(*vs):
        return [v.key for v in vs if isinstance(v, V)]

    @staticmethod
    def _a(x):
        return x.ap if isinstance(x, V) else x

    def mm(self, out, lhsT, rhs, start=True, stop=True):
        return self.op("pe", lambda e: e.matmul(out.ap, lhsT.ap, rhs.ap, start=start, stop=stop),
                       reads=self._k(lhsT, rhs), writes=[out.key])

    def tr(self, out, in_, ident):
        return self.op("pe", lambda e: e.transpose(out.ap, in_.ap, ident.ap),
                       reads=self._k(in_, ident), writes=[out.key])

    def act(self, out, in_, func, bias=0.0, scale=1.0, accum=None, eng="act"):
        a = self._a
        kw = {}
        if accum is not None:
            kw["accum_out"] = accum.ap
        return self.op(eng, lambda e: e.activation(out.ap, in_.ap, func, bias=a(bias), scale=a(scale), **kw),
                       reads=self._k(in_, bias, scale), writes=self._k(out, accum))

    def ts(self, eng, out, in0, s1, s2, op0, op1=None):
        a = self._a
        if op1 is None:
            f = lambda e: e.tensor_scalar(out.ap, in0.ap, a(s1), None, op0)
        else:
            f = lambda e: e.tensor_scalar(out.ap, in0.ap, a(s1), a(s2), op0, op1)
        return self.op(eng, f, reads=self._k(in0, s1, s2), writes=[out.key])

    def tt(self, eng, out, in0, in1, op):
        return self.op(eng, lambda e: e.tensor_tensor(out.ap, in0.ap, in1.ap, op),
                       reads=self._k(in0, in1), writes=[out.key])

    def stt(self, eng, out, in0, s, in1, op0, op1):
        a = self._a
        eng = "dve"
        return self.op(eng, lambda e: e.scalar_tensor_tensor(out.ap, in0.ap, a(s), in1.ap, op0, op1),
                       reads=self._k(in0, s, in1), writes=[out.key])

    def copy(self, eng, out, in_):
        if eng == "act":
            return self.act(out, in_, AF.Copy)
        return self.op(eng, lambda e: e.tensor_copy(out.ap, in_.ap), reads=[in_.key], writes=[out.key])

    def memset(self, eng, out, val):
        return self.op(eng, lambda e: e.memset(out.ap, val), writes=[out.key])

    def load(self, out, src_ap, q="sp"):
        return self.dma(lambda e: e.dma_start(out=out.ap, in_=src_ap), writes=[out.key], q=q)

    def store(self, dst_ap, in_, q="sp", dkey=None):
        w = [dkey] if dkey is not None else []
        return self.dma(lambda e: e.dma_start(out=dst_ap, in_=in_.ap), reads=[in_.key], writes=w, q=q)


class PsumPool:
    def __init__(self, nc, n_big, prefix):
        self.banks = [Buf(nc, "%s_ps%d" % (prefix, b), [128, 512], F32, psum=True) for b in range(8)]
        self.configure(n_big)

    def configure(self, n_big):
        self.big = self.banks[:n_big]
        self.small = [(buf, q) for buf in self.banks[n_big:] for q in range(4)]
        self.bi = 0
        self.si = 0

    def bank(self):
        b = self.big[self.bi]
        self.bi = (self.bi + 1) % len(self.big)
        return b

    def quarter(self, w=128):
        nb = len(self.small) // 4
        i = self.si
        self.si = (self.si + 1) % len(self.small)
        buf, _ = self.small[(i % nb) * 4]
        q = (i // nb) % 4
        return V(buf.t[:, q * 128:q * 128 + w], buf.key)


def ada_broadcast(S, nc, PS, condB, ones_row, adaw_ap, adab_ap, col0, ncols, stage, outs, ident_unused=None):
    npieces = ncols // 512
    for pi in range(npieces):
        c0 = col0 + pi * 512
        st = stage
        S.load(V(st.t[:, :, :], st.key), adaw_ap[:, :, c0:c0 + 512])
        brow = outs["brow"]
        S.load(V(brow.t[0:1, 0:512], brow.key), adab_ap[0:1, c0:c0 + 512])
        ps = PS.bank()
        for kc in range(8):
            S.mm(ps[:, :], V(condB.t[:, kc, :], condB.key), V(st.t[:, kc, :], st.key), start=(kc == 0), stop=False)
        S.mm(ps[:, :], V(ones_row.t[0:1, :], ones_row.key), V(brow.t[0:1, 0:512], brow.key), start=False, stop=True)
        dst, add_one = outs["dst"][pi]
        if add_one:
            S.ts("dve", dst, ps[:, :], 1.0, None, ALU.add)
        else:
            S.copy("dve", dst, ps[:, :])


def build_phaseA(n_tok=SEQ, fz=None):
    if fz is None:
        nc = bass.Bass("TRN2", target_bir_lowering=False)
        dt = nc.dram_tensor
        x_d = dt("x", [n_tok, D], F32, kind="ExternalInput").ap()
        cb_d = dt("cb", [128, 8], F32, kind="ExternalInput").ap()
        adaw_d = dt("adaw", [128, 8, 2048], F32, kind="ExternalInput").ap()
        adab_d = dt("adab", [1, 2048], F32, kind="ExternalInput").ap()
        win_d = dt("win", [128, 8, 1544], F32, kind="ExternalInput").ap()
        cw_d = dt("cw", [128, 8, 4], F32, kind="ExternalInput").ap()
        alog_d = dt("alog", [128, 4], F32, kind="ExternalInput").ap()
        dtb_d = dt("dtb", [128, 4], F32, kind="ExternalInput").ap()
        onw_d = dt("onw", [128, 512], F32, kind="ExternalInput").ap()
        cst_d = dt("cst", [128, 4, 128], F32, kind="ExternalInput").ap()
        og_d = dt("og", [n_tok, 512], F32, kind="ExternalOutput").ap()
        S = Sched(nc)
        PS = PsumPool(nc, 4, "a")
    else:
        nc, S, PS = fz["nc"], fz["S"], fz["PS"]
        PS.configure(4)
        x_d, cb_d, cst_d = fz["x"], fz["cb"], fz["cst"]
        adaw_d, adab_d = fz["adaw"][0], fz["adab"][0]
        win_d, cw_d, alog_d, dtb_d, onw_d = fz["win"], fz["cw"], fz["alog"], fz["dtb"], fz["onw"]
        og_d = fz["ogpad"][128:128 + n_tok, :]
    scA = Scope(nc)
    scA.plain = True
    B = lambda name, shape, dtp=F32: scA.buf(name, shape, dtp)

    cst = B("cst", [128, 4, 128])
    ident = V(cst.t[:, 0, :], "cst")
    ones = V(cst.t[:, 1, :], "cst")
    U = V(cst.t[:, 2, :], "cst")
    LS = V(cst.t[:, 3, :], "cst")
    S.load(V(cst.t[:, :, :], "cst"), cst_d)
    win = B("win", [128, 8, 1544], BF16)
    for kc in range(8):
        S.load(V(win.t[:, kc, :], "win"), win_d[:, kc, :], q="pool")
    cw = B("cw", [128, 8, 4])
    S.load(V(cw.t[:, :, :], "cw"), cw_d)
    alog = B("alog", [128, 4]); dtb = B("dtb", [128, 4]); onw = B("onw", [128, 512])
    S.load(alog[:, :], alog_d); S.load(dtb[:, :], dtb_d); S.load(onw[:, :], onw_d)
    negA = B("negA", [128, 4])
    S.act(negA[:, :], alog[:, :], AF.Exp)
    S.ts("dve", negA[:, :], negA[:, :], -1.0, None, ALU.mult)

    cbt = B("cbt", [128, 8])
    S.load(cbt[:, :], cb_d)
    cond = B("cond", [128, 8])
    S.act(cond[:, :], cbt[:, :], AF.Silu)
    condB = B("condB", [128, 8, 128])
    for kc in range(8):
        S.ts("dve", V(condB.t[:, kc, :], "condB"), ones, V(cond.t[:, kc:kc + 1], "cond"), None, ALU.mult)
    h4 = B("h4", [128, 4, 1024])
    sh_bc = B("sh_bc", [128, 1024]); scp_bc = B("scp_bc", [128, 1024])
    brow = B("brow", [1, 512])
    stg = B("adastage", [128, 8, 512])
    outs = {"brow": brow, "dst": [(V(sh_bc.t[:, 0:512], "sh_bc"), False), (V(sh_bc.t[:, 512:1024], "sh_bc"), False),
                                  (V(scp_bc.t[:, 0:512], "scp_bc"), True), (V(scp_bc.t[:, 512:1024], "scp_bc"), True)]}
    ada_broadcast(S, nc, PS, condB, Buf_view(cst, 1), adaw_d, adab_d, 0, 2048, stg, outs)

    xt = [B("xt%d" % i, [128, 1024]) for i in range(2)]
    hT = B("hT", [128, 8, 512], BF16)
    pre = B("pre", [128, 8, 515])
    S.memset("dve", V(pre.t[:, :, :], ("pre", "all")), 0.0)
    cv = [B("cv%d" % i, [128, 512]) for i in range(2)]
    qkvs = B("qkvs", [128, 8, 512])
    rn = B("rn", [128, 512])
    vtok = B("vtok", [128, 512]); ktok = B("ktok", [128, 256]); gz = B("gz", [128, 512])
    og = [B("og%d" % i, [128, 512]) for i in range(2)]
    sm = B("sm", [128, 64])
    Sst = [[B("S%d_%d" % (h, i), [128, 128]) for i in range(2)] for h in range(4)]
    for h in range(4):
        S.memset("pool", Sst[h][0][:, :], 0.0)
    W = {}
    for h in range(4):
        for nm in ("rhsU", "Gs", "e1", "DLs", "e2", "DUi", "P0", "N0", "P1", "N1", "R", "vb", "kbg", "negwT",
                   "vnew", "eg", "qgT", "kd", "QKmT", "junk"):
            W[(h, nm)] = B("w%d_%s" % (h, nm), [128, 128])
        W[(h, "s")] = B("w%d_s" % h, [128, 8])
    S.barrier()

    nblk = n_tok // 512
    og_i = 0
    S.mark("setup_done")
    for blk in range(nblk):
        t0 = blk * 512
        for c in range(4):
            xb = xt[c % 2]
            S.load(xb[:, :], x_d[t0 + c * 128:t0 + (c + 1) * 128, :])
            hv = h4.v((slice(None), c, slice(None)), c)
            S.tt("dve", hv, xb[:, :], scp_bc[:, :], ALU.mult)
            S.tt("pool", hv, hv, sh_bc[:, :], ALU.add)
        S.mark("modulated")
        for kc in range(8):
            ps = PS.bank()
            for c in range(4):
                hv = h4.v((slice(None), c, slice(kc * 128, (kc + 1) * 128)), c)
                S.tr(ps[:, c * 128:(c + 1) * 128], hv, ident)
            S.copy("act" if kc % 2 else "dve", hT.v((slice(None), kc, slice(None)), kc), ps[:, :])
        S.mark("transposed")
        for fc in range(8):
            ps = PS.bank()
            for kc in range(8):
                S.mm(ps[:, :], V(win.t[:, kc, fc * 128:(fc + 1) * 128], "win"),
                     hT.v((slice(None), kc, slice(None)), kc), start=(kc == 0), stop=(kc == 7))
            pk = ("pre", fc) if blk > 0 or True else None
            S.copy("act", V(pre.t[:, fc, 3:515], ("pre", fc)), ps[:, :])
            c_ = cv[fc % 2]
            e2_ = "dve"
            S.ts(e2_, c_[:, :], V(pre.t[:, fc, 0:512], ("pre", fc)), V(cw.t[:, fc, 0:1], "cw"), None, ALU.mult)
            for j in range(1, 4):
                S.stt(e2_, c_[:, :], V(pre.t[:, fc, j:j + 512], ("pre", fc)), V(cw.t[:, fc, j:j + 1], "cw"),
                      c_[:, :], ALU.mult, ALU.add)
            S.act(qkvs.v((slice(None), fc, slice(None)), fc), c_[:, :], AF.Silu)
            S.copy("pool", V(pre.t[:, fc, 0:3], ("pre", fc)), V(pre.t[:, fc, 512:515], ("pre", fc)))
        S.mark("conv_done")
        for fc in range(4):
            qv = qkvs.v((slice(None), fc, slice(None)), fc)
            c_ = cv[fc % 2]
            S.act(c_[:, :], qv, AF.Square)
            ps = PS.bank()
            S.mm(ps[:, :], ones, c_[:, :])
            S.ts("dve", rn[:, :], ps[:, :], L2_EPS, None, ALU.add)
            S.act(rn[:, :], rn[:, :], AF.Sqrt)
            S.op("dve", lambda e: e.reciprocal(rn.t[:, :], rn.t[:, :]), reads=["rn"], writes=["rn"])
            if fc < 2:
                S.stt("dve", qv, qv, 128.0 ** -0.5, rn[:, :], ALU.mult, ALU.mult)
            else:
                S.tt("dve", qv, qv, rn[:, :], ALU.mult)

        S.mark("l2norm_done")
        for c in range(4):
            cs = slice(c * 128, (c + 1) * 128)
            ps = PS.bank()
            for h in range(4):
                S.tr(ps[:, h * 128:(h + 1) * 128], qkvs.v((slice(None), 4 + h, cs), 4 + h), ident)
            S.copy("act", vtok[:, :], ps[:, :])
            ps = PS.bank()
            for qh in range(2):
                S.tr(ps[:, qh * 128:(qh + 1) * 128], qkvs.v((slice(None), 2 + qh, cs), 2 + qh), ident)
            S.copy("dve", ktok[:, :], ps[:, 0:256])
            ps = PS.bank()
            for kc in range(8):
                S.mm(ps[:, :], hT.v((slice(None), kc, cs), kc), V(win.t[:, kc, 1024:1536], "win"),
                     start=(kc == 0), stop=(kc == 7))
            S.act(gz[:, :], ps[:, :], AF.Silu)
            S.tt("pool", gz[:, :], gz[:, :], onw[:, :], ALU.mult)
            pq = PS.quarter(8)
            for kc in range(8):
                S.mm(pq, hT.v((slice(None), kc, cs), kc), V(win.t[:, kc, 1536:1544], "win"),
                     start=(kc == 0), stop=(kc == 7))
            smv = lambda a, b: V(sm.t[:, a:b], "sm")
            S.copy("dve", smv(32, 40), pq)
            S.act(smv(0, 4), smv(32, 36), AF.Sigmoid)
            S.ts("dve", smv(4, 8), smv(0, 4), -1.0, None, ALU.mult)
            S.tt("dve", smv(8, 12), smv(36, 40), dtb[:, :], ALU.add)
            S.act(smv(8, 12), smv(8, 12), AF.Exp)
            S.act(smv(8, 12), smv(8, 12), AF.Ln, bias=1.0)
            S.tt("dve", smv(12, 16), smv(8, 12), negA[:, :], ALU.mult)
            pq2 = PS.quarter(4)
            S.mm(pq2, U, smv(12, 16))
            S.copy("dve", smv(16, 20), pq2)
            S.act(smv(20, 24), smv(16, 20), AF.Exp)
            S.tt("dve", smv(24, 28), smv(0, 4), smv(20, 24), ALU.mult)

            S.mark("chunkprep_done")
            KK = []
            QKT = []
            for qh in range(2):
                kT = qkvs.v((slice(None), 2 + qh, cs), 2 + qh)
                qT = qkvs.v((slice(None), qh, cs), qh)
                p1 = PS.quarter()
                S.mm(p1, kT, kT)
                p2 = PS.quarter()
                S.mm(p2, kT, qT)
                KK.append(p1)
                QKT.append(p2)
            w = lambda h, nm: W[(h, nm)][:, :]
            Nps = {}
            for h in range(4):
                qh = h // 2
                gcol = V(sm.t[:, 16 + h:17 + h], "sm")
                S.act(w(h, "rhsU"), U, AF.Copy, scale=V(sm.t[:, 12 + h:13 + h], "sm"))
                pg = PS.quarter()
                S.mm(pg, ones, w(h, "rhsU"))
                S.copy("act", w(h, "Gs"), pg)
                S.ts("dve", w(h, "e1"), w(h, "Gs"), gcol, 0.0, ALU.subtract, ALU.max)
                S.act(w(h, "e1"), w(h, "e1"), AF.Exp, scale=-1.0)
                S.tt("pool", w(h, "DLs"), w(h, "e1"), LS, ALU.mult)
                S.ts("dve", w(h, "e2"), w(h, "Gs"), gcol, 0.0, ALU.subtract, ALU.min)
                S.act(w(h, "e2"), w(h, "e2"), AF.Exp)
                S.tt("pool", w(h, "DUi"), w(h, "e2"), U, ALU.mult)
                S.stt("dve", w(h, "P0"), KK[qh], V(sm.t[:, 4 + h:5 + h], "sm"), w(h, "DLs"), ALU.mult, ALU.mult)
                pn = PS.quarter()
                S.tr(pn, w(h, "P0"), ident)
                S.copy("act", w(h, "N0"), pn)
                S.tt("dve", w(h, "R"), w(h, "N0"), ident, ALU.add)
                S.tt("dve", w(h, "QKmT"), QKT[qh], w(h, "DUi"), ALU.mult)
                S.act(w(h, "vb"), V(vtok.t[:, h * 128:(h + 1) * 128], "vtok"), AF.Copy, scale=V(sm.t[:, h:h + 1], "sm"))
                S.act(w(h, "kbg"), V(ktok.t[:, qh * 128:(qh + 1) * 128], "ktok"), AF.Copy, scale=V(sm.t[:, 24 + h:25 + h], "sm"))
                S.act(w(h, "eg"), w(h, "Gs"), AF.Exp)
                S.tt("pool", w(h, "qgT"), qkvs.v((slice(None), qh, cs), qh), w(h, "eg"), ALU.mult)
                sv = lambda a, b, h=h: V(W[(h, "s")].t[:, a:b], W[(h, "s")].key)
                S.act(sv(0, 1), gcol, AF.Exp, bias=V(W[(h, "Gs")].t[:, 127:128], W[(h, "Gs")].key), scale=-1.0)
                S.act(w(h, "kd"), V(ktok.t[:, qh * 128:(qh + 1) * 128], "ktok"), AF.Copy, scale=sv(0, 1))
                S.act(sv(1, 2), V(W[(h, "Gs")].t[:, 127:128], W[(h, "Gs")].key), AF.Exp)
            S.mark("stage1_done")
            cur = {h: ("P0", "N0") for h in range(4)}
            for m in range(1, 7):
                for h in range(4):
                    Pn, Nn = cur[h]
                    Pq, Nq = ("P1", "N1") if Pn == "P0" else ("P0", "N0")
                    pp = PS.quarter()
                    S.mm(pp, w(h, Nn), w(h, Pn))
                    if m < 6:
                        pnn = PS.quarter()
                        S.mm(pnn, w(h, Pn), w(h, Nn))
                    S.copy("act", w(h, Pq), pp)
                    if m < 6:
                        S.copy("dve", w(h, Nq), pnn)
                    pr = PS.quarter()
                    S.mm(pr, w(h, Pq), w(h, "R"))
                    S.tt("dve", w(h, "R"), pr, w(h, "R"), ALU.add)
                    cur[h] = (Pq, Nq)
            S.mark("stage2_done")
            ogb = og[og_i % 2]
            og_i += 1
            for h in range(4):
                pw = PS.quarter()
                S.mm(pw, w(h, "kbg"), w(h, "R"))
                S.act(w(h, "negwT"), pw, AF.Copy, scale=-1.0)
            for h in range(4):
                Sc = Sst[h][(blk * 4 + c) % 2]
                Sn = Sst[h][(blk * 4 + c + 1) % 2]
                sv = lambda a, b, h=h: V(W[(h, "s")].t[:, a:b], W[(h, "s")].key)
                pv = PS.quarter()
                S.mm(pv, w(h, "R"), w(h, "vb"), start=True, stop=False)
                S.mm(pv, w(h, "negwT"), Sc[:, :], start=False, stop=True)
                S.copy("dve", w(h, "vnew"), pv)
                po = PS.quarter()
                S.mm(po, w(h, "qgT"), Sc[:, :], start=True, stop=False)
                S.mm(po, w(h, "QKmT"), w(h, "vnew"), start=False, stop=True)
                pu = PS.quarter()
                S.mm(pu, w(h, "kd"), w(h, "vnew"))
                S.stt("dve", Sn[:, :], Sc[:, :], sv(1, 2), pu, ALU.mult, ALU.add)
                S.copy("dve", w(h, "e1"), po)
                S.act(w(h, "junk"), w(h, "e1"), AF.Square, accum=sv(2, 3))
                S.ts("dve", sv(3, 4), sv(2, 3), 1.0 / 128.0, RMS_EPS, ALU.mult, ALU.add)
                S.act(sv(3, 4), sv(3, 4), AF.Sqrt)
                S.op("dve", lambda e, h=h: e.reciprocal(W[(h, "s")].t[:, 3:4], W[(h, "s")].t[:, 3:4]),
                     reads=[W[(h, "s")].key], writes=[W[(h, "s")].key])
                S.stt("dve", V(ogb.t[:, h * 128:(h + 1) * 128], ogb.key), w(h, "e1"), sv(3, 4),
                      V(gz.t[:, h * 128:(h + 1) * 128], "gz"), ALU.mult, ALU.mult)
            S.store(og_d[t0 + c * 128:t0 + (c + 1) * 128, :], ogb[:, :])
            S.mark("chunk0_done")
    if fz is None:
        S.flush(final=True)
        return nc
    zt = V(W[(0, "junk")].t[:, :], W[(0, "junk")].key)
    S.memset("dve", zt, 0.0)
    for q4 in range(4):
        S.store(fz["ogpad"][0:128, q4 * 128:(q4 + 1) * 128], zt)
    scA.close(S)
    return nc


def Buf_view(buf, idx):
    b = Buf.__new__(Buf)
    b.t = buf.t[:, idx, :]
    b.key = buf.key
    return b


def consts_np():
    i = np.arange(128)
    ident = np.eye(128, dtype=np.float32)
    ones = np.ones((128, 128), np.float32)
    U = (i[:, None] <= i[None, :]).astype(np.float32)
    LS = (i[:, None] > i[None, :]).astype(np.float32)
    return np.ascontiguousarray(np.stack([ident, ones, U, LS], axis=1))


def pkc(w):
    K, F = w.shape
    return np.ascontiguousarray(w.reshape(K // 128, 128, F).transpose(1, 0, 2))


def prep_phaseA(inp, b, hg, n_tok=SEQ):
    f = np.float32
    in_w = inp["dn_in_w"][0]
    cols = np.concatenate([np.arange(256 * hg, 256 * hg + 256), 1024 + np.arange(256 * hg, 256 * hg + 256),
                           2048 + np.arange(512 * hg, 512 * hg + 512), 4096 + np.arange(512 * hg, 512 * hg + 512),
                           6144 + np.arange(4 * hg, 4 * hg + 4), 6160 + np.arange(4 * hg, 4 * hg + 4)])
    ccols = cols[:1024]
    cw = inp["dn_conv_w"][0][:, ccols]
    cw = np.ascontiguousarray(cw.reshape(4, 8, 128).transpose(2, 1, 0))
    hs = slice(4 * hg, 4 * hg + 4)
    return {
        "x": np.ascontiguousarray(inp["x"][b, :n_tok]).astype(f),
        "cb": np.ascontiguousarray(inp["c"][b].reshape(8, 128).T),
        "adaw": pkc(inp["ada_w"][0][:, :2048]),
        "adab": np.ascontiguousarray(inp["ada_b"][0][None, :2048]),
        "win": pkc(in_w[:, cols]),
        "cw": cw,
        "alog": np.ascontiguousarray(np.broadcast_to(inp["dn_A_log"][0][hs][None, :], (128, 4))),
        "dtb": np.ascontiguousarray(np.broadcast_to(inp["dn_dt_bias"][0][hs][None, :], (128, 4))),
        "onw": np.ascontiguousarray(np.broadcast_to(np.tile(inp["dn_onorm_w"][0], 4)[None, :], (128, 512))),
        "cst": consts_np(),
    }


class Scope:
    _n = 0

    def __init__(self, nc):
        self.nc = nc
        self.st = contextlib.ExitStack()
        Scope._n += 1
        self.sfx = "_s%d" % Scope._n
        self.plain = False

    def buf(self, name, shape, dtp=F32):
        b = Buf.__new__(Buf)
        b.t = self.st.enter_context(self.nc.sbuf_tensor("sb_" + name + self.sfx, shape, dtp))
        b.key = name if self.plain else name + self.sfx
        return b

    def close(self, S):
        S.barrier()
        S.flush()
        self.st.close()


NTILE = 9
NTOK = NTILE * 128
OGROWS = SEQ + 128


def declare_B(nc, with_og=True):
    dt = nc.dram_tensor
    I = lambda name, shape: dt(name, shape, F32, kind="ExternalInput").ap()
    d = {}
    d["xh"] = I("xh", [2, NTOK, D])
    if with_og:
        d["ogh"] = I("ogh", [2, NTOK, 2048])
    d["flag"] = I("flag", [128, 2])
    d["cb"] = I("cb", [128, 8])
    d["adaw"] = I("adaw", [2, 128, 8, 6144])
    d["adab"] = I("adab", [2, 1, 6144])
    d["outw"] = I("outw", [128, 16, 1024])
    d["pw1w"] = I("pw1w", [128, 8, 2048])
    d["pw1b"] = I("pw1b", [128, 16])
    d["dww"] = I("dww", [128, 8, 31])
    d["dwb"] = I("dwb", [128, 8])
    d["cfg"] = I("cfg", [128, 8])
    d["cfb"] = I("cfb", [128, 8])
    d["pw2w"] = I("pw2w", [128, 8, 1024])
    d["pw2b"] = I("pw2b", [1, 1024])
    d["lnp"] = I("lnp", [2, 4, 128, 1024])
    d["rw"] = I("rw", [2, 128, 8, 32])
    d["rb"] = I("rb", [2, 1, 32])
    d["ew1"] = I("ew1", [2, NEXP, 1024, 2048])
    d["eb1"] = I("eb1", [2, 4, 128, 128])
    d["ew2"] = I("ew2", [2, NEXP, 1024, 1024])
    d["eb2"] = I("eb2", [2, NEXP, 1024])
    d["cst"] = I("cst", [128, 4, 128])
    d["out"] = dt("out", [2, 1024, D], F32, kind="ExternalOutput").ap()
    d["modbc"] = dt("modbc", [2, 6, 128, 1024], F32).ap()
    return d


def build_phaseB(n_exp=NEXP, layers=(0, 1), npass=2, fz=None):
    if fz is None:
        nc = bass.Bass("TRN2", target_bir_lowering=False)
        d = declare_B(nc)
    else:
        nc = fz["nc"]
        d = fz
    xh_d, flag_d, cb_d, adaw_d, adab_d = d["xh"], d["flag"], d["cb"], d["adaw"], d["adab"]
    og_d = d.get("ogh")
    outw_d, pw1w_d, pw1b_d, dww_d, dwb_d, cfg_d, cfb_d = (d[k] for k in ("outw", "pw1w", "pw1b", "dww", "dwb", "cfg", "cfb"))
    pw2w_d, pw2b_d, lnp_d, rw_d, rb_d = (d[k] for k in ("pw2w", "pw2b", "lnp", "rw", "rb"))
    ew1_d, eb1_d, ew2_d, eb2_d, cst_d, out_d, modbc_d = (d[k] for k in ("ew1", "eb1", "ew2", "eb2", "cst", "out", "modbc"))

    if fz is None:
        S = Sched(nc)
        PS = PsumPool(nc, 6, "b")
    else:
        S, PS = fz["S"], fz["PS"]
        PS.configure(6)
    B = lambda name, shape, dtp=F32: Buf(nc, "b_" + name, shape, dtp)
    cst = B("cst", [128, 4, 128])
    cst.key = "cst"
    ident = V(cst.t[:, 0, :], "cst")
    ones = V(cst.t[:, 1, :], "cst")
    ones_row = V(cst.t[0:1, 1, :], "cst")
    S.load(V(cst.t[:, :, :], "cst"), cst_d)
    flag = B("flag", [128, 2]); flag.key = "flag"
    S.load(flag[:, :], flag_d)
    acc = B("acc", [128, NTILE, 1024]); acc.key = "acc"
    hT = B("hT", [128, 8, NTOK], BF16); hT.key = "hT"
    gates = B("gates", [128, NTILE, 32]); gates.key = "gates"
    gT = B("gT", [32, NTOK]); gT.key = "gT"
    lns = B("lns", [128, NTILE, 16]); lns.key = "lns"
    if fz is not None:
        sel = B("sel", [128, 4]); sel.key = "sel"
        S.load(sel[:, :], fz["sel"])

    A = lambda t, half=None: (acc.v((slice(None), t, slice(None)), t) if half is None else
                              acc.v((slice(None), t, slice(half * 512, half * 512 + 512)), t))

    def ld(out, src, rkey=None, q="sp"):
        r = [rkey] if rkey is not None else []
        return S.dma(lambda e: e.dma_start(out=out.ap, in_=src), reads=r, writes=[out.key], q=q)

    sc = Scope(nc)
    cbt = sc.buf("cbt", [128, 8]); cond = sc.buf("cond", [128, 8]); condB = sc.buf("condB", [128, 8, 128])
    ld(cbt[:, :], cb_d)
    S.act(cond[:, :], cbt[:, :], AF.Silu)
    for kc in range(8):
        S.ts("dve", V(condB.t[:, kc, :], condB.key), ones, V(cond.t[:, kc:kc + 1], cond.key), None, ALU.mult)
    stg = [sc.buf("stg%d" % i, [128, 8, 512]) for i in range(2)]
    brow = [sc.buf("brow%d" % i, [1, 512]) for i in range(2)]
    mtmp = [sc.buf("mtmp%d" % i, [128, 512]) for i in range(2)]
    pi = 0
    for l in range(2):
        for ch in range(6):
            for half in range(2):
                c0 = ch * 1024 + half * 512
                st_, br_, mt_ = stg[pi % 2], brow[pi % 2], mtmp[pi % 2]
                pi += 1
                ld(V(st_.t[:, :, :], st_.key), adaw_d[l, :, :, c0:c0 + 512])
                ld(V(br_.t[0:1, :], br_.key), adab_d[l, 0:1, c0:c0 + 512])
                ps = PS.bank()
                for kc in range(8):
                    S.mm(ps[:, :], V(condB.t[:, kc, :], condB.key), V(st_.t[:, kc, :], st_.key), start=(kc == 0), stop=False)
                S.mm(ps[:, :], ones_row, V(br_.t[0:1, :], br_.key), start=False, stop=True)
                if ch in (1, 2, 4, 5):
                    S.ts("dve", mt_[:, :], ps[:, :], 1.0, None, ALU.add)
                else:
                    S.copy("dve", mt_[:, :], ps[:, :])
                S.store(modbc_d[l, ch, :, half * 512:half * 512 + 512], mt_[:, :], dkey=("modbc", l, ch, half))
    sc.close(S)

    def ln_tile(t):
        sv = lambda a, b: V(lns.t[:, t, a:b], ("lns", t))
        S.op("dve", lambda e: e.bn_stats(lns.t[:, t, 0:6], acc.t[:, t, 0:512]), reads=[("acc", t)], writes=[("lns", t)])
        S.op("dve", lambda e: e.bn_stats(lns.t[:, t, 6:12], acc.t[:, t, 512:1024]), reads=[("acc", t)], writes=[("lns", t)])
        S.op("dve", lambda e: e.bn_aggr(lns.t[:, t, 12:14], lns.t[:, t, 0:12]), reads=[("lns", t)], writes=[("lns", t)])
        S.ts("dve", sv(14, 15), sv(13, 14), LN_EPS, None, ALU.add)
        S.act(sv(14, 15), sv(14, 15), AF.Sqrt)
        S.op("dve", lambda e: e.reciprocal(lns.t[:, t, 14:15], lns.t[:, t, 14:15]), reads=[("lns", t)], writes=[("lns", t)])
        S.ts("dve", A(t), A(t), sv(12, 13), sv(14, 15), ALU.subtract, ALU.mult)

    def transpose_tile(src, t, hTf=None):
        for b2 in range(2):
            ps = PS.bank()
            for k4 in range(4):
                kc = b2 * 4 + k4
                S.tr(ps[:, k4 * 128:(k4 + 1) * 128], V(src.t[:, kc * 128:(kc + 1) * 128], src.key), ident)
            psv = V(ps.t[:, :].rearrange("p (k t) -> p k t", k=4), ps.key)
            if hTf is not None:
                S.copy("act", V(hTf.t[:, b2 * 4:b2 * 4 + 4, :], hTf.key), psv)
                S.copy("pool", V(hT.t[:, b2 * 4:b2 * 4 + 4, t * 128:(t + 1) * 128], ("hT", t)),
                       V(hTf.t[:, b2 * 4:b2 * 4 + 4, :], hTf.key))
            else:
                S.copy("act" if b2 else "dve", V(hT.t[:, b2 * 4:b2 * 4 + 4, t * 128:(t + 1) * 128], ("hT", t)), psv)

    def bc_tile(sc_, name, src, rkey=None):
        b = sc_.buf(name, [128, 1024])
        ld(b[:, :], src, rkey)
        return b

    def mix0(p):
        sc = Scope(nc)
        outw = sc.buf("outw", [128, 16, 1024], BF16)
        for fc in range(16):
            ld(V(outw.t[:, fc, :], outw.key), outw_d[:, fc, :], q="pool")
        gt1p = bc_tile(sc, "gt1p", modbc_d[0, 2])
        ogt = [sc.buf("ogt%d" % i, [128, 2048]) for i in range(2)]
        cand = [sc.buf("cand%d" % i, [128, 2048]) for i in range(2)] if fz is not None else None
        ogT = [sc.buf("ogT%d" % i, [128, 2048], BF16) for i in range(2)]
        xt = [sc.buf("xt%d" % i, [128, 1024]) for i in range(2)]
        t1 = [sc.buf("t1%d" % i, [128, 512]) for i in range(2)]
        for t in range(NTILE):
            o_, oT, x_ = ogt[t % 2], ogT[t % 2], xt[t % 2]
            if fz is None:
                ld(o_[:, :], og_d[p, t * 128:(t + 1) * 128, :])
            else:
                for sp in range(4):
                    cd = cand[sp % 2]
                    r0 = sp * 2048 + p * 1024 + t * 128
                    for hg in range(4):
                        S.dma(lambda e, cd=cd, hg=hg, r0=r0: e.dma_start(
                            out=cd.t[:, hg * 512:(hg + 1) * 512], in_=fz["ogg"][hg * OGROWS + r0:hg * OGROWS + r0 + 128, :]),
                            reads=["ogg"], writes=[cd.key])
                    if sp == 0:
                        S.ts("dve", o_[:, :], cd[:, :], V(sel.t[:, 0:1], "sel"), None, ALU.mult)
                    else:
                        S.stt("dve", o_[:, :], cd[:, :], V(sel.t[:, sp:sp + 1], "sel"), o_[:, :], ALU.mult, ALU.add)
            ld(x_[:, :], xh_d[p, t * 128:(t + 1) * 128, :])
            for b4 in range(4):
                ps = PS.bank()
                for k4 in range(4):
                    fc = b4 * 4 + k4
                    S.tr(ps[:, k4 * 128:(k4 + 1) * 128], V(o_.t[:, fc * 128:(fc + 1) * 128], o_.key), ident)
                S.copy("act" if b4 % 2 else "dve", V(oT.t[:, b4 * 512:(b4 + 1) * 512], oT.key), ps[:, :])
            for half in range(2):
                ps = PS.bank()
                for fc in range(16):
                    S.mm(ps[:, :], V(oT.t[:, fc * 128:(fc + 1) * 128], oT.key),
                         V(outw.t[:, fc, half * 512:(half + 1) * 512], outw.key), start=(fc == 0), stop=(fc == 15))
                tt_ = t1[half]
                S.tt("dve", tt_[:, :], ps[:, :], V(gt1p.t[:, half * 512:(half + 1) * 512], gt1p.key), ALU.mult)
                S.stt("dve", A(t, half), V(x_.t[:, half * 512:(half + 1) * 512], x_.key), ALPHA, tt_[:, :], ALU.mult, ALU.add)
        sc.close(S)

    def mix1(p):
        sc = Scope(nc)
        pw1w = sc.buf("pw1w", [128, 8, 2048], BF16)
        pw2w = sc.buf("pw2w", [128, 8, 1024], BF16)
        for kc in range(8):
            ld(V(pw1w.t[:, kc, :], pw1w.key), pw1w_d[:, kc, :], q="pool")
            ld(V(pw2w.t[:, kc, :], pw2w.key), pw2w_d[:, kc, :], q="pool")
        pw1b = sc.buf("pw1b", [128, 16]); dww = sc.buf("dww", [128, 8, 31]); dwb = sc.buf("dwb", [128, 8])
        cfg = sc.buf("cfg", [128, 8]); cfb = sc.buf("cfb", [128, 8]); pw2b = sc.buf("pw2b", [1, 1024])
        ld(pw1b[:, :], pw1b_d); ld(V(dww.t[:, :, :], dww.key), dww_d); ld(dwb[:, :], dwb_d)
        ld(cfg[:, :], cfg_d); ld(cfb[:, :], cfb_d); ld(V(pw2b.t[0:1, :], pw2b.key), pw2b_d)
        gt1p = bc_tile(sc, "gt1p", modbc_d[1, 2])
        ut = [sc.buf("ut%d" % i, [128, NTOK]) for i in range(2)]
        sg = [sc.buf("sg%d" % i, [128, 512]) for i in range(2)]
        cvT = sc.buf("cvT", [128, 8, 1024])
        sT = sc.buf("sT", [128, 8, 1024], BF16)
        mean = sc.buf("mean", [128, 1024]); rstd = sc.buf("rstd", [128, 1024]); m2 = sc.buf("m2", [128, 512])
        groups = [(0, 128), (128, 640), (640, 1152)]
        gi = 0
        for fc in range(8):
            u_ = ut[fc % 2]
            for (g0, g1) in groups:
                n = g1 - g0
                psa = PS.bank(); psg = PS.bank()
                for kc in range(8):
                    S.mm(psa[:, 0:n], V(pw1w.t[:, kc, fc * 128:(fc + 1) * 128], pw1w.key),
                         V(hT.t[:, kc, g0:g1], ("hT", "all")), start=(kc == 0), stop=(kc == 7))
                for kc in range(8):
                    S.mm(psg[:, 0:n], V(pw1w.t[:, kc, 1024 + fc * 128:1024 + (fc + 1) * 128], pw1w.key),
                         V(hT.t[:, kc, g0:g1], ("hT", "all")), start=(kc == 0), stop=(kc == 7))
                s_ = sg[gi % 2]; gi += 1
                S.act(V(s_.t[:, 0:n], s_.key), psg[:, 0:n], AF.Sigmoid, bias=V(pw1b.t[:, 8 + fc:9 + fc], pw1b.key))
                S.stt("dve", V(u_.t[:, g0:g1], u_.key), psa[:, 0:n], V(pw1b.t[:, fc:fc + 1], pw1b.key),
                      V(s_.t[:, 0:n], s_.key), ALU.add, ALU.mult)
            S.ts("dve", V(u_.t[:, 0:128], u_.key), V(u_.t[:, 0:128], u_.key), V(flag.t[:, p:p + 1], "flag"), None, ALU.mult)
            cv = V(cvT.t[:, fc, :], (cvT.key, fc))
            S.ts("dve", cv, V(u_.t[:, 98:98 + 1024], u_.key), V(dww.t[:, fc, 0:1], dww.key),
                 V(dwb.t[:, fc:fc + 1], dwb.key), ALU.mult, ALU.add)
            for j in range(1, 31):
                S.stt("dve", cv, V(u_.t[:, 98 + j:98 + j + 1024], u_.key), V(dww.t[:, fc, j:j + 1], dww.key), cv,
                      ALU.mult, ALU.add)
        for tg in range(2):
            ts_ = slice(tg * 512, (tg + 1) * 512)
            pss = PS.bank(); psq = PS.bank()
            for fc in range(8):
                cvs = V(cvT.t[:, fc, ts_], (cvT.key, fc))
                S.mm(pss[:, :], ones, cvs, start=(fc == 0), stop=(fc == 7))
                s_ = sg[fc % 2]
                S.act(s_[:, :], cvs, AF.Square)
                S.mm(psq[:, :], ones, s_[:, :], start=(fc == 0), stop=(fc == 7))
            mv = V(mean.t[:, ts_], (mean.key, tg)); rv = V(rstd.t[:, ts_], (rstd.key, tg))
            S.ts("dve", mv, pss[:, :], 1.0 / 1024.0, None, ALU.mult)
            S.tt("pool", m2[:, :], mv, mv, ALU.mult)
            S.stt("dve", rv, psq[:, :], 1.0 / 1024.0, m2[:, :], ALU.mult, ALU.subtract)
            S.ts("dve", rv, rv, LN_EPS, None, ALU.add)
            S.act(rv, rv, AF.Sqrt)
            S.op("dve", lambda e, ts_=ts_: e.reciprocal(rstd.t[:, ts_], rstd.t[:, ts_]), reads=[rv.key], writes=[rv.key])
            for fc in range(8):
                cvs = V(cvT.t[:, fc, ts_], (cvT.key, fc))
                S.tt("dve", cvs, cvs, mv, ALU.subtract)
                S.tt("pool", cvs, cvs, rv, ALU.mult)
                S.act(V(sT.t[:, fc, ts_], (sT.key, fc)), cvs, AF.Silu, bias=V(cfb.t[:, fc:fc + 1], cfb.key),
                      scale=V(cfg.t[:, fc:fc + 1], cfg.key))
        t1 = [sc.buf("t1%d" % i, [128, 512]) for i in range(2)]
        for t in range(1, NTILE):
            m0 = (t - 1) * 128
            for half in range(2):
                ps = PS.bank()
                for fc in range(8):
                    S.mm(ps[:, :], V(sT.t[:, fc, m0:m0 + 128], (sT.key, fc)),
                         V(pw2w.t[:, fc, half * 512:(half + 1) * 512], pw2w.key), start=(fc == 0), stop=False)
                S.mm(ps[:, :], ones_row, V(pw2b.t[0:1, half * 512:(half + 1) * 512], pw2b.key), start=False, stop=True)
                tt_ = t1[half]
                S.tt("dve", tt_[:, :], ps[:, :], V(gt1p.t[:, half * 512:(half + 1) * 512], gt1p.key), ALU.mult)
                S.tt("pool", A(t, half), A(t, half), tt_[:, :], ALU.add)
        sc.close(S)

    def post(l, p):
        tiles = list(range(NTILE)) if l == 0 else list(range(1, NTILE))
        sc = Scope(nc)
        lg = bc_tile(sc, "lg", lnp_d[l, 0]); lb = bc_tile(sc, "lb", lnp_d[l, 1])
        G2 = bc_tile(sc, "G2", modbc_d[l, 4]); B2 = bc_tile(sc, "B2", modbc_d[l, 3])
        S.tt("dve", B2[:, :], B2[:, :], B2[:, :], ALU.bypass) if False else None
        tmpb = sc.buf("tmpb", [128, 1024])
        S.tt("dve", tmpb[:, :], lb[:, :], G2[:, :], ALU.mult)
        S.tt("dve", B2[:, :], B2[:, :], tmpb[:, :], ALU.add)
        S.tt("dve", G2[:, :], G2[:, :], lg[:, :], ALU.mult)
        S.ts("dve", lg[:, :], lg[:, :], ALPHA, None, ALU.mult)
        S.ts("dve", lb[:, :], lb[:, :], ALPHA, None, ALU.mult)
        rw = sc.buf("rw", [128, 8, 32]); rb = sc.buf("rb", [1, 32])
        ld(V(rw.t[:, :, :], rw.key), rw_d[l]); ld(V(rb.t[0:1, :], rb.key), rb_d[l])
        h2 = [sc.buf("h2%d" % i, [128, 1024]) for i in range(2)]
        hTf = [sc.buf("hTf%d" % i, [128, 8, 128]) for i in range(2)]
        rt = sc.buf("rt", [128, NTILE, 128])
        for t in tiles:
            ln_tile(t)
            h_ = h2[t % 2]; hf = hTf[t % 2]
            S.tt("dve", h_[:, :], A(t), G2[:, :], ALU.mult)
            S.tt("pool", h_[:, :], h_[:, :], B2[:, :], ALU.add)
            S.tt("dve", A(t), A(t), lg[:, :], ALU.mult)
            S.tt("pool", A(t), A(t), lb[:, :], ALU.add)
            transpose_tile(h_, t, hf)
            pq = PS.quarter(32)
            for kc in range(8):
                S.mm(pq, V(hf.t[:, kc, :], hf.key), V(rw.t[:, kc, :], rw.key), start=(kc == 0), stop=False)
            S.mm(pq, ones_row, V(rb.t[0:1, :], rb.key), start=False, stop=True)
            r = lambda a, b, t=t: V(rt.t[:, t, a:b], (rt.key, t))
            S.copy("dve", r(0, 32), pq)
            S.op("dve", lambda e, t=t: e.max(rt.t[:, t, 32:40], rt.t[:, t, 0:32]), reads=[(rt.key, t)], writes=[(rt.key, t)])
            S.ts("dve", r(40, 72), r(0, 32), r(35, 36), None, ALU.is_ge)
            S.ts("dve", r(72, 73), r(32, 33), -1.0, None, ALU.mult)
            S.act(r(80, 112), r(0, 32), AF.Exp, bias=r(72, 73))
            S.tt("dve", r(80, 112), r(80, 112), r(40, 72), ALU.mult)
            S.op("dve", lambda e, t=t: e.reduce_sum(rt.t[:, t, 73:74], rt.t[:, t, 80:112], mybir.AxisListType.X),
                 reads=[(rt.key, t)], writes=[(rt.key, t)])
            S.op("dve", lambda e, t=t: e.reciprocal(rt.t[:, t, 74:75], rt.t[:, t, 73:74]), reads=[(rt.key, t)], writes=[(rt.key, t)])
            gv = V(gates.t[:, t, :], ("gates", t))
            S.ts("dve", gv, r(80, 112), r(74, 75), None, ALU.mult)
            pq2 = PS.quarter(128)
            S.tr(V(pq2.ap[0:32, :], pq2.key), gv, ident)
            S.copy("act", V(gT.t[0:32, t * 128:(t + 1) * 128], ("gT", t)), V(pq2.ap[0:32, :], pq2.key))
        sc.close(S)

        sc = Scope(nc)
        gt2p = bc_tile(sc, "gt2p", modbc_d[l, 5])
        b2s = sc.buf("b2s", [32, 1024])
        ld(V(b2s.t[0:32, :], b2s.key), eb2_d[l])
        S.tt("dve", V(b2s.t[0:32, :], b2s.key), V(b2s.t[0:32, :], b2s.key), V(gt2p.t[0:32, :], gt2p.key), ALU.mult)
        b1r = sc.buf("b1r", [128, 4, 128]); b1T = sc.buf("b1T", [128, 512])
        ld(V(b1r.t[:, :, :], b1r.key), eb1_d[l].rearrange("a p f -> p a f"))
        ps = PS.bank()
        for a4 in range(4):
            S.tr(ps[:, a4 * 128:(a4 + 1) * 128], V(b1r.t[:, a4, :], b1r.key), ident)
        S.copy("dve", b1T[:, :], ps[:, :])
        b1v = b1T.t[:, :].rearrange("p (e f) -> p e f", f=16)[:, :, 8:16]
        S.op("dve", lambda e: e.tensor_scalar(b1v, b1v, 1.0, None, ALU.add), reads=[b1T.key], writes=[b1T.key])
        for t in tiles:
            for half in range(2):
                ps = PS.bank()
                S.mm(ps[:, :], V(gT.t[0:32, t * 128:(t + 1) * 128], ("gT", t)),
                     V(b2s.t[0:32, half * 512:(half + 1) * 512], b2s.key))
                S.tt("dve", A(t, half), ps[:, :], A(t, half), ALU.add)
        stg = [sc.buf("stg%d" % i, [128, 8, 512]) for i in range(2)]
        w1b = [sc.buf("w1b%d" % i, [128, 8, 512], BF16) for i in range(3)]
        w2b = [sc.buf("w2b%d" % i, [128, 8, 1024], BF16) for i in range(2)]
        actb = sc.buf("actb", [128, 8, NTOK], BF16)
        xg = [sc.buf("xg%d" % i, [128, 512]) for i in range(2)]
        sgm = [sc.buf("sgm%d" % i, [128, 512]) for i in range(2)]
        xl = [sc.buf("xl%d" % i, [128, 512]) for i in range(2)]
        groups = [(0, 128), (128, 640), (640, 1152)] if l == 0 else [(128, 640), (640, 1152)]
        si = 0; wi = 0; ci = 0
        for e_ in range(n_exp):
            w2_ = w2b[e_ % 2]
            for hh in range(2):
                st_ = stg[si % 2]; si += 1
                st4 = st_.t[:, :, :].rearrange("p a f -> p (a f)").rearrange("p (k d) -> p k d", k=4)
                ld(V(st4, st_.key), ew2_d[l, e_, hh * 512:(hh + 1) * 512, :].rearrange("(k p) d -> p k d", p=128))
                for k in range(4):
                    for a in range(2):
                        S.tt("pool", V(w2_.t[:, hh * 4 + k, a * 512:(a + 1) * 512], w2_.key),
                             V(st4[:, k, a * 512:(a + 1) * 512], st_.key), V(gt2p.t[:, a * 512:(a + 1) * 512], gt2p.key), ALU.mult)
            for j in range(4):
                st_ = stg[si % 2]; si += 1
                w1_ = w1b[wi % 3]; wi += 1
                ld(V(st_.t[:, :, 0:256], st_.key),
                   ew1_d[l, e_, :, 256 * j:256 * j + 256].rearrange("(k p) f -> p k f", p=128))
                ld(V(st_.t[:, :, 256:512], st_.key),
                   ew1_d[l, e_, :, 1024 + 256 * j:1024 + 256 * j + 256].rearrange("(k p) f -> p k f", p=128))
                S.copy("pool", V(w1_.t[:, :, :], w1_.key), V(st_.t[:, :, :], st_.key))
                for (g0, g1) in groups:
                    n = g1 - g0
                    for ii in range(2):
                        i = 2 * j + ii
                        psg = PS.bank(); psl = PS.bank()
                        for kc in range(8):
                            S.mm(psg[:, 0:n], V(w1_.t[:, kc, ii * 128:(ii + 1) * 128], w1_.key),
                                 V(hT.t[:, kc, g0:g1], ("hT", "all")), start=(kc == 0), stop=(kc == 7))
                        for kc in range(8):
                            S.mm(psl[:, 0:n], V(w1_.t[:, kc, 256 + ii * 128:256 + (ii + 1) * 128], w1_.key),
                                 V(hT.t[:, kc, g0:g1], ("hT", "all")), start=(kc == 0), stop=(kc == 7))
                        x_, s_, l_ = xg[ci % 2], sgm[ci % 2], xl[ci % 2]; ci += 1
                        bg = V(b1T.t[:, e_ * 16 + i:e_ * 16 + i + 1], b1T.key)
                        bl = V(b1T.t[:, e_ * 16 + 8 + i:e_ * 16 + 8 + i + 1], b1T.key)
                        S.ts("dve", V(x_.t[:, 0:n], x_.key), psg[:, 0:n], bg, LIMIT, ALU.add, ALU.min)
                        S.act(V(s_.t[:, 0:n], s_.key), V(x_.t[:, 0:n], x_.key), AF.Sigmoid, scale=SW_ALPHA)
                        S.ts("dve", V(l_.t[:, 0:n], l_.key), psl[:, 0:n], bl, 1.0 - LIMIT, ALU.add, ALU.max)
                        S.tt("pool", V(x_.t[:, 0:n], x_.key), V(x_.t[:, 0:n], x_.key), V(s_.t[:, 0:n], s_.key), ALU.mult)
                        S.stt("dve", V(actb.t[:, i, g0:g1], (actb.key, i)), V(l_.t[:, 0:n], l_.key), 1.0 + LIMIT,
                              V(x_.t[:, 0:n], x_.key), ALU.min, ALU.mult)
            for t in tiles:
                for half in range(2):
                    ps = PS.bank()
                    for fc in range(8):
                        S.mm(ps[:, :], V(actb.t[:, fc, t * 128:(t + 1) * 128], (actb.key, fc)),
                             V(w2_.t[:, fc, half * 512:(half + 1) * 512], w2_.key), start=(fc == 0), stop=(fc == 7))
                    S.stt("dve", A(t, half), ps[:, :], V(gates.t[:, t, e_:e_ + 1], ("gates", t)), A(t, half),
                          ALU.mult, ALU.add)
        sc.close(S)

        sc = Scope(nc)
        g2 = bc_tile(sc, "g2", lnp_d[l, 2]); b2 = bc_tile(sc, "b2", lnp_d[l, 3])
        if l == 0:
            G3 = bc_tile(sc, "G3", modbc_d[1, 1]); B3 = bc_tile(sc, "B3", modbc_d[1, 0])
            tmpb = sc.buf("tmpb", [128, 1024])
            S.tt("dve", tmpb[:, :], b2[:, :], G3[:, :], ALU.mult)
            S.tt("dve", B3[:, :], B3[:, :], tmpb[:, :], ALU.add)
            S.tt("dve", G3[:, :], G3[:, :], g2[:, :], ALU.mult)
            S.ts("dve", g2[:, :], g2[:, :], ALPHA, None, ALU.mult)
            S.ts("dve", b2[:, :], b2[:, :], ALPHA, None, ALU.mult)
            h2 = [sc.buf("h2%d" % i, [128, 1024]) for i in range(2)]
            for t in tiles:
                ln_tile(t)
                h_ = h2[t % 2]
                S.tt("dve", h_[:, :], A(t), G3[:, :], ALU.mult)
                S.tt("pool", h_[:, :], h_[:, :], B3[:, :], ALU.add)
                S.tt("dve", A(t), A(t), g2[:, :], ALU.mult)
                S.tt("pool", A(t), A(t), b2[:, :], ALU.add)
                transpose_tile(h_, t)
                if 1 not in layers and t >= 1:
                    S.store(out_d[p, (t - 1) * 128:t * 128, :], A(t))
        else:
            for t in tiles:
                ln_tile(t)
                S.tt("dve", A(t), A(t), g2[:, :], ALU.mult)
                S.tt("pool", A(t), A(t), b2[:, :], ALU.add)
                S.store(out_d[p, (t - 1) * 128:t * 128, :], A(t))
        sc.close(S)

    for p in range(npass):
        if 0 in layers:
            mix0(p)
            post(0, p)
        if 1 in layers:
            mix1(p)
            post(1, p)
    S.barrier()
    S.flush(final=True)
    return nc


def prep_phaseB(inp, og_b, b, s, shared=None):
    f = np.float32
    xh = np.zeros((2, NTOK, D), f)
    ogh = np.zeros((2, NTOK, 2048), f)
    flag = np.ones((128, 2), f)
    for p in range(2):
        t_lo = s * 2048 + p * 1024 - 128
        if t_lo < 0:
            flag[:, p] = 0.0
            xh[p, 128:] = inp["x"][b, 0:1024]
            ogh[p, 128:] = og_b[0:1024]
        else:
            xh[p] = inp["x"][b, t_lo:t_lo + NTOK]
            ogh[p] = og_b[t_lo:t_lo + NTOK]
    m = {"xh": xh, "ogh": ogh, "flag": flag,
         "cb": np.ascontiguousarray(inp["c"][b].reshape(8, 128).T)}
    if shared is None:
        shared = prep_phaseB_shared(inp)
    m.update(shared)
    return m


def prep_phaseB_shared(inp):
    f = np.float32
    bc = lambda v: np.ascontiguousarray(np.broadcast_to(v[None, :], (128, v.shape[0])))
    pp = lambda v, n: np.ascontiguousarray(v.reshape(n, 128).T)
    lnp = np.stack([np.stack([bc(inp[k][l]) for k in ("ln1_g", "ln1_b", "ln2_g", "ln2_b")]) for l in range(2)])
    return {
        "adaw": np.stack([pkc(inp["ada_w"][l]) for l in range(2)]),
        "adab": np.ascontiguousarray(inp["ada_b"][:, None, :]),
        "outw": pkc(inp["dn_out_w"][0]),
        "pw1w": pkc(inp["cf_pw1_w"][0]),
        "pw1b": pp(inp["cf_pw1_b"][0], 16),
        "dww": np.ascontiguousarray(inp["cf_dw_w"][0].reshape(31, 8, 128).transpose(2, 1, 0)),
        "dwb": pp(inp["cf_dw_b"][0], 8),
        "cfg": pp(inp["cf_ln_g"][0], 8),
        "cfb": pp(inp["cf_ln_b"][0], 8),
        "pw2w": pkc(inp["cf_pw2_w"][0]),
        "pw2b": np.ascontiguousarray(inp["cf_pw2_b"][0][None, :]),
        "lnp": np.ascontiguousarray(lnp.astype(f)),
        "rw": np.stack([pkc(inp["router_w"][l]) for l in range(2)]),
        "rb": np.ascontiguousarray(inp["router_b"][:, None, :]),
        "ew1": np.ascontiguousarray(inp["e_w1"]),
        "eb1": np.ascontiguousarray(inp["e_b1"].reshape(2, 4, 128, 128)),
        "ew2": np.ascontiguousarray(inp["e_w2"]),
        "eb2": np.ascontiguousarray(inp["e_b2"]),
        "cst": consts_np(),
    }


def build_fused():
    nc = bass.Bass("TRN2", target_bir_lowering=False)
    dt = nc.dram_tensor
    I = lambda name, shape: dt(name, shape, F32, kind="ExternalInput").ap()
    fz = declare_B(nc, with_og=False)
    fz["nc"] = nc
    fz["x"] = I("x", [SEQ, D])
    win4 = I("win", [4, 128, 8, 1544])
    cw4 = I("cw", [4, 128, 8, 4])
    alog4 = I("alog", [4, 128, 4])
    dtb4 = I("dtb", [4, 128, 4])
    fz["onw"] = I("onw", [128, 512])
    fz["sel"] = I("sel", [128, 4])
    fz["ogg"] = dt("ogg", [4 * OGROWS, 512], F32).ap()
    S = Sched(nc)
    fz["S"] = S
    fz["PS"] = PsumPool(nc, 4, "p")
    for hg in range(4):
        fh = dict(fz)
        fh["win"], fh["cw"], fh["alog"], fh["dtb"] = win4[hg], cw4[hg], alog4[hg], dtb4[hg]
        fh["ogpad"] = fz["ogg"][hg * OGROWS:(hg + 1) * OGROWS, :]
        build_phaseA(SEQ, fh)
    build_phaseB(fz=fz)
    return nc


def kernel(**inp):
    inp = {k: np.asarray(v) for k, v in inp.items()}
    nc = build_fused()
    shared = prep_phaseB_shared(inp)
    dummy_og = np.zeros((SEQ, 2048), np.float32)
    mA = {b: [prep_phaseA(inp, b, hg, SEQ) for hg in range(4)] for b in range(2)}
    stk = {b: {k: np.ascontiguousarray(np.stack([mA[b][hg][k] for hg in range(4)])) for k in ("win", "cw", "alog", "dtb")}
           for b in range(2)}
    maps = []
    for i in range(8):
        b, r = i // 4, i % 4
        mB = prep_phaseB(inp, dummy_og, b, r, shared)
        m = {k: v for k, v in mB.items() if k != "ogh"}
        m["x"] = mA[b][0]["x"]
        m["onw"] = mA[b][0]["onw"]
        m.update(stk[b])
        sel = np.zeros((128, 4), np.float32)
        sel[:, r] = 1.0
        m["sel"] = sel
        maps.append(m)
    res = run_bass_kernel_spmd(nc, maps, core_ids=list(range(8)))
    out = np.zeros((2, SEQ, D), np.float32)
    for i in range(8):
        b, s_ = i // 4, i % 4
        o = res.results[i]["out"]
        out[b, s_ * 2048:s_ * 2048 + 1024] = o[0]
        out[b, s_ * 2048 + 1024:s_ * 2048 + 2048] = o[1]
    return out
```

```python
import contextlib
import numpy as np
import concourse.bass as bass
import concourse.mybir as mybir
from concourse.bass_utils import run_bass_kernel_spmd

F32 = mybir.dt.float32
BF16 = mybir.dt.bfloat16
ALU = mybir.AluOpType
AF = mybir.ActivationFunctionType

ENGS = ("pe", "dve", "act", "pool", "sp")
N_DMA_SEMS = 12

D = 1024
SEQ = 8192
DEPTH = 2
NEXP = 32
ALPHA = (2 * DEPTH) ** 0.25
LN_EPS = 1e-5
RMS_EPS = 1e-6
L2_EPS = 1e-6
LIMIT = 7.0
SW_ALPHA = 1.702


class V:
    def __init__(self, ap, key):
        self.ap, self.key = ap, key


class Buf:
    def __init__(self, nc, name, shape, dtype, psum=False):
        if psum:
            self.t = nc.alloc_psum_tensor(name, shape, dtype)
        else:
            self.t = nc.alloc_sbuf_tensor("sb_" + name, shape, dtype)
        self.key = name

    def __getitem__(self, idx):
        return V(self.t[idx], self.key)

    def v(self, idx, sub):
        return V(self.t[idx], (self.key, sub))


class Sched:
    def __init__(self, nc):
        self.nc = nc
        self.ops = []
        self.flushed = 0
        self.cnt = {e: 0 for e in ENGS}
        self.last_w = {}
        self.readers = {}
        self.dma_tot = [0] * (N_DMA_SEMS * 3)
        self.dma_last = [None] * (N_DMA_SEMS * 3)
        self.dma_rr = {"sp": 0, "act": 0, "pool": 0}
        self.opinfo = []
        self.esem = {e: nc.alloc_semaphore("s_" + e) for e in ENGS}
        self.dsem = [nc.alloc_semaphore("d%d" % i) for i in range(N_DMA_SEMS * 3)]
        self.known = {e: {} for e in ENGS}
        self.last_on = {e: None for e in ENGS}
        import os
        self.maxops = int(os.environ.get("KSTOP", "0")) or None
        self.marks = {}
        self.rar_keys = set()

    def mark(self, label):
        self.marks.setdefault(label, len(self.ops))

    def _deps(self, reads, writes):
        d = set()
        for k in reads:
            w = self.last_w.get(k)
            if w is not None:
                d.add(w)
            if k in self.rar_keys:
                for r in self.readers.get(k, ()):
                    d.add(r)
        for k in writes:
            w = self.last_w.get(k)
            if w is not None:
                d.add(w)
            for r in self.readers.get(k, ()):
                d.add(r)
        return d

    def _commit(self, oid, reads, writes):
        for k in reads:
            self.readers.setdefault(k, []).append(oid)
        for k in writes:
            self.last_w[k] = oid
            self.readers[k] = []

    def op(self, eng, fn, reads=(), writes=()):
        if self.maxops is not None and len(self.ops) >= self.maxops:
            return -1
        reads = tuple(reads)
        writes = tuple(writes)
        deps = self._deps(reads, writes)
        oid = len(self.ops)
        self.cnt[eng] += 1
        self.opinfo.append(("c", eng, self.cnt[eng]))
        self.ops.append((eng, fn, deps, None))
        self._commit(oid, reads, writes)
        self.last_on[eng] = oid
        return oid

    def dma(self, fn, reads=(), writes=(), q="sp"):
        if self.maxops is not None and len(self.ops) >= self.maxops:
            return -1
        reads = tuple(reads)
        writes = tuple(writes)
        deps = self._deps(reads, writes)
        base = {"sp": 0, "act": N_DMA_SEMS, "pool": 2 * N_DMA_SEMS}[q]
        slot = base + self.dma_rr[q]
        self.dma_rr[q] = (self.dma_rr[q] + 1) % N_DMA_SEMS
        if self.dma_last[slot] is not None:
            deps.add(self.dma_last[slot])
        oid = len(self.ops)
        self.dma_tot[slot] += 16
        self.opinfo.append(("d", slot, self.dma_tot[slot]))
        self.dma_last[slot] = oid
        self.ops.append((q, fn, deps, slot))
        self._commit(oid, reads, writes)
        return oid

    def barrier(self):
        if self.maxops is not None and len(self.ops) >= self.maxops:
            return
        deps = set()
        for e in ENGS:
            if self.last_on[e] is not None:
                deps.add(self.last_on[e])
        for o in self.dma_last:
            if o is not None:
                deps.add(o)
        for e in ENGS:
            self.ops.append((e, None, set(deps), "bar"))
            self.opinfo.append(("n", None, 0))
        self.last_w = {}
        self.readers = {}

    def flush(self, final=False):
        nc = self.nc
        lo, hi = self.flushed, len(self.ops)
        self.flushed = hi
        per_eng = {e: [] for e in ENGS}
        for oid in range(lo, hi):
            per_eng[self.ops[oid][0]].append(oid)
        final_deps = []
        if final:
            for o in self.dma_last:
                if o is not None:
                    final_deps.append(o)

        def run(engname, engobj):
            known = self.known[engname]

            def wait_for(dlist, is_pe_compute):
                need = {}
                for d in dlist:
                    kind, a, v = self.opinfo[d]
                    if kind == "n":
                        continue
                    if kind == "c":
                        if a == "pe" and is_pe_compute:
                            continue
                        key = ("c", a)
                    else:
                        key = ("d", a)
                    if v > need.get(key, 0):
                        need[key] = v
                for key, v in need.items():
                    if known.get(key, 0) >= v:
                        continue
                    known[key] = v
                    sem = self.esem[key[1]] if key[0] == "c" else self.dsem[key[1]]
                    engobj.wait_ge(sem, v)

            for oid in per_eng[engname]:
                eng, fn, deps, slot = self.ops[oid]
                if slot == "bar":
                    wait_for(deps, False)
                    continue
                wait_for(deps, engname == "pe" and slot is None)
                ins = fn(engobj)
                if slot is None:
                    ins.then_inc(self.esem[engname], 1)
                else:
                    ins.then_inc(self.dsem[slot], 16)
            if final and engname == "sp":
                wait_for(final_deps, False)

        with nc.Block() as block:
            block.tensor(lambda e: run("pe", e))
            block.vector(lambda e: run("dve", e))
            block.scalar(lambda e: run("act", e))
            block.gpsimd(lambda e: run("pool", e))
            block.sync(lambda e: run("sp", e))

    @staticmethod
    def _k(*vs):
        return [v.key for v in vs if isinstance(v, V)]

    @staticmethod
    def _a(x):
        return x.ap if isinstance(x, V) else x

    def mm(self, out, lhsT, rhs, start=True, stop=True):
        return self.op("pe", lambda e: e.matmul(out.ap, lhsT.ap, rhs.ap, start=start, stop=stop),
                       reads=self._k(lhsT, rhs), writes=[out.key])

    def tr(self, out, in_, ident):
        return self.op("pe", lambda e: e.transpose(out.ap, in_.ap, ident.ap),
                       reads=self._k(in_, ident), writes=[out.key])

    def act(self, out, in_, func, bias=0.0, scale=1.0, accum=None, eng="act"):
        a = self._a
        kw = {}
        if accum is not None:
            kw["accum_out"] = accum.ap
        return self.op(eng, lambda e: e.activation(out.ap, in_.ap, func, bias=a(bias), scale=a(scale), **kw),
                       reads=self._k(in_, bias, scale), writes=self._k(out, accum))

    def ts(self, eng, out, in0, s1, s2, op0, op1=None):
        a = self._a
        if op1 is None:
            f = lambda e: e.tensor_scalar(out.ap, in0.ap, a(s1), None, op0)
        else:
            f = lambda e: e.tensor_scalar(out.ap, in0.ap, a(s1), a(s2), op0, op1)
        return self.op(eng, f, reads=self._k(in0, s1, s2), writes=[out.key])

    def tt(self, eng, out, in0, in1, op):
        return self.op(eng, lambda e: e.tensor_tensor(out.ap, in0.ap, in1.ap, op),
                       reads=self._k(in0, in1), writes=[out.key])

    def stt(self, eng, out, in0, s, in1, op0, op1):
        a = self._a
        eng = "dve"
        return self.op(eng, lambda e: e.scalar_tensor_tensor(out.ap, in0.ap, a(s), in1.ap, op0, op1),
                       reads=self._k(in0, s, in1), writes=[out.key])

    def copy(self, eng, out, in_):
        if eng == "act":
            return self.act(out, in_, AF.Copy)
        return self.op(eng, lambda e: e.tensor_copy(out.ap, in_.ap), reads=[in_.key], writes=[out.key])

    def memset(self, eng, out, val):
        return self.op(eng, lambda e: e.memset(out.ap, val), writes=[out.key])

    def load(self, out, src_ap, q="sp"):
        return self.dma(lambda e: e.dma_start(out=out.ap, in_=src_ap), writes=[out.key], q=q)

    def store(self, dst_ap, in_, q="sp", dkey=None):
        w = [dkey] if dkey is not None else []
        return self.dma(lambda e: e.dma_start(out=dst_ap, in_=in_.ap), reads=[in_.key], writes=w, q=q)


class PsumPool:
    def __init__(self, nc, n_big, prefix):
        self.banks = [Buf(nc, "%s_ps%d" % (prefix, b), [128, 512], F32, psum=True) for b in range(8)]
        self.configure(n_big)

    def register(self, S):
        S.rar_keys.update(b.key for b in self.banks)

    def configure(self, n_big):
        self.big = self.banks[:n_big]
        self.small = [(buf, q) for buf in self.banks[n_big:] for q in range(4)]
        self.bi = 0
        self.si = 0

    def bank(self):
        b = self.big[self.bi]
        self.bi = (self.bi + 1) % len(self.big)
        return b

    def quarter(self, w=128):
        nb = len(self.small) // 4
        i = self.si
        self.si = (self.si + 1) % len(self.small)
        buf, _ = self.small[(i % nb) * 4]
        q = (i // nb) % 4
        return V(buf.t[:, q * 128:q * 128 + w], buf.key)


def ada_broadcast(S, nc, PS, condB, ones_row, adaw_ap, adab_ap, col0, ncols, stage, outs, ident_unused=None):
    npieces = ncols // 512
    for pi in range(npieces):
        c0 = col0 + pi * 512
        st = stage
        S.load(V(st.t[:, :, :], st.key), adaw_ap[:, :, c0:c0 + 512])
        brow = outs["brow"]
        S.load(V(brow.t[0:1, 0:512], brow.key), adab_ap[0:1, c0:c0 + 512])
        ps = PS.bank()
        for kc in range(8):
            S.mm(ps[:, :], V(condB.t[:, kc, :], condB.key), V(st.t[:, kc, :], st.key), start=(kc == 0), stop=False)
        S.mm(ps[:, :], V(ones_row.t[0:1, :], ones_row.key), V(brow.t[0:1, 0:512], brow.key), start=False, stop=True)
        dst, add_one = outs["dst"][pi]
        if add_one:
            S.ts("dve", dst, ps[:, :], 1.0, None, ALU.add)
        else:
            S.copy("dve", dst, ps[:, :])


def build_phaseA(n_tok=SEQ, fz=None):
    if fz is None:
        nc = bass.Bass("TRN2", target_bir_lowering=False)
        dt = nc.dram_tensor
        x_d = dt("x", [n_tok, D], F32, kind="ExternalInput").ap()
        cb_d = dt("cb", [128, 8], F32, kind="ExternalInput").ap()
        adaw_d = dt("adaw", [128, 8, 2048], F32, kind="ExternalInput").ap()
        adab_d = dt("adab", [1, 2048], F32, kind="ExternalInput").ap()
        win_d = dt("win", [128, 8, 1544], F32, kind="ExternalInput").ap()
        cw_d = dt("cw", [128, 8, 4], F32, kind="ExternalInput").ap()
        alog_d = dt("alog", [128, 4], F32, kind="ExternalInput").ap()
        dtb_d = dt("dtb", [128, 4], F32, kind="ExternalInput").ap()
        onw_d = dt("onw", [128, 512], F32, kind="ExternalInput").ap()
        cst_d = dt("cst", [128, 4, 128], F32, kind="ExternalInput").ap()
        og_d = dt("og", [n_tok, 512], F32, kind="ExternalOutput").ap()
        S = Sched(nc)
        PS = PsumPool(nc, 4, "a")
        PS.register(S)
    else:
        nc, S, PS = fz["nc"], fz["S"], fz["PS"]
        PS.configure(4)
        x_d, cb_d, cst_d = fz["x"], fz["cb"], fz["cst"]
        adaw_d, adab_d = fz["adaw"][0], fz["adab"][0]
        win_d, cw_d, alog_d, dtb_d, onw_d = fz["win"], fz["cw"], fz["alog"], fz["dtb"], fz["onw"]
        og_d = fz["ogpad"][128:128 + n_tok, :]
    scA = Scope(nc)
    scA.plain = True
    B = lambda name, shape, dtp=F32: scA.buf(name, shape, dtp)

    cst = B("cst", [128, 4, 128])
    ident = V(cst.t[:, 0, :], "cst")
    ones = V(cst.t[:, 1, :], "cst")
    U = V(cst.t[:, 2, :], "cst")
    LS = V(cst.t[:, 3, :], "cst")
    S.load(V(cst.t[:, :, :], "cst"), cst_d)
    win = B("win", [128, 8, 1544], BF16)
    for kc in range(8):
        S.load(V(win.t[:, kc, :], "win"), win_d[:, kc, :], q="pool")
    cw = B("cw", [128, 8, 4])
    S.load(V(cw.t[:, :, :], "cw"), cw_d)
    alog = B("alog", [128, 4]); dtb = B("dtb", [128, 4]); onw = B("onw", [128, 512])
    S.load(alog[:, :], alog_d); S.load(dtb[:, :], dtb_d); S.load(onw[:, :], onw_d)
    negA = B("negA", [128, 4])
    S.act(negA[:, :], alog[:, :], AF.Exp)
    S.ts("dve", negA[:, :], negA[:, :], -1.0, None, ALU.mult)

    cbt = B("cbt", [128, 8])
    S.load(cbt[:, :], cb_d)
    cond = B("cond", [128, 8])
    S.act(cond[:, :], cbt[:, :], AF.Silu)
    condB = B("condB", [128, 8, 128])
    for kc in range(8):
        S.ts("dve", V(condB.t[:, kc, :], "condB"), ones, V(cond.t[:, kc:kc + 1], "cond"), None, ALU.mult)
    h4 = B("h4", [128, 4, 1024])
    sh_bc = B("sh_bc", [128, 1024]); scp_bc = B("scp_bc", [128, 1024])
    brow = B("brow", [1, 512])
    stg = B("adastage", [128, 8, 512])
    outs = {"brow": brow, "dst": [(V(sh_bc.t[:, 0:512], "sh_bc"), False), (V(sh_bc.t[:, 512:1024], "sh_bc"), False),
                                  (V(scp_bc.t[:, 0:512], "scp_bc"), True), (V(scp_bc.t[:, 512:1024], "scp_bc"), True)]}
    ada_broadcast(S, nc, PS, condB, Buf_view(cst, 1), adaw_d, adab_d, 0, 2048, stg, outs)

    xt = [B("xt%d" % i, [128, 1024]) for i in range(2)]
    hT = B("hT", [128, 8, 512], BF16)
    pre = B("pre", [128, 8, 515])
    S.memset("dve", V(pre.t[:, :, :], ("pre", "all")), 0.0)
    cv = [B("cv%d" % i, [128, 512]) for i in range(2)]
    qkvs = B("qkvs", [128, 8, 512])
    rn = B("rn", [128, 512])
    vtok = B("vtok", [128, 512]); ktok = B("ktok", [128, 256]); gz = B("gz", [128, 512])
    og = [B("og%d" % i, [128, 512]) for i in range(2)]
    sm = B("sm", [128, 64])
    Sst = [[B("S%d_%d" % (h, i), [128, 128]) for i in range(2)] for h in range(4)]
    for h in range(4):
        S.memset("pool", Sst[h][0][:, :], 0.0)
    W = {}
    for h in range(4):
        for nm in ("rhsU", "Gs", "e1", "DLs", "e2", "DUi", "P0", "N0", "P1", "N1", "R", "vb", "kbg", "negwT",
                   "vnew", "eg", "qgT", "kd", "QKmT", "junk"):
            W[(h, nm)] = B("w%d_%s" % (h, nm), [128, 128])
        W[(h, "s")] = B("w%d_s" % h, [128, 8])
    S.barrier()

    nblk = n_tok // 512
    og_i = 0
    S.mark("setup_done")
    for blk in range(nblk):
        t0 = blk * 512
        for c in range(4):
            xb = xt[c % 2]
            S.load(xb[:, :], x_d[t0 + c * 128:t0 + (c + 1) * 128, :])
            hv = h4.v((slice(None), c, slice(None)), c)
            S.tt("dve", hv, xb[:, :], scp_bc[:, :], ALU.mult)
            S.tt("pool", hv, hv, sh_bc[:, :], ALU.add)
        S.mark("modulated")
        for kc in range(8):
            ps = PS.bank()
            for c in range(4):
                hv = h4.v((slice(None), c, slice(kc * 128, (kc + 1) * 128)), c)
                S.tr(ps[:, c * 128:(c + 1) * 128], hv, ident)
            S.copy("act" if kc % 2 else "dve", hT.v((slice(None), kc, slice(None)), kc), ps[:, :])
        S.mark("transposed")
        for fc in range(8):
            ps = PS.bank()
            for kc in range(8):
                S.mm(ps[:, :], V(win.t[:, kc, fc * 128:(fc + 1) * 128], "win"),
                     hT.v((slice(None), kc, slice(None)), kc), start=(kc == 0), stop=(kc == 7))
            pk = ("pre", fc) if blk > 0 or True else None
            S.copy("act", V(pre.t[:, fc, 3:515], ("pre", fc)), ps[:, :])
            c_ = cv[fc % 2]
            e2_ = "dve"
            S.ts(e2_, c_[:, :], V(pre.t[:, fc, 0:512], ("pre", fc)), V(cw.t[:, fc, 0:1], "cw"), None, ALU.mult)
            for j in range(1, 4):
                S.stt(e2_, c_[:, :], V(pre.t[:, fc, j:j + 512], ("pre", fc)), V(cw.t[:, fc, j:j + 1], "cw"),
                      c_[:, :], ALU.mult, ALU.add)
            S.act(qkvs.v((slice(None), fc, slice(None)), fc), c_[:, :], AF.Silu)
            S.copy("pool", V(pre.t[:, fc, 0:3], ("pre", fc)), V(pre.t[:, fc, 512:515], ("pre", fc)))
        S.mark("conv_done")
        for fc in range(4):
            qv = qkvs.v((slice(None), fc, slice(None)), fc)
            c_ = cv[fc % 2]
            S.act(c_[:, :], qv, AF.Square)
            ps = PS.bank()
            S.mm(ps[:, :], ones, c_[:, :])
            S.ts("dve", rn[:, :], ps[:, :], L2_EPS, None, ALU.add)
            S.act(rn[:, :], rn[:, :], AF.Sqrt)
            S.op("dve", lambda e: e.reciprocal(rn.t[:, :], rn.t[:, :]), reads=["rn"], writes=["rn"])
            if fc < 2:
                S.stt("dve", qv, qv, 128.0 ** -0.5, rn[:, :], ALU.mult, ALU.mult)
            else:
                S.tt("dve", qv, qv, rn[:, :], ALU.mult)

        S.mark("l2norm_done")
        for c in range(4):
            cs = slice(c * 128, (c + 1) * 128)
            ps = PS.bank()
            for h in range(4):
                S.tr(ps[:, h * 128:(h + 1) * 128], qkvs.v((slice(None), 4 + h, cs), 4 + h), ident)
            S.copy("act", vtok[:, :], ps[:, :])
            ps = PS.bank()
            for qh in range(2):
                S.tr(ps[:, qh * 128:(qh + 1) * 128], qkvs.v((slice(None), 2 + qh, cs), 2 + qh), ident)
            S.copy("dve", ktok[:, :], ps[:, 0:256])
            ps = PS.bank()
            for kc in range(8):
                S.mm(ps[:, :], hT.v((slice(None), kc, cs), kc), V(win.t[:, kc, 1024:1536], "win"),
                     start=(kc == 0), stop=(kc == 7))
            S.act(gz[:, :], ps[:, :], AF.Silu)
            S.tt("pool", gz[:, :], gz[:, :], onw[:, :], ALU.mult)
            pq = PS.quarter(8)
            for kc in range(8):
                S.mm(pq, hT.v((slice(None), kc, cs), kc), V(win.t[:, kc, 1536:1544], "win"),
                     start=(kc == 0), stop=(kc == 7))
            smv = lambda a, b: V(sm.t[:, a:b], "sm")
            S.copy("dve", smv(32, 40), pq)
            S.act(smv(0, 4), smv(32, 36), AF.Sigmoid)
            S.ts("dve", smv(4, 8), smv(0, 4), -1.0, None, ALU.mult)
            S.tt("dve", smv(8, 12), smv(36, 40), dtb[:, :], ALU.add)
            S.act(smv(8, 12), smv(8, 12), AF.Exp)
            S.act(smv(8, 12), smv(8, 12), AF.Ln, bias=1.0)
            S.tt("dve", smv(12, 16), smv(8, 12), negA[:, :], ALU.mult)
            pq2 = PS.quarter(4)
            S.mm(pq2, U, smv(12, 16))
            S.copy("dve", smv(16, 20), pq2)
            S.act(smv(20, 24), smv(16, 20), AF.Exp)
            S.tt("dve", smv(24, 28), smv(0, 4), smv(20, 24), ALU.mult)

            S.mark("chunkprep_done")
            KK = []
            QKT = []
            for qh in range(2):
                kT = qkvs.v((slice(None), 2 + qh, cs), 2 + qh)
                qT = qkvs.v((slice(None), qh, cs), qh)
                p1 = PS.quarter()
                S.mm(p1, kT, kT)
                p2 = PS.quarter()
                S.mm(p2, kT, qT)
                KK.append(p1)
                QKT.append(p2)
            w = lambda h, nm: W[(h, nm)][:, :]
            H4 = range(4)
            gcol = lambda h: V(sm.t[:, 16 + h:17 + h], "sm")
            sv = lambda h, a_, b_: V(W[(h, "s")].t[:, a_:b_], W[(h, "s")].key)
            glast = lambda h: V(W[(h, "Gs")].t[:, 127:128], W[(h, "Gs")].key)
            ktk = lambda h: V(ktok.t[:, (h // 2) * 128:(h // 2 + 1) * 128], "ktok")
            for h in H4:
                S.act(w(h, "rhsU"), U, AF.Copy, scale=V(sm.t[:, 12 + h:13 + h], "sm"))
            pg = {}
            for h in H4:
                pg[h] = PS.quarter()
                S.mm(pg[h], ones, w(h, "rhsU"))
            for h in H4:
                S.copy("act", w(h, "Gs"), pg[h])
            for h in H4:
                S.ts("dve", w(h, "e1"), w(h, "Gs"), gcol(h), 0.0, ALU.subtract, ALU.max)
                S.ts("dve", w(h, "e2"), w(h, "Gs"), gcol(h), 0.0, ALU.subtract, ALU.min)
            for h in H4:
                S.act(w(h, "e1"), w(h, "e1"), AF.Exp, scale=-1.0)
                S.act(w(h, "e2"), w(h, "e2"), AF.Exp)
                S.act(w(h, "eg"), w(h, "Gs"), AF.Exp)
                S.act(sv(h, 0, 1), gcol(h), AF.Exp, bias=glast(h), scale=-1.0)
                S.act(sv(h, 1, 2), glast(h), AF.Exp)
            for h in H4:
                S.tt("pool", w(h, "DLs"), w(h, "e1"), LS, ALU.mult)
                S.tt("pool", w(h, "DUi"), w(h, "e2"), U, ALU.mult)
                S.tt("pool", w(h, "qgT"), qkvs.v((slice(None), h // 2, cs), h // 2), w(h, "eg"), ALU.mult)
            for h in H4:
                S.stt("dve", w(h, "P0"), KK[h // 2], V(sm.t[:, 4 + h:5 + h], "sm"), w(h, "DLs"), ALU.mult, ALU.mult)
            pn = {}
            for h in H4:
                pn[h] = PS.quarter()
                S.tr(pn[h], w(h, "P0"), ident)
            for h in H4:
                S.copy("act", w(h, "N0"), pn[h])
            for h in H4:
                S.act(w(h, "vb"), V(vtok.t[:, h * 128:(h + 1) * 128], "vtok"), AF.Copy, scale=V(sm.t[:, h:h + 1], "sm"))
                S.act(w(h, "kbg"), ktk(h), AF.Copy, scale=V(sm.t[:, 24 + h:25 + h], "sm"))
                S.act(w(h, "kd"), ktk(h), AF.Copy, scale=sv(h, 0, 1))
            for h in H4:
                S.tt("dve", w(h, "R"), w(h, "N0"), ident, ALU.add)
                S.tt("dve", w(h, "QKmT"), QKT[h // 2], w(h, "DUi"), ALU.mult)
            S.mark("stage1_done")
            cur = {h: ("P0", "N0") for h in H4}
            for m in range(1, 7):
                nxt = {h: (("P1", "N1") if cur[h][0] == "P0" else ("P0", "N0")) for h in H4}
                pp, pnn, pr = {}, {}, {}
                for h in H4:
                    pp[h] = PS.quarter()
                    S.mm(pp[h], w(h, cur[h][1]), w(h, cur[h][0]))
                if m < 6:
                    PS.quarter(); PS.quarter()
                    for h in H4:
                        pnn[h] = PS.quarter()
                        S.mm(pnn[h], w(h, cur[h][0]), w(h, cur[h][1]))
                    PS.quarter(); PS.quarter()
                for h in H4:
                    S.copy("act", w(h, nxt[h][0]), pp[h])
                if m < 6:
                    for h in H4:
                        S.copy("dve", w(h, nxt[h][1]), pnn[h])
                for h in H4:
                    pr[h] = PS.quarter()
                    S.mm(pr[h], w(h, nxt[h][0]), w(h, "R"))
                for h in H4:
                    S.tt("dve", w(h, "R"), pr[h], w(h, "R"), ALU.add)
                cur = nxt
            S.mark("stage2_done")
            ogb = og[og_i % 2]
            og_i += 1
            Sc = {h: Sst[h][(blk * 4 + c) % 2] for h in H4}
            Sn = {h: Sst[h][(blk * 4 + c + 1) % 2] for h in H4}
            pw, pv, po, pu = {}, {}, {}, {}
            for h in H4:
                pw[h] = PS.quarter()
                S.mm(pw[h], w(h, "kbg"), w(h, "R"))
            for h in H4:
                S.act(w(h, "negwT"), pw[h], AF.Copy, scale=-1.0)
            for h in H4:
                pv[h] = PS.quarter()
                S.mm(pv[h], w(h, "R"), w(h, "vb"), start=True, stop=False)
                S.mm(pv[h], w(h, "negwT"), Sc[h][:, :], start=False, stop=True)
            for h in H4:
                S.copy("dve", w(h, "vnew"), pv[h])
            for h in H4:
                po[h] = PS.quarter()
                S.mm(po[h], w(h, "qgT"), Sc[h][:, :], start=True, stop=False)
                S.mm(po[h], w(h, "QKmT"), w(h, "vnew"), start=False, stop=True)
            for h in H4:
                pu[h] = PS.quarter()
                S.mm(pu[h], w(h, "kd"), w(h, "vnew"))
            for h in H4:
                S.stt("dve", Sn[h][:, :], Sc[h][:, :], sv(h, 1, 2), pu[h], ALU.mult, ALU.add)
            for h in H4:
                S.copy("dve", w(h, "e1"), po[h])
            for h in H4:
                S.act(w(h, "junk"), w(h, "e1"), AF.Square, accum=sv(h, 2, 3))
            for h in H4:
                S.ts("dve", sv(h, 3, 4), sv(h, 2, 3), 1.0 / 128.0, RMS_EPS, ALU.mult, ALU.add)
            for h in H4:
                S.act(sv(h, 3, 4), sv(h, 3, 4), AF.Sqrt)
            for h in H4:
                S.op("dve", lambda e, h=h: e.reciprocal(W[(h, "s")].t[:, 3:4], W[(h, "s")].t[:, 3:4]),
                     reads=[W[(h, "s")].key], writes=[W[(h, "s")].key])
            for h in H4:
                S.stt("dve", V(ogb.t[:, h * 128:(h + 1) * 128], ogb.key), w(h, "e1"), sv(h, 3, 4),
                      V(gz.t[:, h * 128:(h + 1) * 128], "gz"), ALU.mult, ALU.mult)
            S.store(og_d[t0 + c * 128:t0 + (c + 1) * 128, :], ogb[:, :])
            S.mark("chunk0_done")
    if fz is None:
        S.flush(final=True)
        return nc
    zt = V(W[(0, "junk")].t[:, :], W[(0, "junk")].key)
    S.memset("dve", zt, 0.0)
    for q4 in range(4):
        S.store(fz["ogpad"][0:128, q4 * 128:(q4 + 1) * 128], zt)
    scA.close(S)
    return nc


def Buf_view(buf, idx):
    b = Buf.__new__(Buf)
    b.t = buf.t[:, idx, :]
    b.key = buf.key
    return b


def consts_np():
    i = np.arange(128)
    ident = np.eye(128, dtype=np.float32)
    ones = np.ones((128, 128), np.float32)
    U = (i[:, None] <= i[None, :]).astype(np.float32)
    LS = (i[:, None] > i[None, :]).astype(np.float32)
    return np.ascontiguousarray(np.stack([ident, ones, U, LS], axis=1))


def pkc(w):
    K, F = w.shape
    return np.ascontiguousarray(w.reshape(K // 128, 128, F).transpose(1, 0, 2))


def prep_phaseA(inp, b, hg, n_tok=SEQ):
    f = np.float32
    in_w = inp["dn_in_w"][0]
    cols = np.concatenate([np.arange(256 * hg, 256 * hg + 256), 1024 + np.arange(256 * hg, 256 * hg + 256),
                           2048 + np.arange(512 * hg, 512 * hg + 512), 4096 + np.arange(512 * hg, 512 * hg + 512),
                           6144 + np.arange(4 * hg, 4 * hg + 4), 6160 + np.arange(4 * hg, 4 * hg + 4)])
    ccols = cols[:1024]
    cw = inp["dn_conv_w"][0][:, ccols]
    cw = np.ascontiguousarray(cw.reshape(4, 8, 128).transpose(2, 1, 0))
    hs = slice(4 * hg, 4 * hg + 4)
    return {
        "x": np.ascontiguousarray(inp["x"][b, :n_tok]).astype(f),
        "cb": np.ascontiguousarray(inp["c"][b].reshape(8, 128).T),
        "adaw": pkc(inp["ada_w"][0][:, :2048]),
        "adab": np.ascontiguousarray(inp["ada_b"][0][None, :2048]),
        "win": pkc(in_w[:, cols]),
        "cw": cw,
        "alog": np.ascontiguousarray(np.broadcast_to(inp["dn_A_log"][0][hs][None, :], (128, 4))),
        "dtb": np.ascontiguousarray(np.broadcast_to(inp["dn_dt_bias"][0][hs][None, :], (128, 4))),
        "onw": np.ascontiguousarray(np.broadcast_to(np.tile(inp["dn_onorm_w"][0], 4)[None, :], (128, 512))),
        "cst": consts_np(),
    }


class Scope:
    _n = 0

    def __init__(self, nc):
        self.nc = nc
        self.st = contextlib.ExitStack()
        Scope._n += 1
        self.sfx = "_s%d" % Scope._n
        self.plain = False

    def buf(self, name, shape, dtp=F32):
        b = Buf.__new__(Buf)
        b.t = self.st.enter_context(self.nc.sbuf_tensor("sb_" + name + self.sfx, shape, dtp))
        b.key = name if self.plain else name + self.sfx
        return b

    def close(self, S):
        S.barrier()
        S.flush()
        self.st.close()


NTILE = 9
NTOK = NTILE * 128
OGROWS = SEQ + 128


def declare_B(nc, with_og=True):
    dt = nc.dram_tensor
    I = lambda name, shape: dt(name, shape, F32, kind="ExternalInput").ap()
    d = {}
    d["xh"] = I("xh", [2, NTOK, D])
    if with_og:
        d["ogh"] = I("ogh", [2, NTOK, 2048])
    d["flag"] = I("flag", [128, 2])
    d["cb"] = I("cb", [128, 8])
    d["adaw"] = I("adaw", [2, 128, 8, 6144])
    d["adab"] = I("adab", [2, 1, 6144])
    d["outw"] = I("outw", [128, 16, 1024])
    d["pw1w"] = I("pw1w", [128, 8, 2048])
    d["pw1b"] = I("pw1b", [128, 16])
    d["dww"] = I("dww", [128, 8, 31])
    d["dwb"] = I("dwb", [128, 8])
    d["cfg"] = I("cfg", [128, 8])
    d["cfb"] = I("cfb", [128, 8])
    d["pw2w"] = I("pw2w", [128, 8, 1024])
    d["pw2b"] = I("pw2b", [1, 1024])
    d["lnp"] = I("lnp", [2, 4, 128, 1024])
    d["rw"] = I("rw", [2, 128, 8, 32])
    d["rb"] = I("rb", [2, 1, 32])
    d["ew1"] = I("ew1", [2, NEXP, 1024, 2048])
    d["eb1"] = I("eb1", [2, 4, 128, 128])
    d["ew2"] = I("ew2", [2, NEXP, 1024, 1024])
    d["eb2"] = I("eb2", [2, NEXP, 1024])
    d["cst"] = I("cst", [128, 4, 128])
    d["out"] = dt("out", [2, 1024, D], F32, kind="ExternalOutput").ap()
    d["modbc"] = dt("modbc", [2, 6, 128, 1024], F32).ap()
    return d


def build_phaseB(n_exp=NEXP, layers=(0, 1), npass=2, fz=None):
    if fz is None:
        nc = bass.Bass("TRN2", target_bir_lowering=False)
        d = declare_B(nc)
    else:
        nc = fz["nc"]
        d = fz
    xh_d, flag_d, cb_d, adaw_d, adab_d = d["xh"], d["flag"], d["cb"], d["adaw"], d["adab"]
    og_d = d.get("ogh")
    outw_d, pw1w_d, pw1b_d, dww_d, dwb_d, cfg_d, cfb_d = (d[k] for k in ("outw", "pw1w", "pw1b", "dww", "dwb", "cfg", "cfb"))
    pw2w_d, pw2b_d, lnp_d, rw_d, rb_d = (d[k] for k in ("pw2w", "pw2b", "lnp", "rw", "rb"))
    ew1_d, eb1_d, ew2_d, eb2_d, cst_d, out_d, modbc_d = (d[k] for k in ("ew1", "eb1", "ew2", "eb2", "cst", "out", "modbc"))

    if fz is None:
        S = Sched(nc)
        PS = PsumPool(nc, 6, "b")
        PS.register(S)
    else:
        S, PS = fz["S"], fz["PS"]
        PS.configure(6)
    B = lambda name, shape, dtp=F32: Buf(nc, "b_" + name, shape, dtp)
    cst = B("cst", [128, 4, 128])
    cst.key = "cst"
    ident = V(cst.t[:, 0, :], "cst")
    ones = V(cst.t[:, 1, :], "cst")
    ones_row = V(cst.t[0:1, 1, :], "cst")
    S.load(V(cst.t[:, :, :], "cst"), cst_d)
    flag = B("flag", [128, 2]); flag.key = "flag"
    S.load(flag[:, :], flag_d)
    acc = B("acc", [128, NTILE, 1024]); acc.key = "acc"
    hT = B("hT", [128, 8, NTOK], BF16); hT.key = "hT"
    gates = B("gates", [128, NTILE, 32]); gates.key = "gates"
    gT = B("gT", [32, NTOK]); gT.key = "gT"
    lns = B("lns", [128, NTILE, 16]); lns.key = "lns"
    if fz is not None:
        sel = B("sel", [128, 4]); sel.key = "sel"
        S.load(sel[:, :], fz["sel"])

    A = lambda t, half=None: (acc.v((slice(None), t, slice(None)), t) if half is None else
                              acc.v((slice(None), t, slice(half * 512, half * 512 + 512)), t))

    def ld(out, src, rkey=None, q="sp"):
        r = [rkey] if rkey is not None else []
        return S.dma(lambda e: e.dma_start(out=out.ap, in_=src), reads=r, writes=[out.key], q=q)

    sc = Scope(nc)
    cbt = sc.buf("cbt", [128, 8]); cond = sc.buf("cond", [128, 8]); condB = sc.buf("condB", [128, 8, 128])
    ld(cbt[:, :], cb_d)
    S.act(cond[:, :], cbt[:, :], AF.Silu)
    for kc in range(8):
        S.ts("dve", V(condB.t[:, kc, :], condB.key), ones, V(cond.t[:, kc:kc + 1], cond.key), None, ALU.mult)
    stg = [sc.buf("stg%d" % i, [128, 8, 512]) for i in range(2)]
    brow = [sc.buf("brow%d" % i, [1, 512]) for i in range(2)]
    mtmp = [sc.buf("mtmp%d" % i, [128, 512]) for i in range(2)]
    pi = 0
    for l in range(2):
        for ch in range(6):
            for half in range(2):
                c0 = ch * 1024 + half * 512
                st_, br_, mt_ = stg[pi % 2], brow[pi % 2], mtmp[pi % 2]
                pi += 1
                ld(V(st_.t[:, :, :], st_.key), adaw_d[l, :, :, c0:c0 + 512])
                ld(V(br_.t[0:1, :], br_.key), adab_d[l, 0:1, c0:c0 + 512])
                ps = PS.bank()
                for kc in range(8):
                    S.mm(ps[:, :], V(condB.t[:, kc, :], condB.key), V(st_.t[:, kc, :], st_.key), start=(kc == 0), stop=False)
                S.mm(ps[:, :], ones_row, V(br_.t[0:1, :], br_.key), start=False, stop=True)
                if ch in (1, 2, 4, 5):
                    S.ts("dve", mt_[:, :], ps[:, :], 1.0, None, ALU.add)
                else:
                    S.copy("dve", mt_[:, :], ps[:, :])
                S.store(modbc_d[l, ch, :, half * 512:half * 512 + 512], mt_[:, :], dkey=("modbc", l, ch, half))
    sc.close(S)

    def ln_tile(t):
        sv = lambda a, b: V(lns.t[:, t, a:b], ("lns", t))
        S.op("dve", lambda e: e.bn_stats(lns.t[:, t, 0:6], acc.t[:, t, 0:512]), reads=[("acc", t)], writes=[("lns", t)])
        S.op("dve", lambda e: e.bn_stats(lns.t[:, t, 6:12], acc.t[:, t, 512:1024]), reads=[("acc", t)], writes=[("lns", t)])
        S.op("dve", lambda e: e.bn_aggr(lns.t[:, t, 12:14], lns.t[:, t, 0:12]), reads=[("lns", t)], writes=[("lns", t)])
        S.ts("dve", sv(14, 15), sv(13, 14), LN_EPS, None, ALU.add)
        S.act(sv(14, 15), sv(14, 15), AF.Sqrt)
        S.op("dve", lambda e: e.reciprocal(lns.t[:, t, 14:15], lns.t[:, t, 14:15]), reads=[("lns", t)], writes=[("lns", t)])
        S.ts("dve", A(t), A(t), sv(12, 13), sv(14, 15), ALU.subtract, ALU.mult)

    def transpose_tile(src, t, hTf=None):
        for b2 in range(2):
            ps = PS.bank()
            for k4 in range(4):
                kc = b2 * 4 + k4
                S.tr(ps[:, k4 * 128:(k4 + 1) * 128], V(src.t[:, kc * 128:(kc + 1) * 128], src.key), ident)
            psv = V(ps.t[:, :].rearrange("p (k t) -> p k t", k=4), ps.key)
            if hTf is not None:
                S.copy("act", V(hTf.t[:, b2 * 4:b2 * 4 + 4, :], hTf.key), psv)
                S.copy("pool", V(hT.t[:, b2 * 4:b2 * 4 + 4, t * 128:(t + 1) * 128], ("hT", t)),
                       V(hTf.t[:, b2 * 4:b2 * 4 + 4, :], hTf.key))
            else:
                S.copy("act" if b2 else "dve", V(hT.t[:, b2 * 4:b2 * 4 + 4, t * 128:(t + 1) * 128], ("hT", t)), psv)

    def bc_tile(sc_, name, src, rkey=None):
        b = sc_.buf(name, [128, 1024])
        ld(b[:, :], src, rkey)
        return b

    def mix0(p):
        sc = Scope(nc)
        outw = sc.buf("outw", [128, 16, 1024], BF16)
        for fc in range(16):
            ld(V(outw.t[:, fc, :], outw.key), outw_d[:, fc, :], q="pool")
        gt1p = bc_tile(sc, "gt1p", modbc_d[0, 2])
        ogt = [sc.buf("ogt%d" % i, [128, 2048]) for i in range(2)]
        cand = [sc.buf("cand%d" % i, [128, 2048]) for i in range(2)] if fz is not None else None
        ogT = [sc.buf("ogT%d" % i, [128, 2048], BF16) for i in range(2)]
        xt = [sc.buf("xt%d" % i, [128, 1024]) for i in range(2)]
        t1 = [sc.buf("t1%d" % i, [128, 512]) for i in range(2)]
        for t in range(NTILE):
            o_, oT, x_ = ogt[t % 2], ogT[t % 2], xt[t % 2]
            if fz is None:
                ld(o_[:, :], og_d[p, t * 128:(t + 1) * 128, :])
            else:
                for sp in range(4):
                    cd = cand[sp % 2]
                    r0 = sp * 2048 + p * 1024 + t * 128
                    for hg in range(4):
                        S.dma(lambda e, cd=cd, hg=hg, r0=r0: e.dma_start(
                            out=cd.t[:, hg * 512:(hg + 1) * 512], in_=fz["ogg"][hg * OGROWS + r0:hg * OGROWS + r0 + 128, :]),
                            reads=["ogg"], writes=[cd.key])
                    if sp == 0:
                        S.ts("dve", o_[:, :], cd[:, :], V(sel.t[:, 0:1], "sel"), None, ALU.mult)
                    else:
                        S.stt("dve", o_[:, :], cd[:, :], V(sel.t[:, sp:sp + 1], "sel"), o_[:, :], ALU.mult, ALU.add)
            ld(x_[:, :], xh_d[p, t * 128:(t + 1) * 128, :])
            for b4 in range(4):
                ps = PS.bank()
                for k4 in range(4):
                    fc = b4 * 4 + k4
                    S.tr(ps[:, k4 * 128:(k4 + 1) * 128], V(o_.t[:, fc * 128:(fc + 1) * 128], o_.key), ident)
                S.copy("act" if b4 % 2 else "dve", V(oT.t[:, b4 * 512:(b4 + 1) * 512], oT.key), ps[:, :])
            for half in range(2):
                ps = PS.bank()
                for fc in range(16):
                    S.mm(ps[:, :], V(oT.t[:, fc * 128:(fc + 1) * 128], oT.key),
                         V(outw.t[:, fc, half * 512:(half + 1) * 512], outw.key), start=(fc == 0), stop=(fc == 15))
                tt_ = t1[half]
                S.tt("dve", tt_[:, :], ps[:, :], V(gt1p.t[:, half * 512:(half + 1) * 512], gt1p.key), ALU.mult)
                S.stt("dve", A(t, half), V(x_.t[:, half * 512:(half + 1) * 512], x_.key), ALPHA, tt_[:, :], ALU.mult, ALU.add)
        sc.close(S)

    def mix1(p):
        sc = Scope(nc)
        pw1w = sc.buf("pw1w", [128, 8, 2048], BF16)
        pw2w = sc.buf("pw2w", [128, 8, 1024], BF16)
        for kc in range(8):
            ld(V(pw1w.t[:, kc, :], pw1w.key), pw1w_d[:, kc, :], q="pool")
            ld(V(pw2w.t[:, kc, :], pw2w.key), pw2w_d[:, kc, :], q="pool")
        pw1b = sc.buf("pw1b", [128, 16]); dww = sc.buf("dww", [128, 8, 31]); dwb = sc.buf("dwb", [128, 8])
        cfg = sc.buf("cfg", [128, 8]); cfb = sc.buf("cfb", [128, 8]); pw2b = sc.buf("pw2b", [1, 1024])
        ld(pw1b[:, :], pw1b_d); ld(V(dww.t[:, :, :], dww.key), dww_d); ld(dwb[:, :], dwb_d)
        ld(cfg[:, :], cfg_d); ld(cfb[:, :], cfb_d); ld(V(pw2b.t[0:1, :], pw2b.key), pw2b_d)
        gt1p = bc_tile(sc, "gt1p", modbc_d[1, 2])
        ut = [sc.buf("ut%d" % i, [128, NTOK]) for i in range(2)]
        sg = [sc.buf("sg%d" % i, [128, 512]) for i in range(2)]
        cvT = sc.buf("cvT", [128, 8, 1024])
        sT = sc.buf("sT", [128, 8, 1024], BF16)
        mean = sc.buf("mean", [128, 1024]); rstd = sc.buf("rstd", [128, 1024]); m2 = sc.buf("m2", [128, 512])
        groups = [(0, 128), (128, 640), (640, 1152)]
        gi = 0
        for fc in range(8):
            u_ = ut[fc % 2]
            for (g0, g1) in groups:
                n = g1 - g0
                psa = PS.bank(); psg = PS.bank()
                for kc in range(8):
                    S.mm(psa[:, 0:n], V(pw1w.t[:, kc, fc * 128:(fc + 1) * 128], pw1w.key),
                         V(hT.t[:, kc, g0:g1], ("hT", "all")), start=(kc == 0), stop=(kc == 7))
                for kc in range(8):
                    S.mm(psg[:, 0:n], V(pw1w.t[:, kc, 1024 + fc * 128:1024 + (fc + 1) * 128], pw1w.key),
                         V(hT.t[:, kc, g0:g1], ("hT", "all")), start=(kc == 0), stop=(kc == 7))
                s_ = sg[gi % 2]; gi += 1
                S.act(V(s_.t[:, 0:n], s_.key), psg[:, 0:n], AF.Sigmoid, bias=V(pw1b.t[:, 8 + fc:9 + fc], pw1b.key))
                S.stt("dve", V(u_.t[:, g0:g1], u_.key), psa[:, 0:n], V(pw1b.t[:, fc:fc + 1], pw1b.key),
                      V(s_.t[:, 0:n], s_.key), ALU.add, ALU.mult)
            S.ts("dve", V(u_.t[:, 0:128], u_.key), V(u_.t[:, 0:128], u_.key), V(flag.t[:, p:p + 1], "flag"), None, ALU.mult)
            cv = V(cvT.t[:, fc, :], (cvT.key, fc))
            S.ts("dve", cv, V(u_.t[:, 98:98 + 1024], u_.key), V(dww.t[:, fc, 0:1], dww.key),
                 V(dwb.t[:, fc:fc + 1], dwb.key), ALU.mult, ALU.add)
            for j in range(1, 31):
                S.stt("dve", cv, V(u_.t[:, 98 + j:98 + j + 1024], u_.key), V(dww.t[:, fc, j:j + 1], dww.key), cv,
                      ALU.mult, ALU.add)
        for tg in range(2):
            ts_ = slice(tg * 512, (tg + 1) * 512)
            pss = PS.bank(); psq = PS.bank()
            for fc in range(8):
                cvs = V(cvT.t[:, fc, ts_], (cvT.key, fc))
                S.mm(pss[:, :], ones, cvs, start=(fc == 0), stop=(fc == 7))
                s_ = sg[fc % 2]
                S.act(s_[:, :], cvs, AF.Square)
                S.mm(psq[:, :], ones, s_[:, :], start=(fc == 0), stop=(fc == 7))
            mv = V(mean.t[:, ts_], (mean.key, tg)); rv = V(rstd.t[:, ts_], (rstd.key, tg))
            S.ts("dve", mv, pss[:, :], 1.0 / 1024.0, None, ALU.mult)
            S.tt("pool", m2[:, :], mv, mv, ALU.mult)
            S.stt("dve", rv, psq[:, :], 1.0 / 1024.0, m2[:, :], ALU.mult, ALU.subtract)
            S.ts("dve", rv, rv, LN_EPS, None, ALU.add)
            S.act(rv, rv, AF.Sqrt)
            S.op("dve", lambda e, ts_=ts_: e.reciprocal(rstd.t[:, ts_], rstd.t[:, ts_]), reads=[rv.key], writes=[rv.key])
            for fc in range(8):
                cvs = V(cvT.t[:, fc, ts_], (cvT.key, fc))
                S.tt("dve", cvs, cvs, mv, ALU.subtract)
                S.tt("pool", cvs, cvs, rv, ALU.mult)
                S.act(V(sT.t[:, fc, ts_], (sT.key, fc)), cvs, AF.Silu, bias=V(cfb.t[:, fc:fc + 1], cfb.key),
                      scale=V(cfg.t[:, fc:fc + 1], cfg.key))
        t1 = [sc.buf("t1%d" % i, [128, 512]) for i in range(2)]
        for t in range(1, NTILE):
            m0 = (t - 1) * 128
            for half in range(2):
                ps = PS.bank()
                for fc in range(8):
                    S.mm(ps[:, :], V(sT.t[:, fc, m0:m0 + 128], (sT.key, fc)),
                         V(pw2w.t[:, fc, half * 512:(half + 1) * 512], pw2w.key), start=(fc == 0), stop=False)
                S.mm(ps[:, :], ones_row, V(pw2b.t[0:1, half * 512:(half + 1) * 512], pw2b.key), start=False, stop=True)
                tt_ = t1[half]
                S.tt("dve", tt_[:, :], ps[:, :], V(gt1p.t[:, half * 512:(half + 1) * 512], gt1p.key), ALU.mult)
                S.tt("pool", A(t, half), A(t, half), tt_[:, :], ALU.add)
        sc.close(S)

    def post(l, p):
        tiles = list(range(NTILE)) if l == 0 else list(range(1, NTILE))
        sc = Scope(nc)
        lg = bc_tile(sc, "lg", lnp_d[l, 0]); lb = bc_tile(sc, "lb", lnp_d[l, 1])
        G2 = bc_tile(sc, "G2", modbc_d[l, 4]); B2 = bc_tile(sc, "B2", modbc_d[l, 3])
        S.tt("dve", B2[:, :], B2[:, :], B2[:, :], ALU.bypass) if False else None
        tmpb = sc.buf("tmpb", [128, 1024])
        S.tt("dve", tmpb[:, :], lb[:, :], G2[:, :], ALU.mult)
        S.tt("dve", B2[:, :], B2[:, :], tmpb[:, :], ALU.add)
        S.tt("dve", G2[:, :], G2[:, :], lg[:, :], ALU.mult)
        S.ts("dve", lg[:, :], lg[:, :], ALPHA, None, ALU.mult)
        S.ts("dve", lb[:, :], lb[:, :], ALPHA, None, ALU.mult)
        rw = sc.buf("rw", [128, 8, 32]); rb = sc.buf("rb", [1, 32])
        ld(V(rw.t[:, :, :], rw.key), rw_d[l]); ld(V(rb.t[0:1, :], rb.key), rb_d[l])
        h2 = [sc.buf("h2%d" % i, [128, 1024]) for i in range(2)]
        hTf = [sc.buf("hTf%d" % i, [128, 8, 128]) for i in range(2)]
        rt = sc.buf("rt", [128, NTILE, 128])
        for t in tiles:
            ln_tile(t)
            h_ = h2[t % 2]; hf = hTf[t % 2]
            S.tt("dve", h_[:, :], A(t), G2[:, :], ALU.mult)
            S.tt("pool", h_[:, :], h_[:, :], B2[:, :], ALU.add)
            S.tt("dve", A(t), A(t), lg[:, :], ALU.mult)
            S.tt("pool", A(t), A(t), lb[:, :], ALU.add)
            transpose_tile(h_, t, hf)
            pq = PS.quarter(32)
            for kc in range(8):
                S.mm(pq, V(hf.t[:, kc, :], hf.key), V(rw.t[:, kc, :], rw.key), start=(kc == 0), stop=False)
            S.mm(pq, ones_row, V(rb.t[0:1, :], rb.key), start=False, stop=True)
            r = lambda a, b, t=t: V(rt.t[:, t, a:b], (rt.key, t))
            S.copy("dve", r(0, 32), pq)
            S.op("dve", lambda e, t=t: e.max(rt.t[:, t, 32:40], rt.t[:, t, 0:32]), reads=[(rt.key, t)], writes=[(rt.key, t)])
            S.ts("dve", r(40, 72), r(0, 32), r(35, 36), None, ALU.is_ge)
            S.ts("dve", r(72, 73), r(32, 33), -1.0, None, ALU.mult)
            S.act(r(80, 112), r(0, 32), AF.Exp, bias=r(72, 73))
            S.tt("dve", r(80, 112), r(80, 112), r(40, 72), ALU.mult)
            S.op("dve", lambda e, t=t: e.reduce_sum(rt.t[:, t, 73:74], rt.t[:, t, 80:112], mybir.AxisListType.X),
                 reads=[(rt.key, t)], writes=[(rt.key, t)])
            S.op("dve", lambda e, t=t: e.reciprocal(rt.t[:, t, 74:75], rt.t[:, t, 73:74]), reads=[(rt.key, t)], writes=[(rt.key, t)])
            gv = V(gates.t[:, t, :], ("gates", t))
            S.ts("dve", gv, r(80, 112), r(74, 75), None, ALU.mult)
            pq2 = PS.quarter(128)
            S.tr(V(pq2.ap[0:32, :], pq2.key), gv, ident)
            S.copy("act", V(gT.t[0:32, t * 128:(t + 1) * 128], ("gT", t)), V(pq2.ap[0:32, :], pq2.key))
        sc.close(S)

        sc = Scope(nc)
        gt2p = bc_tile(sc, "gt2p", modbc_d[l, 5])
        b2s = sc.buf("b2s", [32, 1024])
        ld(V(b2s.t[0:32, :], b2s.key), eb2_d[l])
        S.tt("dve", V(b2s.t[0:32, :], b2s.key), V(b2s.t[0:32, :], b2s.key), V(gt2p.t[0:32, :], gt2p.key), ALU.mult)
        b1r = sc.buf("b1r", [128, 4, 128]); b1T = sc.buf("b1T", [128, 512])
        ld(V(b1r.t[:, :, :], b1r.key), eb1_d[l].rearrange("a p f -> p a f"))
        ps = PS.bank()
        for a4 in range(4):
            S.tr(ps[:, a4 * 128:(a4 + 1) * 128], V(b1r.t[:, a4, :], b1r.key), ident)
        S.copy("dve", b1T[:, :], ps[:, :])
        b1v = b1T.t[:, :].rearrange("p (e f) -> p e f", f=16)[:, :, 8:16]
        S.op("dve", lambda e: e.tensor_scalar(b1v, b1v, 1.0, None, ALU.add), reads=[b1T.key], writes=[b1T.key])
        for t in tiles:
            for half in range(2):
                ps = PS.bank()
                S.mm(ps[:, :], V(gT.t[0:32, t * 128:(t + 1) * 128], ("gT", t)),
                     V(b2s.t[0:32, half * 512:(half + 1) * 512], b2s.key))
                S.tt("dve", A(t, half), ps[:, :], A(t, half), ALU.add)
        stg = [sc.buf("stg%d" % i, [128, 8, 512]) for i in range(2)]
        w1b = [sc.buf("w1b%d" % i, [128, 8, 512], BF16) for i in range(3)]
        w2b = [sc.buf("w2b%d" % i, [128, 8, 1024], BF16) for i in range(2)]
        actb = sc.buf("actb", [128, 8, NTOK], BF16)
        xg = [sc.buf("xg%d" % i, [128, 512]) for i in range(2)]
        sgm = [sc.buf("sgm%d" % i, [128, 512]) for i in range(2)]
        xl = [sc.buf("xl%d" % i, [128, 512]) for i in range(2)]
        groups = [(0, 128), (128, 640), (640, 1152)] if l == 0 else [(128, 640), (640, 1152)]
        si = 0; wi = 0; ci = 0
        for e_ in range(n_exp):
            w2_ = w2b[e_ % 2]
            for hh in range(2):
                st_ = stg[si % 2]; si += 1
                st4 = st_.t[:, :, :].rearrange("p a f -> p (a f)").rearrange("p (k d) -> p k d", k=4)
                ld(V(st4, st_.key), ew2_d[l, e_, hh * 512:(hh + 1) * 512, :].rearrange("(k p) d -> p k d", p=128))
                for k in range(4):
                    for a in range(2):
                        S.tt("pool", V(w2_.t[:, hh * 4 + k, a * 512:(a + 1) * 512], w2_.key),
                             V(st4[:, k, a * 512:(a + 1) * 512], st_.key), V(gt2p.t[:, a * 512:(a + 1) * 512], gt2p.key), ALU.mult)
            for j in range(4):
                st_ = stg[si % 2]; si += 1
                w1_ = w1b[wi % 3]; wi += 1
                ld(V(st_.t[:, :, 0:256], st_.key),
                   ew1_d[l, e_, :, 256 * j:256 * j + 256].rearrange("(k p) f -> p k f", p=128))
                ld(V(st_.t[:, :, 256:512], st_.key),
                   ew1_d[l, e_, :, 1024 + 256 * j:1024 + 256 * j + 256].rearrange("(k p) f -> p k f", p=128))
                S.copy("pool", V(w1_.t[:, :, :], w1_.key), V(st_.t[:, :, :], st_.key))
                for (g0, g1) in groups:
                    n = g1 - g0
                    for ii in range(2):
                        i = 2 * j + ii
                        psg = PS.bank(); psl = PS.bank()
                        for kc in range(8):
                            S.mm(psg[:, 0:n], V(w1_.t[:, kc, ii * 128:(ii + 1) * 128], w1_.key),
                                 V(hT.t[:, kc, g0:g1], ("hT", "all")), start=(kc == 0), stop=(kc == 7))
                        for kc in range(8):
                            S.mm(psl[:, 0:n], V(w1_.t[:, kc, 256 + ii * 128:256 + (ii + 1) * 128], w1_.key),
                                 V(hT.t[:, kc, g0:g1], ("hT", "all")), start=(kc == 0), stop=(kc == 7))
                        x_, s_, l_ = xg[ci % 2], sgm[ci % 2], xl[ci % 2]; ci += 1
                        bg = V(b1T.t[:, e_ * 16 + i:e_ * 16 + i + 1], b1T.key)
                        bl = V(b1T.t[:, e_ * 16 + 8 + i:e_ * 16 + 8 + i + 1], b1T.key)
                        S.ts("dve", V(x_.t[:, 0:n], x_.key), psg[:, 0:n], bg, LIMIT, ALU.add, ALU.min)
                        S.act(V(s_.t[:, 0:n], s_.key), V(x_.t[:, 0:n], x_.key), AF.Sigmoid, scale=SW_ALPHA)
                        S.ts("dve", V(l_.t[:, 0:n], l_.key), psl[:, 0:n], bl, 1.0 - LIMIT, ALU.add, ALU.max)
                        S.tt("pool", V(x_.t[:, 0:n], x_.key), V(x_.t[:, 0:n], x_.key), V(s_.t[:, 0:n], s_.key), ALU.mult)
                        S.stt("dve", V(actb.t[:, i, g0:g1], (actb.key, i)), V(l_.t[:, 0:n], l_.key), 1.0 + LIMIT,
                              V(x_.t[:, 0:n], x_.key), ALU.min, ALU.mult)
            for t in tiles:
                for half in range(2):
                    ps = PS.bank()
                    for fc in range(8):
                        S.mm(ps[:, :], V(actb.t[:, fc, t * 128:(t + 1) * 128], (actb.key, fc)),
                             V(w2_.t[:, fc, half * 512:(half + 1) * 512], w2_.key), start=(fc == 0), stop=(fc == 7))
                    S.stt("dve", A(t, half), ps[:, :], V(gates.t[:, t, e_:e_ + 1], ("gates", t)), A(t, half),
                          ALU.mult, ALU.add)
        sc.close(S)

        sc = Scope(nc)
        g2 = bc_tile(sc, "g2", lnp_d[l, 2]); b2 = bc_tile(sc, "b2", lnp_d[l, 3])
        if l == 0:
            G3 = bc_tile(sc, "G3", modbc_d[1, 1]); B3 = bc_tile(sc, "B3", modbc_d[1, 0])
            tmpb = sc.buf("tmpb", [128, 1024])
            S.tt("dve", tmpb[:, :], b2[:, :], G3[:, :], ALU.mult)
            S.tt("dve", B3[:, :], B3[:, :], tmpb[:, :], ALU.add)
            S.tt("dve", G3[:, :], G3[:, :], g2[:, :], ALU.mult)
            S.ts("dve", g2[:, :], g2[:, :], ALPHA, None, ALU.mult)
            S.ts("dve", b2[:, :], b2[:, :], ALPHA, None, ALU.mult)
            h2 = [sc.buf("h2%d" % i, [128, 1024]) for i in range(2)]
            for t in tiles:
                ln_tile(t)
                h_ = h2[t % 2]
                S.tt("dve", h_[:, :], A(t), G3[:, :], ALU.mult)
                S.tt("pool", h_[:, :], h_[:, :], B3[:, :], ALU.add)
                S.tt("dve", A(t), A(t), g2[:, :], ALU.mult)
                S.tt("pool", A(t), A(t), b2[:, :], ALU.add)
                transpose_tile(h_, t)
                if 1 not in layers and t >= 1:
                    S.store(out_d[p, (t - 1) * 128:t * 128, :], A(t))
        else:
            for t in tiles:
                ln_tile(t)
                S.tt("dve", A(t), A(t), g2[:, :], ALU.mult)
                S.tt("pool", A(t), A(t), b2[:, :], ALU.add)
                S.store(out_d[p, (t - 1) * 128:t * 128, :], A(t))
        sc.close(S)

    for p in range(npass):
        if 0 in layers:
            mix0(p)
            post(0, p)
        if 1 in layers:
            mix1(p)
            post(1, p)
    S.barrier()
    S.flush(final=True)
    return nc


def prep_phaseB(inp, og_b, b, s, shared=None):
    f = np.float32
    xh = np.zeros((2, NTOK, D), f)
    ogh = np.zeros((2, NTOK, 2048), f)
    flag = np.ones((128, 2), f)
    for p in range(2):
        t_lo = s * 2048 + p * 1024 - 128
        if t_lo < 0:
            flag[:, p] = 0.0
            xh[p, 128:] = inp["x"][b, 0:1024]
            ogh[p, 128:] = og_b[0:1024]
        else:
            xh[p] = inp["x"][b, t_lo:t_lo + NTOK]
            ogh[p] = og_b[t_lo:t_lo + NTOK]
    m = {"xh": xh, "ogh": ogh, "flag": flag,
         "cb": np.ascontiguousarray(inp["c"][b].reshape(8, 128).T)}
    if shared is None:
        shared = prep_phaseB_shared(inp)
    m.update(shared)
    return m


def prep_phaseB_shared(inp):
    f = np.float32
    bc = lambda v: np.ascontiguousarray(np.broadcast_to(v[None, :], (128, v.shape[0])))
    pp = lambda v, n: np.ascontiguousarray(v.reshape(n, 128).T)
    lnp = np.stack([np.stack([bc(inp[k][l]) for k in ("ln1_g", "ln1_b", "ln2_g", "ln2_b")]) for l in range(2)])
    return {
        "adaw": np.stack([pkc(inp["ada_w"][l]) for l in range(2)]),
        "adab": np.ascontiguousarray(inp["ada_b"][:, None, :]),
        "outw": pkc(inp["dn_out_w"][0]),
        "pw1w": pkc(inp["cf_pw1_w"][0]),
        "pw1b": pp(inp["cf_pw1_b"][0], 16),
        "dww": np.ascontiguousarray(inp["cf_dw_w"][0].reshape(31, 8, 128).transpose(2, 1, 0)),
        "dwb": pp(inp["cf_dw_b"][0], 8),
        "cfg": pp(inp["cf_ln_g"][0], 8),
        "cfb": pp(inp["cf_ln_b"][0], 8),
        "pw2w": pkc(inp["cf_pw2_w"][0]),
        "pw2b": np.ascontiguousarray(inp["cf_pw2_b"][0][None, :]),
        "lnp": np.ascontiguousarray(lnp.astype(f)),
        "rw": np.stack([pkc(inp["router_w"][l]) for l in range(2)]),
        "rb": np.ascontiguousarray(inp["router_b"][:, None, :]),
        "ew1": np.ascontiguousarray(inp["e_w1"]),
        "eb1": np.ascontiguousarray(inp["e_b1"].reshape(2, 4, 128, 128)),
        "ew2": np.ascontiguousarray(inp["e_w2"]),
        "eb2": np.ascontiguousarray(inp["e_b2"]),
        "cst": consts_np(),
    }


def build_fused():
    nc = bass.Bass("TRN2", target_bir_lowering=False)
    dt = nc.dram_tensor
    I = lambda name, shape: dt(name, shape, F32, kind="ExternalInput").ap()
    fz = declare_B(nc, with_og=False)
    fz["nc"] = nc
    fz["x"] = I("x", [SEQ, D])
    win4 = I("win", [4, 128, 8, 1544])
    cw4 = I("cw", [4, 128, 8, 4])
    alog4 = I("alog", [4, 128, 4])
    dtb4 = I("dtb", [4, 128, 4])
    fz["onw"] = I("onw", [128, 512])
    fz["sel"] = I("sel", [128, 4])
    fz["ogg"] = dt("ogg", [4 * OGROWS, 512], F32).ap()
    S = Sched(nc)
    fz["S"] = S
    fz["PS"] = PsumPool(nc, 4, "p")
    fz["PS"].register(S)
    for hg in range(4):
        fh = dict(fz)
        fh["win"], fh["cw"], fh["alog"], fh["dtb"] = win4[hg], cw4[hg], alog4[hg], dtb4[hg]
        fh["ogpad"] = fz["ogg"][hg * OGROWS:(hg + 1) * OGROWS, :]
        build_phaseA(SEQ, fh)
    build_phaseB(fz=fz)
    return nc


def kernel(**inp):
    inp = {k: np.asarray(v) for k, v in inp.items()}
    nc = build_fused()
    shared = prep_phaseB_shared(inp)
    dummy_og = np.zeros((SEQ, 2048), np.float32)
    mA = {b: [prep_phaseA(inp, b, hg, SEQ) for hg in range(4)] for b in range(2)}
    stk = {b: {k: np.ascontiguousarray(np.stack([mA[b][hg][k] for hg in range(4)])) for k in ("win", "cw", "alog", "dtb")}
           for b in range(2)}
    maps = []
    for i in range(8):
        b, r = i // 4, i % 4
        mB = prep_phaseB(inp, dummy_og, b, r, shared)
        m = {k: v for k, v in mB.items() if k != "ogh"}
        m["x"] = mA[b][0]["x"]
        m["onw"] = mA[b][0]["onw"]
        m.update(stk[b])
        sel = np.zeros((128, 4), np.float32)
        sel[:, r] = 1.0
        m["sel"] = sel
        maps.append(m)
    res = run_bass_kernel_spmd(nc, maps, core_ids=list(range(8)))
    out = np.zeros((2, SEQ, D), np.float32)
    for i in range(8):
        b, s_ = i // 4, i % 4
        o = res.results[i]["out"]
        out[b, s_ * 2048:s_ * 2048 + 1024] = o[0]
        out[b, s_ * 2048 + 1024:s_ * 2048 + 2048] = o[1]
    return out
```

```python
import contextlib
import numpy as np
import concourse.bass as bass
import concourse.mybir as mybir
from concourse.bass_utils import run_bass_kernel_spmd

F32 = mybir.dt.float32
BF16 = mybir.dt.bfloat16
ALU = mybir.AluOpType
AF = mybir.ActivationFunctionType

ENGS = ("pe", "dve", "act", "pool", "sp")
N_DMA_SEMS = 12

D = 1024
SEQ = 8192
DEPTH = 2
NEXP = 32
ALPHA = (2 * DEPTH) ** 0.25
LN_EPS = 1e-5
RMS_EPS = 1e-6
L2_EPS = 1e-6
LIMIT = 7.0
SW_ALPHA = 1.702


class V:
    def __init__(self, ap, key):
        self.ap, self.key = ap, key


class Buf:
    def __init__(self, nc, name, shape, dtype, psum=False):
        if psum:
            self.t = nc.alloc_psum_tensor(name, shape, dtype)
        else:
            self.t = nc.alloc_sbuf_tensor("sb_" + name, shape, dtype)
        self.key = name

    def __getitem__(self, idx):
        return V(self.t[idx], self.key)

    def v(self, idx, sub):
        return V(self.t[idx], (self.key, sub))


class Sched:
    def __init__(self, nc):
        self.nc = nc
        self.ops = []
        self.flushed = 0
        self.cnt = {e: 0 for e in ENGS}
        self.last_w = {}
        self.readers = {}
        self.dma_tot = [0] * (N_DMA_SEMS * 3)
        self.dma_last = [None] * (N_DMA_SEMS * 3)
        self.dma_rr = {"sp": 0, "act": 0, "pool": 0}
        self.opinfo = []
        self.esem = {e: nc.alloc_semaphore("s_" + e) for e in ENGS}
        self.dsem = [nc.alloc_semaphore("d%d" % i) for i in range(N_DMA_SEMS * 3)]
        self.known = {e: {} for e in ENGS}
        self.last_on = {e: None for e in ENGS}
        import os
        self.maxops = int(os.environ.get("KSTOP", "0")) or None
        self.marks = {}
        self.rar_keys = set()

    def mark(self, label):
        self.marks.setdefault(label, len(self.ops))

    def _deps(self, reads, writes):
        d = set()
        for k in reads:
            w = self.last_w.get(k)
            if w is not None:
                d.add(w)
            if k in self.rar_keys:
                for r in self.readers.get(k, ()):
                    d.add(r)
        for k in writes:
            w = self.last_w.get(k)
            if w is not None:
                d.add(w)
            for r in self.readers.get(k, ()):
                d.add(r)
        return d

    def _commit(self, oid, reads, writes):
        for k in reads:
            self.readers.setdefault(k, []).append(oid)
        for k in writes:
            self.last_w[k] = oid
            self.readers[k] = []

    def op(self, eng, fn, reads=(), writes=()):
        if self.maxops is not None and len(self.ops) >= self.maxops:
            return -1
        reads = tuple(reads)
        writes = tuple(writes)
        deps = self._deps(reads, writes)
        oid = len(self.ops)
        self.cnt[eng] += 1
        self.opinfo.append(("c", eng, self.cnt[eng]))
        self.ops.append((eng, fn, deps, None))
        self._commit(oid, reads, writes)
        self.last_on[eng] = oid
        return oid

    def dma(self, fn, reads=(), writes=(), q="sp"):
        if self.maxops is not None and len(self.ops) >= self.maxops:
            return -1
        reads = tuple(reads)
        writes = tuple(writes)
        deps = self._deps(reads, writes)
        base = {"sp": 0, "act": N_DMA_SEMS, "pool": 2 * N_DMA_SEMS}[q]
        slot = base + self.dma_rr[q]
        self.dma_rr[q] = (self.dma_rr[q] + 1) % N_DMA_SEMS
        if self.dma_last[slot] is not None:
            deps.add(self.dma_last[slot])
        oid = len(self.ops)
        self.dma_tot[slot] += 16
        self.opinfo.append(("d", slot, self.dma_tot[slot]))
        self.dma_last[slot] = oid
        self.ops.append((q, fn, deps, slot))
        self._commit(oid, reads, writes)
        return oid

    def barrier(self):
        if self.maxops is not None and len(self.ops) >= self.maxops:
            return
        deps = set()
        for e in ENGS:
            if self.last_on[e] is not None:
                deps.add(self.last_on[e])
        for o in self.dma_last:
            if o is not None:
                deps.add(o)
        for e in ENGS:
            self.ops.append((e, None, set(deps), "bar"))
            self.opinfo.append(("n", None, 0))
        self.last_w = {}
        self.readers = {}

    def flush(self, final=False):
        nc = self.nc
        lo, hi = self.flushed, len(self.ops)
        self.flushed = hi
        per_eng = {e: [] for e in ENGS}
        for oid in range(lo, hi):
            per_eng[self.ops[oid][0]].append(oid)
        final_deps = []
        if final:
            for o in self.dma_last:
                if o is not None:
                    final_deps.append(o)

        def run(engname, engobj):
            known = self.known[engname]

            def wait_for(dlist, is_pe_compute):
                need = {}
                for d in dlist:
                    kind, a, v = self.opinfo[d]
                    if kind == "n":
                        continue
                    if kind == "c":
                        if a == "pe" and is_pe_compute:
                            continue
                        key = ("c", a)
                    else:
                        key = ("d", a)
                    if v > need.get(key, 0):
                        need[key] = v
                for key, v in need.items():
                    if known.get(key, 0) >= v:
                        continue
                    known[key] = v
                    sem = self.esem[key[1]] if key[0] == "c" else self.dsem[key[1]]
                    engobj.wait_ge(sem, v)

            for oid in per_eng[engname]:
                eng, fn, deps, slot = self.ops[oid]
                if slot == "bar":
                    wait_for(deps, False)
                    continue
                wait_for(deps, engname == "pe" and slot is None)
                ins = fn(engobj)
                if slot is None:
                    ins.then_inc(self.esem[engname], 1)
                else:
                    ins.then_inc(self.dsem[slot], 16)
            if final and engname == "sp":
                wait_for(final_deps, False)

        with nc.Block() as block:
            block.tensor(lambda e: run("pe", e))
            block.vector(lambda e: run("dve", e))
            block.scalar(lambda e: run("act", e))
            block.gpsimd(lambda e: run("pool", e))
            block.sync(lambda e: run("sp", e))

    @staticmethod
    def _k(*vs):
        return [v.key for v in vs if isinstance(v, V)]

    @staticmethod
    def _a(x):
        return x.ap if isinstance(x, V) else x

    def mm(self, out, lhsT, rhs, start=True, stop=True):
        return self.op("pe", lambda e: e.matmul(out.ap, lhsT.ap, rhs.ap, start=start, stop=stop),
                       reads=self._k(lhsT, rhs), writes=[out.key])

    def tr(self, out, in_, ident):
        return self.op("pe", lambda e: e.transpose(out.ap, in_.ap, ident.ap),
                       reads=self._k(in_, ident), writes=[out.key])

    def act(self, out, in_, func, bias=0.0, scale=1.0, accum=None, eng="act"):
        a = self._a
        kw = {}
        if accum is not None:
            kw["accum_out"] = accum.ap
        return self.op(eng, lambda e: e.activation(out.ap, in_.ap, func, bias=a(bias), scale=a(scale), **kw),
                       reads=self._k(in_, bias, scale), writes=self._k(out, accum))

    def ts(self, eng, out, in0, s1, s2, op0, op1=None):
        a = self._a
        if op1 is None:
            f = lambda e: e.tensor_scalar(out.ap, in0.ap, a(s1), None, op0)
        else:
            f = lambda e: e.tensor_scalar(out.ap, in0.ap, a(s1), a(s2), op0, op1)
        return self.op(eng, f, reads=self._k(in0, s1, s2), writes=[out.key])

    def tt(self, eng, out, in0, in1, op):
        return self.op(eng, lambda e: e.tensor_tensor(out.ap, in0.ap, in1.ap, op),
                       reads=self._k(in0, in1), writes=[out.key])

    def stt(self, eng, out, in0, s, in1, op0, op1):
        a = self._a
        eng = "dve"
        return self.op(eng, lambda e: e.scalar_tensor_tensor(out.ap, in0.ap, a(s), in1.ap, op0, op1),
                       reads=self._k(in0, s, in1), writes=[out.key])

    def copy(self, eng, out, in_):
        if eng == "act":
            return self.act(out, in_, AF.Copy)
        return self.op(eng, lambda e: e.tensor_copy(out.ap, in_.ap), reads=[in_.key], writes=[out.key])

    def memset(self, eng, out, val):
        return self.op(eng, lambda e: e.memset(out.ap, val), writes=[out.key])

    def load(self, out, src_ap, q="sp"):
        return self.dma(lambda e: e.dma_start(out=out.ap, in_=src_ap), writes=[out.key], q=q)

    def store(self, dst_ap, in_, q="sp", dkey=None):
        w = [dkey] if dkey is not None else []
        return self.dma(lambda e: e.dma_start(out=dst_ap, in_=in_.ap), reads=[in_.key], writes=w, q=q)


class PsumPool:
    def __init__(self, nc, n_big, prefix):
        self.banks = [Buf(nc, "%s_ps%d" % (prefix, b), [128, 512], F32, psum=True) for b in range(8)]
        self.configure(n_big)

    def register(self, S):
        S.rar_keys.update(b.key for b in self.banks)

    def configure(self, n_big):
        self.big = self.banks[:n_big]
        self.small = [(buf, q) for buf in self.banks[n_big:] for q in range(4)]
        self.bi = 0
        self.si = 0

    def bank(self):
        b = self.big[self.bi]
        self.bi = (self.bi + 1) % len(self.big)
        return b

    def quarter(self, w=128):
        nb = len(self.small) // 4
        i = self.si
        self.si = (self.si + 1) % len(self.small)
        buf, _ = self.small[(i % nb) * 4]
        q = (i // nb) % 4
        return V(buf.t[:, q * 128:q * 128 + w], buf.key)


def ada_broadcast(S, nc, PS, condB, ones_row, adaw_ap, adab_ap, col0, ncols, stage, outs, ident_unused=None):
    npieces = ncols // 512
    for pi in range(npieces):
        c0 = col0 + pi * 512
        st = stage
        S.load(V(st.t[:, :, :], st.key), adaw_ap[:, :, c0:c0 + 512])
        brow = outs["brow"]
        S.load(V(brow.t[0:1, 0:512], brow.key), adab_ap[0:1, c0:c0 + 512])
        ps = PS.bank()
        for kc in range(8):
            S.mm(ps[:, :], V(condB.t[:, kc, :], condB.key), V(st.t[:, kc, :], st.key), start=(kc == 0), stop=False)
        S.mm(ps[:, :], V(ones_row.t[0:1, :], ones_row.key), V(brow.t[0:1, 0:512], brow.key), start=False, stop=True)
        dst, add_one = outs["dst"][pi]
        if add_one:
            S.ts("dve", dst, ps[:, :], 1.0, None, ALU.add)
        else:
            S.copy("dve", dst, ps[:, :])


def build_phaseA(n_tok=SEQ, fz=None):
    if fz is None:
        nc = bass.Bass("TRN2", target_bir_lowering=False)
        dt = nc.dram_tensor
        x_d = dt("x", [n_tok, D], F32, kind="ExternalInput").ap()
        cb_d = dt("cb", [128, 8], F32, kind="ExternalInput").ap()
        adaw_d = dt("adaw", [128, 8, 2048], F32, kind="ExternalInput").ap()
        adab_d = dt("adab", [1, 2048], F32, kind="ExternalInput").ap()
        win_d = dt("win", [128, 8, 1544], F32, kind="ExternalInput").ap()
        cw_d = dt("cw", [128, 8, 4], F32, kind="ExternalInput").ap()
        alog_d = dt("alog", [128, 4], F32, kind="ExternalInput").ap()
        dtb_d = dt("dtb", [128, 4], F32, kind="ExternalInput").ap()
        onw_d = dt("onw", [128, 512], F32, kind="ExternalInput").ap()
        cst_d = dt("cst", [128, 4, 128], F32, kind="ExternalInput").ap()
        og_d = dt("og", [n_tok, 512], F32, kind="ExternalOutput").ap()
        S = Sched(nc)
        PS = PsumPool(nc, 4, "a")
        PS.register(S)
    else:
        nc, S, PS = fz["nc"], fz["S"], fz["PS"]
        PS.configure(4)
        x_d, cb_d, cst_d = fz["x"], fz["cb"], fz["cst"]
        adaw_d, adab_d = fz["adaw"][0], fz["adab"][0]
        win_d, cw_d, alog_d, dtb_d, onw_d = fz["win"], fz["cw"], fz["alog"], fz["dtb"], fz["onw"]
        og_d = fz["ogpad"][128:128 + n_tok, :]
    scA = Scope(nc)
    scA.plain = True
    B = lambda name, shape, dtp=F32: scA.buf(name, shape, dtp)

    cst = B("cst", [128, 4, 128])
    ident = V(cst.t[:, 0, :], "cst")
    ones = V(cst.t[:, 1, :], "cst")
    U = V(cst.t[:, 2, :], "cst")
    LS = V(cst.t[:, 3, :], "cst")
    S.load(V(cst.t[:, :, :], "cst"), cst_d)
    win = B("win", [128, 8, 1544], BF16)
    for kc in range(8):
        S.load(V(win.t[:, kc, :], "win"), win_d[:, kc, :], q="pool")
    cw = B("cw", [128, 8, 4])
    S.load(V(cw.t[:, :, :], "cw"), cw_d)
    alog = B("alog", [128, 4]); dtb = B("dtb", [128, 4]); onw = B("onw", [128, 512])
    S.load(alog[:, :], alog_d); S.load(dtb[:, :], dtb_d); S.load(onw[:, :], onw_d)
    negA = B("negA", [128, 4])
    S.act(negA[:, :], alog[:, :], AF.Exp)
    S.ts("dve", negA[:, :], negA[:, :], -1.0, None, ALU.mult)

    cbt = B("cbt", [128, 8])
    S.load(cbt[:, :], cb_d)
    cond = B("cond", [128, 8])
    S.act(cond[:, :], cbt[:, :], AF.Silu)
    condB = B("condB", [128, 8, 128])
    for kc in range(8):
        S.ts("dve", V(condB.t[:, kc, :], "condB"), ones, V(cond.t[:, kc:kc + 1], "cond"), None, ALU.mult)
    h4 = B("h4", [128, 4, 1024])
    sh_bc = B("sh_bc", [128, 1024]); scp_bc = B("scp_bc", [128, 1024])
    brow = B("brow", [1, 512])
    stg = B("adastage", [128, 8, 512])
    outs = {"brow": brow, "dst": [(V(sh_bc.t[:, 0:512], "sh_bc"), False), (V(sh_bc.t[:, 512:1024], "sh_bc"), False),
                                  (V(scp_bc.t[:, 0:512], "scp_bc"), True), (V(scp_bc.t[:, 512:1024], "scp_bc"), True)]}
    ada_broadcast(S, nc, PS, condB, Buf_view(cst, 1), adaw_d, adab_d, 0, 2048, stg, outs)

    xt = [B("xt%d" % i, [128, 1024]) for i in range(2)]
    hT = B("hT", [128, 8, 512], BF16)
    pre = B("pre", [128, 8, 515])
    S.memset("dve", V(pre.t[:, :, :], ("pre", "all")), 0.0)
    cv = [B("cv%d" % i, [128, 512]) for i in range(2)]
    qkvs = B("qkvs", [128, 8, 512])
    rn = B("rn", [128, 512])
    vtok = B("vtok", [128, 512]); ktok = B("ktok", [128, 256]); gz = B("gz", [128, 512])
    og = [B("og%d" % i, [128, 512]) for i in range(2)]
    sm = B("sm", [128, 64])
    Sst = [[B("S%d_%d" % (h, i), [128, 128]) for i in range(2)] for h in range(4)]
    for h in range(4):
        S.memset("pool", Sst[h][0][:, :], 0.0)
    W = {}
    for h in range(4):
        for nm in ("rhsU", "Gs", "e1", "DLs", "e2", "DUi", "P0", "N0", "P1", "N1", "R", "vb", "kbg", "negwT",
                   "vnew", "eg", "qgT", "kd", "QKmT", "junk"):
            W[(h, nm)] = B("w%d_%s" % (h, nm), [128, 128])
        W[(h, "s")] = B("w%d_s" % h, [128, 8])
    S.barrier()

    nblk = n_tok // 512
    og_i = 0
    S.mark("setup_done")
    for blk in range(nblk):
        t0 = blk * 512
        for c in range(4):
            xb = xt[c % 2]
            S.load(xb[:, :], x_d[t0 + c * 128:t0 + (c + 1) * 128, :])
            hv = h4.v((slice(None), c, slice(None)), c)
            S.tt("dve", hv, xb[:, :], scp_bc[:, :], ALU.mult)
            S.tt("pool", hv, hv, sh_bc[:, :], ALU.add)
        S.mark("modulated")
        for kc in range(8):
            ps = PS.bank()
            for c in range(4):
                hv = h4.v((slice(None), c, slice(kc * 128, (kc + 1) * 128)), c)
                S.tr(ps[:, c * 128:(c + 1) * 128], hv, ident)
            S.copy("act" if kc % 2 else "dve", hT.v((slice(None), kc, slice(None)), kc), ps[:, :])
        S.mark("transposed")
        for fc in range(8):
            ps = PS.bank()
            for kc in range(8):
                S.mm(ps[:, :], V(win.t[:, kc, fc * 128:(fc + 1) * 128], "win"),
                     hT.v((slice(None), kc, slice(None)), kc), start=(kc == 0), stop=(kc == 7))
            pk = ("pre", fc) if blk > 0 or True else None
            S.copy("act", V(pre.t[:, fc, 3:515], ("pre", fc)), ps[:, :])
            c_ = cv[fc % 2]
            e2_ = "dve"
            S.ts(e2_, c_[:, :], V(pre.t[:, fc, 0:512], ("pre", fc)), V(cw.t[:, fc, 0:1], "cw"), None, ALU.mult)
            for j in range(1, 4):
                S.stt(e2_, c_[:, :], V(pre.t[:, fc, j:j + 512], ("pre", fc)), V(cw.t[:, fc, j:j + 1], "cw"),
                      c_[:, :], ALU.mult, ALU.add)
            S.act(qkvs.v((slice(None), fc, slice(None)), fc), c_[:, :], AF.Silu)
            S.copy("pool", V(pre.t[:, fc, 0:3], ("pre", fc)), V(pre.t[:, fc, 512:515], ("pre", fc)))
        S.mark("conv_done")
        for fc in range(4):
            qv = qkvs.v((slice(None), fc, slice(None)), fc)
            c_ = cv[fc % 2]
            S.act(c_[:, :], qv, AF.Square)
            ps = PS.bank()
            S.mm(ps[:, :], ones, c_[:, :])
            S.ts("dve", rn[:, :], ps[:, :], L2_EPS, None, ALU.add)
            S.act(rn[:, :], rn[:, :], AF.Sqrt)
            S.op("dve", lambda e: e.reciprocal(rn.t[:, :], rn.t[:, :]), reads=["rn"], writes=["rn"])
            if fc < 2:
                S.stt("dve", qv, qv, 128.0 ** -0.5, rn[:, :], ALU.mult, ALU.mult)
            else:
                S.tt("dve", qv, qv, rn[:, :], ALU.mult)

        S.mark("l2norm_done")
        for c in range(4):
            cs = slice(c * 128, (c + 1) * 128)
            ps = PS.bank()
            for h in range(4):
                S.tr(ps[:, h * 128:(h + 1) * 128], qkvs.v((slice(None), 4 + h, cs), 4 + h), ident)
            S.copy("act", vtok[:, :], ps[:, :])
            ps = PS.bank()
            for qh in range(2):
                S.tr(ps[:, qh * 128:(qh + 1) * 128], qkvs.v((slice(None), 2 + qh, cs), 2 + qh), ident)
            S.copy("dve", ktok[:, :], ps[:, 0:256])
            ps = PS.bank()
            for kc in range(8):
                S.mm(ps[:, :], hT.v((slice(None), kc, cs), kc), V(win.t[:, kc, 1024:1536], "win"),
                     start=(kc == 0), stop=(kc == 7))
            S.act(gz[:, :], ps[:, :], AF.Silu)
            S.tt("pool", gz[:, :], gz[:, :], onw[:, :], ALU.mult)
            pq = PS.quarter(8)
            for kc in range(8):
                S.mm(pq, hT.v((slice(None), kc, cs), kc), V(win.t[:, kc, 1536:1544], "win"),
                     start=(kc == 0), stop=(kc == 7))
            smv = lambda a, b: V(sm.t[:, a:b], "sm")
            S.copy("dve", smv(32, 40), pq)
            S.act(smv(0, 4), smv(32, 36), AF.Sigmoid)
            S.ts("dve", smv(4, 8), smv(0, 4), -1.0, None, ALU.mult)
            S.tt("dve", smv(8, 12), smv(36, 40), dtb[:, :], ALU.add)
            S.act(smv(8, 12), smv(8, 12), AF.Exp)
            S.act(smv(8, 12), smv(8, 12), AF.Ln, bias=1.0)
            S.tt("dve", smv(12, 16), smv(8, 12), negA[:, :], ALU.mult)
            pq2 = PS.quarter(4)
            S.mm(pq2, U, smv(12, 16))
            S.copy("dve", smv(16, 20), pq2)
            S.act(smv(20, 24), smv(16, 20), AF.Exp)
            S.tt("dve", smv(24, 28), smv(0, 4), smv(20, 24), ALU.mult)

            S.mark("chunkprep_done")
            KK = []
            QKT = []
            for qh in range(2):
                kT = qkvs.v((slice(None), 2 + qh, cs), 2 + qh)
                qT = qkvs.v((slice(None), qh, cs), qh)
                p1 = PS.quarter()
                S.mm(p1, kT, kT)
                p2 = PS.quarter()
                S.mm(p2, kT, qT)
                KK.append(p1)
                QKT.append(p2)
            w = lambda h, nm: W[(h, nm)][:, :]
            H4 = range(4)
            gcol = lambda h: V(sm.t[:, 16 + h:17 + h], "sm")
            sv = lambda h, a_, b_: V(W[(h, "s")].t[:, a_:b_], W[(h, "s")].key)
            glast = lambda h: V(W[(h, "Gs")].t[:, 127:128], W[(h, "Gs")].key)
            ktk = lambda h: V(ktok.t[:, (h // 2) * 128:(h // 2 + 1) * 128], "ktok")
            for h in H4:
                S.act(w(h, "rhsU"), U, AF.Copy, scale=V(sm.t[:, 12 + h:13 + h], "sm"))
            pg = {}
            for h in H4:
                pg[h] = PS.quarter()
                S.mm(pg[h], ones, w(h, "rhsU"))
            for h in H4:
                S.copy("act", w(h, "Gs"), pg[h])
            for h in H4:
                S.ts("dve", w(h, "e1"), w(h, "Gs"), gcol(h), 0.0, ALU.subtract, ALU.max)
                S.ts("dve", w(h, "e2"), w(h, "Gs"), gcol(h), 0.0, ALU.subtract, ALU.min)
            for h in H4:
                S.act(w(h, "e1"), w(h, "e1"), AF.Exp, scale=-1.0)
                S.act(w(h, "e2"), w(h, "e2"), AF.Exp)
                S.act(w(h, "eg"), w(h, "Gs"), AF.Exp)
                S.act(sv(h, 0, 1), gcol(h), AF.Exp, bias=glast(h), scale=-1.0)
                S.act(sv(h, 1, 2), glast(h), AF.Exp)
            for h in H4:
                S.tt("pool", w(h, "DLs"), w(h, "e1"), LS, ALU.mult)
                S.tt("pool", w(h, "DUi"), w(h, "e2"), U, ALU.mult)
                S.tt("pool", w(h, "qgT"), qkvs.v((slice(None), h // 2, cs), h // 2), w(h, "eg"), ALU.mult)
            for h in H4:
                S.stt("dve", w(h, "P0"), KK[h // 2], V(sm.t[:, 4 + h:5 + h], "sm"), w(h, "DLs"), ALU.mult, ALU.mult)
            pn = {}
            for h in H4:
                pn[h] = PS.quarter()
                S.tr(pn[h], w(h, "P0"), ident)
            for h in H4:
                S.copy("act", w(h, "N0"), pn[h])
            for h in H4:
                S.act(w(h, "vb"), V(vtok.t[:, h * 128:(h + 1) * 128], "vtok"), AF.Copy, scale=V(sm.t[:, h:h + 1], "sm"))
                S.act(w(h, "kbg"), ktk(h), AF.Copy, scale=V(sm.t[:, 24 + h:25 + h], "sm"))
                S.act(w(h, "kd"), ktk(h), AF.Copy, scale=sv(h, 0, 1))
            for h in H4:
                S.tt("dve", w(h, "R"), w(h, "N0"), ident, ALU.add)
                S.tt("dve", w(h, "QKmT"), QKT[h // 2], w(h, "DUi"), ALU.mult)
            S.mark("stage1_done")
            cur = {h: ("P0", "N0") for h in H4}
            for m in range(1, 7):
                nxt = {h: (("P1", "N1") if cur[h][0] == "P0" else ("P0", "N0")) for h in H4}
                pp, pnn, pr = {}, {}, {}
                for h in H4:
                    pp[h] = PS.quarter()
                    S.mm(pp[h], w(h, cur[h][1]), w(h, cur[h][0]))
                if m < 6:
                    PS.quarter(); PS.quarter()
                    for h in H4:
                        pnn[h] = PS.quarter()
                        S.mm(pnn[h], w(h, cur[h][0]), w(h, cur[h][1]))
                    PS.quarter(); PS.quarter()
                for h in H4:
                    S.copy("act", w(h, nxt[h][0]), pp[h])
                if m < 6:
                    for h in H4:
                        S.copy("dve", w(h, nxt[h][1]), pnn[h])
                for h in H4:
                    pr[h] = PS.quarter()
                    S.mm(pr[h], w(h, nxt[h][0]), w(h, "R"))
                for h in H4:
                    S.tt("dve", w(h, "R"), pr[h], w(h, "R"), ALU.add)
                cur = nxt
            S.mark("stage2_done")
            ogb = og[og_i % 2]
            og_i += 1
            Sc = {h: Sst[h][(blk * 4 + c) % 2] for h in H4}
            Sn = {h: Sst[h][(blk * 4 + c + 1) % 2] for h in H4}
            pw, pv, po, pu = {}, {}, {}, {}
            for h in H4:
                pw[h] = PS.quarter()
                S.mm(pw[h], w(h, "kbg"), w(h, "R"))
            for h in H4:
                S.act(w(h, "negwT"), pw[h], AF.Copy, scale=-1.0)
            for h in H4:
                pv[h] = PS.quarter()
                S.mm(pv[h], w(h, "R"), w(h, "vb"), start=True, stop=False)
                S.mm(pv[h], w(h, "negwT"), Sc[h][:, :], start=False, stop=True)
            for h in H4:
                S.copy("dve", w(h, "vnew"), pv[h])
            for h in H4:
                po[h] = PS.quarter()
                S.mm(po[h], w(h, "qgT"), Sc[h][:, :], start=True, stop=False)
                S.mm(po[h], w(h, "QKmT"), w(h, "vnew"), start=False, stop=True)
            for h in H4:
                pu[h] = PS.quarter()
                S.mm(pu[h], w(h, "kd"), w(h, "vnew"))
            for h in H4:
                S.stt("dve", Sn[h][:, :], Sc[h][:, :], sv(h, 1, 2), pu[h], ALU.mult, ALU.add)
            for h in H4:
                S.copy("dve", w(h, "e1"), po[h])
            for h in H4:
                S.act(w(h, "junk"), w(h, "e1"), AF.Square, accum=sv(h, 2, 3))
            for h in H4:
                S.ts("dve", sv(h, 3, 4), sv(h, 2, 3), 1.0 / 128.0, RMS_EPS, ALU.mult, ALU.add)
            for h in H4:
                S.act(sv(h, 3, 4), sv(h, 3, 4), AF.Sqrt)
            for h in H4:
                S.op("dve", lambda e, h=h: e.reciprocal(W[(h, "s")].t[:, 3:4], W[(h, "s")].t[:, 3:4]),
                     reads=[W[(h, "s")].key], writes=[W[(h, "s")].key])
            for h in H4:
                S.stt("dve", V(ogb.t[:, h * 128:(h + 1) * 128], ogb.key), w(h, "e1"), sv(h, 3, 4),
                      V(gz.t[:, h * 128:(h + 1) * 128], "gz"), ALU.mult, ALU.mult)
            S.store(og_d[t0 + c * 128:t0 + (c + 1) * 128, :], ogb[:, :])
            S.mark("chunk0_done")
    if fz is None:
        S.flush(final=True)
        return nc
    zt = V(W[(0, "junk")].t[:, :], W[(0, "junk")].key)
    S.memset("dve", zt, 0.0)
    for q4 in range(4):
        S.store(fz["ogpad"][0:128, q4 * 128:(q4 + 1) * 128], zt)
    scA.close(S)
    return nc


def Buf_view(buf, idx):
    b = Buf.__new__(Buf)
    b.t = buf.t[:, idx, :]
    b.key = buf.key
    return b


def consts_np():
    i = np.arange(128)
    ident = np.eye(128, dtype=np.float32)
    ones = np.ones((128, 128), np.float32)
    U = (i[:, None] <= i[None, :]).astype(np.float32)
    LS = (i[:, None] > i[None, :]).astype(np.float32)
    return np.ascontiguousarray(np.stack([ident, ones, U, LS], axis=1))


def pkc(w):
    K, F = w.shape
    return np.ascontiguousarray(w.reshape(K // 128, 128, F).transpose(1, 0, 2))


def prep_phaseA(inp, b, hg, n_tok=SEQ):
    f = np.float32
    in_w = inp["dn_in_w"][0]
    cols = np.concatenate([np.arange(256 * hg, 256 * hg + 256), 1024 + np.arange(256 * hg, 256 * hg + 256),
                           2048 + np.arange(512 * hg, 512 * hg + 512), 4096 + np.arange(512 * hg, 512 * hg + 512),
                           6144 + np.arange(4 * hg, 4 * hg + 4), 6160 + np.arange(4 * hg, 4 * hg + 4)])
    ccols = cols[:1024]
    cw = inp["dn_conv_w"][0][:, ccols]
    cw = np.ascontiguousarray(cw.reshape(4, 8, 128).transpose(2, 1, 0))
    hs = slice(4 * hg, 4 * hg + 4)
    return {
        "x": np.ascontiguousarray(inp["x"][b, :n_tok]).astype(f),
        "cb": np.ascontiguousarray(inp["c"][b].reshape(8, 128).T),
        "adaw": pkc(inp["ada_w"][0][:, :2048]),
        "adab": np.ascontiguousarray(inp["ada_b"][0][None, :2048]),
        "win": pkc(in_w[:, cols]),
        "cw": cw,
        "alog": np.ascontiguousarray(np.broadcast_to(inp["dn_A_log"][0][hs][None, :], (128, 4))),
        "dtb": np.ascontiguousarray(np.broadcast_to(inp["dn_dt_bias"][0][hs][None, :], (128, 4))),
        "onw": np.ascontiguousarray(np.broadcast_to(np.tile(inp["dn_onorm_w"][0], 4)[None, :], (128, 512))),
        "cst": consts_np(),
    }


class Scope:
    _n = 0

    def __init__(self, nc):
        self.nc = nc
        self.st = contextlib.ExitStack()
        Scope._n += 1
        self.sfx = "_s%d" % Scope._n
        self.plain = False

    def buf(self, name, shape, dtp=F32):
        b = Buf.__new__(Buf)
        b.t = self.st.enter_context(self.nc.sbuf_tensor("sb_" + name + self.sfx, shape, dtp))
        b.key = name if self.plain else name + self.sfx
        return b

    def close(self, S):
        S.barrier()
        S.flush()
        self.st.close()


NTILE = 9
NTOK = NTILE * 128
OGROWS = SEQ + 128


def declare_B(nc, with_og=True):
    dt = nc.dram_tensor
    I = lambda name, shape: dt(name, shape, F32, kind="ExternalInput").ap()
    d = {}
    d["xh"] = I("xh", [2, NTOK, D])
    if with_og:
        d["ogh"] = I("ogh", [2, NTOK, 2048])
    d["flag"] = I("flag", [128, 2])
    d["cb"] = I("cb", [128, 8])
    d["adaw"] = I("adaw", [2, 128, 8, 6144])
    d["adab"] = I("adab", [2, 1, 6144])
    d["outw"] = I("outw", [128, 16, 1024])
    d["pw1w"] = I("pw1w", [128, 8, 2048])
    d["pw1b"] = I("pw1b", [128, 16])
    d["dww"] = I("dww", [128, 8, 31])
    d["dwb"] = I("dwb", [128, 8])
    d["cfg"] = I("cfg", [128, 8])
    d["cfb"] = I("cfb", [128, 8])
    d["pw2w"] = I("pw2w", [128, 8, 1024])
    d["pw2b"] = I("pw2b", [1, 1024])
    d["lnp"] = I("lnp", [2, 4, 128, 1024])
    d["rw"] = I("rw", [2, 128, 8, 32])
    d["rb"] = I("rb", [2, 1, 32])
    d["ew1"] = I("ew1", [2, NEXP, 1024, 2048])
    d["eb1"] = I("eb1", [2, 4, 128, 128])
    d["ew2"] = I("ew2", [2, NEXP, 1024, 1024])
    d["eb2"] = I("eb2", [2, NEXP, 1024])
    d["cst"] = I("cst", [128, 4, 128])
    d["out"] = dt("out", [2, 1024, D], F32, kind="ExternalOutput").ap()
    d["modbc"] = dt("modbc", [2, 6, 128, 1024], F32).ap()
    return d


def build_phaseB(n_exp=NEXP, layers=(0, 1), npass=2, fz=None):
    if fz is None:
        nc = bass.Bass("TRN2", target_bir_lowering=False)
        d = declare_B(nc)
    else:
        nc = fz["nc"]
        d = fz
    xh_d, flag_d, cb_d, adaw_d, adab_d = d["xh"], d["flag"], d["cb"], d["adaw"], d["adab"]
    og_d = d.get("ogh")
    outw_d, pw1w_d, pw1b_d, dww_d, dwb_d, cfg_d, cfb_d = (d[k] for k in ("outw", "pw1w", "pw1b", "dww", "dwb", "cfg", "cfb"))
    pw2w_d, pw2b_d, lnp_d, rw_d, rb_d = (d[k] for k in ("pw2w", "pw2b", "lnp", "rw", "rb"))
    ew1_d, eb1_d, ew2_d, eb2_d, cst_d, out_d, modbc_d = (d[k] for k in ("ew1", "eb1", "ew2", "eb2", "cst", "out", "modbc"))

    if fz is None:
        S = Sched(nc)
        PS = PsumPool(nc, 6, "b")
        PS.register(S)
    else:
        S, PS = fz["S"], fz["PS"]
        PS.configure(6)
    B = lambda name, shape, dtp=F32: Buf(nc, "b_" + name, shape, dtp)
    cst = B("cst", [128, 4, 128])
    cst.key = "cst"
    ident = V(cst.t[:, 0, :], "cst")
    ones = V(cst.t[:, 1, :], "cst")
    ones_row = V(cst.t[0:1, 1, :], "cst")
    S.load(V(cst.t[:, :, :], "cst"), cst_d)
    flag = B("flag", [128, 2]); flag.key = "flag"
    S.load(flag[:, :], flag_d)
    acc = B("acc", [128, NTILE, 1024]); acc.key = "acc"
    hT = B("hT", [128, 8, NTOK], BF16); hT.key = "hT"
    gates = B("gates", [128, NTILE, 32]); gates.key = "gates"
    gT = B("gT", [32, NTOK]); gT.key = "gT"
    lns = B("lns", [128, NTILE, 16]); lns.key = "lns"
    if fz is not None:
        sel = B("sel", [128, 4]); sel.key = "sel"
        S.load(sel[:, :], fz["sel"])

    A = lambda t, half=None: (acc.v((slice(None), t, slice(None)), t) if half is None else
                              acc.v((slice(None), t, slice(half * 512, half * 512 + 512)), t))

    def ld(out, src, rkey=None, q="sp"):
        r = [rkey] if rkey is not None else []
        return S.dma(lambda e: e.dma_start(out=out.ap, in_=src), reads=r, writes=[out.key], q=q)

    sc = Scope(nc)
    cbt = sc.buf("cbt", [128, 8]); cond = sc.buf("cond", [128, 8]); condB = sc.buf("condB", [128, 8, 128])
    ld(cbt[:, :], cb_d)
    S.act(cond[:, :], cbt[:, :], AF.Silu)
    for kc in range(8):
        S.ts("dve", V(condB.t[:, kc, :], condB.key), ones, V(cond.t[:, kc:kc + 1], cond.key), None, ALU.mult)
    stg = [sc.buf("stg%d" % i, [128, 8, 512]) for i in range(2)]
    brow = [sc.buf("brow%d" % i, [1, 512]) for i in range(2)]
    mtmp = [sc.buf("mtmp%d" % i, [128, 512]) for i in range(2)]
    pi = 0
    for l in range(2):
        for ch in range(6):
            for half in range(2):
                c0 = ch * 1024 + half * 512
                st_, br_, mt_ = stg[pi % 2], brow[pi % 2], mtmp[pi % 2]
                pi += 1
                ld(V(st_.t[:, :, :], st_.key), adaw_d[l, :, :, c0:c0 + 512])
                ld(V(br_.t[0:1, :], br_.key), adab_d[l, 0:1, c0:c0 + 512])
                ps = PS.bank()
                for kc in range(8):
                    S.mm(ps[:, :], V(condB.t[:, kc, :], condB.key), V(st_.t[:, kc, :], st_.key), start=(kc == 0), stop=False)
                S.mm(ps[:, :], ones_row, V(br_.t[0:1, :], br_.key), start=False, stop=True)
                if ch in (1, 2, 4, 5):
                    S.ts("dve", mt_[:, :], ps[:, :], 1.0, None, ALU.add)
                else:
                    S.copy("dve", mt_[:, :], ps[:, :])
                S.store(modbc_d[l, ch, :, half * 512:half * 512 + 512], mt_[:, :], dkey=("modbc", l, ch, half))
    sc.close(S)

    def ln_tile(t):
        sv = lambda a, b: V(lns.t[:, t, a:b], ("lns", t))
        S.op("dve", lambda e: e.bn_stats(lns.t[:, t, 0:6], acc.t[:, t, 0:512]), reads=[("acc", t)], writes=[("lns", t)])
        S.op("dve", lambda e: e.bn_stats(lns.t[:, t, 6:12], acc.t[:, t, 512:1024]), reads=[("acc", t)], writes=[("lns", t)])
        S.op("dve", lambda e: e.bn_aggr(lns.t[:, t, 12:14], lns.t[:, t, 0:12]), reads=[("lns", t)], writes=[("lns", t)])
        S.ts("dve", sv(14, 15), sv(13, 14), LN_EPS, None, ALU.add)
        S.act(sv(14, 15), sv(14, 15), AF.Sqrt)
        S.op("dve", lambda e: e.reciprocal(lns.t[:, t, 14:15], lns.t[:, t, 14:15]), reads=[("lns", t)], writes=[("lns", t)])
        S.ts("dve", A(t), A(t), sv(12, 13), sv(14, 15), ALU.subtract, ALU.mult)

    def transpose_tile(src, t, hTf=None):
        for b2 in range(2):
            ps = PS.bank()
            for k4 in range(4):
                kc = b2 * 4 + k4
                S.tr(ps[:, k4 * 128:(k4 + 1) * 128], V(src.t[:, kc * 128:(kc + 1) * 128], src.key), ident)
            psv = V(ps.t[:, :].rearrange("p (k t) -> p k t", k=4), ps.key)
            if hTf is not None:
                S.copy("act", V(hTf.t[:, b2 * 4:b2 * 4 + 4, :], hTf.key), psv)
                S.copy("pool", V(hT.t[:, b2 * 4:b2 * 4 + 4, t * 128:(t + 1) * 128], ("hT", t)),
                       V(hTf.t[:, b2 * 4:b2 * 4 + 4, :], hTf.key))
            else:
                S.copy("act" if b2 else "dve", V(hT.t[:, b2 * 4:b2 * 4 + 4, t * 128:(t + 1) * 128], ("hT", t)), psv)

    def bc_tile(sc_, name, src, rkey=None):
        b = sc_.buf(name, [128, 1024])
        ld(b[:, :], src, rkey)
        return b

    def mix0(p):
        sc = Scope(nc)
        outw = sc.buf("outw", [128, 16, 1024], BF16)
        for fc in range(16):
            ld(V(outw.t[:, fc, :], outw.key), outw_d[:, fc, :], q="pool")
        gt1p = bc_tile(sc, "gt1p", modbc_d[0, 2])
        ogt = [sc.buf("ogt%d" % i, [128, 2048]) for i in range(2)]
        cand = [sc.buf("cand%d" % i, [128, 2048]) for i in range(2)] if fz is not None else None
        ogT = [sc.buf("ogT%d" % i, [128, 2048], BF16) for i in range(2)]
        xt = [sc.buf("xt%d" % i, [128, 1024]) for i in range(2)]
        t1 = [sc.buf("t1%d" % i, [128, 512]) for i in range(2)]
        for t in range(NTILE):
            o_, oT, x_ = ogt[t % 2], ogT[t % 2], xt[t % 2]
            if fz is None:
                ld(o_[:, :], og_d[p, t * 128:(t + 1) * 128, :])
            else:
                for sp in range(4):
                    cd = cand[sp % 2]
                    r0 = sp * 2048 + p * 1024 + t * 128
                    for hg in range(4):
                        S.dma(lambda e, cd=cd, hg=hg, r0=r0: e.dma_start(
                            out=cd.t[:, hg * 512:(hg + 1) * 512], in_=fz["ogg"][hg * OGROWS + r0:hg * OGROWS + r0 + 128, :]),
                            reads=["ogg"], writes=[cd.key])
                    if sp == 0:
                        S.ts("dve", o_[:, :], cd[:, :], V(sel.t[:, 0:1], "sel"), None, ALU.mult)
                    else:
                        S.stt("dve", o_[:, :], cd[:, :], V(sel.t[:, sp:sp + 1], "sel"), o_[:, :], ALU.mult, ALU.add)
            ld(x_[:, :], xh_d[p, t * 128:(t + 1) * 128, :])
            for b4 in range(4):
                ps = PS.bank()
                for k4 in range(4):
                    fc = b4 * 4 + k4
                    S.tr(ps[:, k4 * 128:(k4 + 1) * 128], V(o_.t[:, fc * 128:(fc + 1) * 128], o_.key), ident)
                S.copy("act" if b4 % 2 else "dve", V(oT.t[:, b4 * 512:(b4 + 1) * 512], oT.key), ps[:, :])
            for half in range(2):
                ps = PS.bank()
                for fc in range(16):
                    S.mm(ps[:, :], V(oT.t[:, fc * 128:(fc + 1) * 128], oT.key),
                         V(outw.t[:, fc, half * 512:(half + 1) * 512], outw.key), start=(fc == 0), stop=(fc == 15))
                tt_ = t1[half]
                S.tt("dve", tt_[:, :], ps[:, :], V(gt1p.t[:, half * 512:(half + 1) * 512], gt1p.key), ALU.mult)
                S.stt("dve", A(t, half), V(x_.t[:, half * 512:(half + 1) * 512], x_.key), ALPHA, tt_[:, :], ALU.mult, ALU.add)
        sc.close(S)

    def mix1(p):
        sc = Scope(nc)
        pw1w = sc.buf("pw1w", [128, 8, 2048], BF16)
        pw2w = sc.buf("pw2w", [128, 8, 1024], BF16)
        for kc in range(8):
            ld(V(pw1w.t[:, kc, :], pw1w.key), pw1w_d[:, kc, :], q="pool")
            ld(V(pw2w.t[:, kc, :], pw2w.key), pw2w_d[:, kc, :], q="pool")
        pw1b = sc.buf("pw1b", [128, 16]); dww = sc.buf("dww", [128, 8, 31]); dwb = sc.buf("dwb", [128, 8])
        cfg = sc.buf("cfg", [128, 8]); cfb = sc.buf("cfb", [128, 8]); pw2b = sc.buf("pw2b", [1, 1024])
        ld(pw1b[:, :], pw1b_d); ld(V(dww.t[:, :, :], dww.key), dww_d); ld(dwb[:, :], dwb_d)
        ld(cfg[:, :], cfg_d); ld(cfb[:, :], cfb_d); ld(V(pw2b.t[0:1, :], pw2b.key), pw2b_d)
        gt1p = bc_tile(sc, "gt1p", modbc_d[1, 2])
        ut = [sc.buf("ut%d" % i, [128, NTOK]) for i in range(2)]
        sg = [sc.buf("sg%d" % i, [128, 512]) for i in range(2)]
        cvT = sc.buf("cvT", [128, 8, 1024])
        sT = sc.buf("sT", [128, 8, 1024], BF16)
        mean = sc.buf("mean", [128, 1024]); rstd = sc.buf("rstd", [128, 1024]); m2 = sc.buf("m2", [128, 512])
        groups = [(0, 128), (128, 640), (640, 1152)]
        gi = 0
        for fc in range(8):
            u_ = ut[fc % 2]
            for (g0, g1) in groups:
                n = g1 - g0
                psa = PS.bank(); psg = PS.bank()
                for kc in range(8):
                    S.mm(psa[:, 0:n], V(pw1w.t[:, kc, fc * 128:(fc + 1) * 128], pw1w.key),
                         V(hT.t[:, kc, g0:g1], ("hT", "all")), start=(kc == 0), stop=(kc == 7))
                for kc in range(8):
                    S.mm(psg[:, 0:n], V(pw1w.t[:, kc, 1024 + fc * 128:1024 + (fc + 1) * 128], pw1w.key),
                         V(hT.t[:, kc, g0:g1], ("hT", "all")), start=(kc == 0), stop=(kc == 7))
                s_ = sg[gi % 2]; gi += 1
                S.act(V(s_.t[:, 0:n], s_.key), psg[:, 0:n], AF.Sigmoid, bias=V(pw1b.t[:, 8 + fc:9 + fc], pw1b.key))
                S.stt("dve", V(u_.t[:, g0:g1], u_.key), psa[:, 0:n], V(pw1b.t[:, fc:fc + 1], pw1b.key),
                      V(s_.t[:, 0:n], s_.key), ALU.add, ALU.mult)
            S.ts("dve", V(u_.t[:, 0:128], u_.key), V(u_.t[:, 0:128], u_.key), V(flag.t[:, p:p + 1], "flag"), None, ALU.mult)
            cv = V(cvT.t[:, fc, :], (cvT.key, fc))
            S.ts("dve", cv, V(u_.t[:, 98:98 + 1024], u_.key), V(dww.t[:, fc, 0:1], dww.key),
                 V(dwb.t[:, fc:fc + 1], dwb.key), ALU.mult, ALU.add)
            for j in range(1, 31):
                S.stt("dve", cv, V(u_.t[:, 98 + j:98 + j + 1024], u_.key), V(dww.t[:, fc, j:j + 1], dww.key), cv,
                      ALU.mult, ALU.add)
        for tg in range(2):
            ts_ = slice(tg * 512, (tg + 1) * 512)
            pss = PS.bank(); psq = PS.bank()
            for fc in range(8):
                cvs = V(cvT.t[:, fc, ts_], (cvT.key, fc))
                S.mm(pss[:, :], ones, cvs, start=(fc == 0), stop=(fc == 7))
                s_ = sg[fc % 2]
                S.act(s_[:, :], cvs, AF.Square)
                S.mm(psq[:, :], ones, s_[:, :], start=(fc == 0), stop=(fc == 7))
            mv = V(mean.t[:, ts_], (mean.key, tg)); rv = V(rstd.t[:, ts_], (rstd.key, tg))
            S.ts("dve", mv, pss[:, :], 1.0 / 1024.0, None, ALU.mult)
            S.tt("pool", m2[:, :], mv, mv, ALU.mult)
            S.stt("dve", rv, psq[:, :], 1.0 / 1024.0, m2[:, :], ALU.mult, ALU.subtract)
            S.ts("dve", rv, rv, LN_EPS, None, ALU.add)
            S.act(rv, rv, AF.Sqrt)
            S.op("dve", lambda e, ts_=ts_: e.reciprocal(rstd.t[:, ts_], rstd.t[:, ts_]), reads=[rv.key], writes=[rv.key])
            for fc in range(8):
                cvs = V(cvT.t[:, fc, ts_], (cvT.key, fc))
                S.tt("dve", cvs, cvs, mv, ALU.subtract)
                S.tt("pool", cvs, cvs, rv, ALU.mult)
                S.act(V(sT.t[:, fc, ts_], (sT.key, fc)), cvs, AF.Silu, bias=V(cfb.t[:, fc:fc + 1], cfb.key),
                      scale=V(cfg.t[:, fc:fc + 1], cfg.key))
        t1 = [sc.buf("t1%d" % i, [128, 512]) for i in range(2)]
        for t in range(1, NTILE):
            m0 = (t - 1) * 128
            for half in range(2):
                ps = PS.bank()
                for fc in range(8):
                    S.mm(ps[:, :], V(sT.t[:, fc, m0:m0 + 128], (sT.key, fc)),
                         V(pw2w.t[:, fc, half * 512:(half + 1) * 512], pw2w.key), start=(fc == 0), stop=False)
                S.mm(ps[:, :], ones_row, V(pw2b.t[0:1, half * 512:(half + 1) * 512], pw2b.key), start=False, stop=True)
                tt_ = t1[half]
                S.tt("dve", tt_[:, :], ps[:, :], V(gt1p.t[:, half * 512:(half + 1) * 512], gt1p.key), ALU.mult)
                S.tt("pool", A(t, half), A(t, half), tt_[:, :], ALU.add)
        sc.close(S)

    def post(l, p):
        tiles = list(range(NTILE)) if l == 0 else list(range(1, NTILE))
        sc = Scope(nc)
        lg = bc_tile(sc, "lg", lnp_d[l, 0]); lb = bc_tile(sc, "lb", lnp_d[l, 1])
        G2 = bc_tile(sc, "G2", modbc_d[l, 4]); B2 = bc_tile(sc, "B2", modbc_d[l, 3])
        S.tt("dve", B2[:, :], B2[:, :], B2[:, :], ALU.bypass) if False else None
        tmpb = sc.buf("tmpb", [128, 1024])
        S.tt("dve", tmpb[:, :], lb[:, :], G2[:, :], ALU.mult)
        S.tt("dve", B2[:, :], B2[:, :], tmpb[:, :], ALU.add)
        S.tt("dve", G2[:, :], G2[:, :], lg[:, :], ALU.mult)
        S.ts("dve", lg[:, :], lg[:, :], ALPHA, None, ALU.mult)
        S.ts("dve", lb[:, :], lb[:, :], ALPHA, None, ALU.mult)
        rw = sc.buf("rw", [128, 8, 32]); rb = sc.buf("rb", [1, 32])
        ld(V(rw.t[:, :, :], rw.key), rw_d[l]); ld(V(rb.t[0:1, :], rb.key), rb_d[l])
        h2 = [sc.buf("h2%d" % i, [128, 1024]) for i in range(2)]
        hTf = [sc.buf("hTf%d" % i, [128, 8, 128]) for i in range(2)]
        rt = sc.buf("rt", [128, NTILE, 128])
        for t in tiles:
            ln_tile(t)
            h_ = h2[t % 2]; hf = hTf[t % 2]
            S.tt("dve", h_[:, :], A(t), G2[:, :], ALU.mult)
            S.tt("pool", h_[:, :], h_[:, :], B2[:, :], ALU.add)
            S.tt("dve", A(t), A(t), lg[:, :], ALU.mult)
            S.tt("pool", A(t), A(t), lb[:, :], ALU.add)
            transpose_tile(h_, t, hf)
            pq = PS.quarter(32)
            for kc in range(8):
                S.mm(pq, V(hf.t[:, kc, :], hf.key), V(rw.t[:, kc, :], rw.key), start=(kc == 0), stop=False)
            S.mm(pq, ones_row, V(rb.t[0:1, :], rb.key), start=False, stop=True)
            r = lambda a, b, t=t: V(rt.t[:, t, a:b], (rt.key, t))
            S.copy("dve", r(0, 32), pq)
            S.op("dve", lambda e, t=t: e.max(rt.t[:, t, 32:40], rt.t[:, t, 0:32]), reads=[(rt.key, t)], writes=[(rt.key, t)])
            S.ts("dve", r(40, 72), r(0, 32), r(35, 36), None, ALU.is_ge)
            S.ts("dve", r(72, 73), r(32, 33), -1.0, None, ALU.mult)
            S.act(r(80, 112), r(0, 32), AF.Exp, bias=r(72, 73))
            S.tt("dve", r(80, 112), r(80, 112), r(40, 72), ALU.mult)
            S.op("dve", lambda e, t=t: e.reduce_sum(rt.t[:, t, 73:74], rt.t[:, t, 80:112], mybir.AxisListType.X),
                 reads=[(rt.key, t)], writes=[(rt.key, t)])
            S.op("dve", lambda e, t=t: e.reciprocal(rt.t[:, t, 74:75], rt.t[:, t, 73:74]), reads=[(rt.key, t)], writes=[(rt.key, t)])
            gv = V(gates.t[:, t, :], ("gates", t))
            S.ts("dve", gv, r(80, 112), r(74, 75), None, ALU.mult)
            pq2 = PS.quarter(128)
            S.tr(V(pq2.ap[0:32, :], pq2.key), gv, ident)
            S.copy("act", V(gT.t[0:32, t * 128:(t + 1) * 128], ("gT", t)), V(pq2.ap[0:32, :], pq2.key))
        sc.close(S)

        sc = Scope(nc)
        gt2p = bc_tile(sc, "gt2p", modbc_d[l, 5])
        b2s = sc.buf("b2s", [32, 1024])
        ld(V(b2s.t[0:32, :], b2s.key), eb2_d[l])
        S.tt("dve", V(b2s.t[0:32, :], b2s.key), V(b2s.t[0:32, :], b2s.key), V(gt2p.t[0:32, :], gt2p.key), ALU.mult)
        b1r = sc.buf("b1r", [128, 4, 128]); b1T = sc.buf("b1T", [128, 512])
        ld(V(b1r.t[:, :, :], b1r.key), eb1_d[l].rearrange("a p f -> p a f"))
        ps = PS.bank()
        for a4 in range(4):
            S.tr(ps[:, a4 * 128:(a4 + 1) * 128], V(b1r.t[:, a4, :], b1r.key), ident)
        S.copy("dve", b1T[:, :], ps[:, :])
        b1v = b1T.t[:, :].rearrange("p (e f) -> p e f", f=16)[:, :, 8:16]
        S.op("dve", lambda e: e.tensor_scalar(b1v, b1v, 1.0, None, ALU.add), reads=[b1T.key], writes=[b1T.key])
        for t in tiles:
            for half in range(2):
                ps = PS.bank()
                S.mm(ps[:, :], V(gT.t[0:32, t * 128:(t + 1) * 128], ("gT", t)),
                     V(b2s.t[0:32, half * 512:(half + 1) * 512], b2s.key))
                S.tt("dve", A(t, half), ps[:, :], A(t, half), ALU.add)
        stg = [sc.buf("stg%d" % i, [128, 8, 512]) for i in range(2)]
        NW1 = 4
        w1b = [sc.buf("w1b%d" % i, [128, 8, 512], BF16) for i in range(NW1)]
        w2b = [sc.buf("w2b%d" % i, [128, 8, 1024], BF16) for i in range(2)]
        actb = sc.buf("actb", [128, 8, NTOK], BF16)
        xg = [sc.buf("xg%d" % i, [128, 512]) for i in range(2)]
        sgm = [sc.buf("sgm%d" % i, [128, 512]) for i in range(2)]
        xl = [sc.buf("xl%d" % i, [128, 512]) for i in range(2)]
        groups = [(0, 128), (128, 640), (640, 1152)] if l == 0 else [(128, 640), (640, 1152)]
        si = 0; wi = 0; ci = 0
        for e_ in range(n_exp):
            w2_ = w2b[e_ % 2]
            for hh in range(2):
                st_ = stg[si % 2]; si += 1
                st4 = st_.t[:, :, :].rearrange("p a f -> p (a f)").rearrange("p (k d) -> p k d", k=4)
                ld(V(st4, st_.key), ew2_d[l, e_, hh * 512:(hh + 1) * 512, :].rearrange("(k p) d -> p k d", p=128))
                for k in range(4):
                    for a in range(2):
                        S.tt("pool", V(w2_.t[:, hh * 4 + k, a * 512:(a + 1) * 512], w2_.key),
                             V(st4[:, k, a * 512:(a + 1) * 512], st_.key), V(gt2p.t[:, a * 512:(a + 1) * 512], gt2p.key), ALU.mult)
            for hf in range(2):
                wgl = []
                for part in range(2):
                    st_ = stg[si % 2]; si += 1
                    w1_ = w1b[wi % NW1]; wi += 1
                    c0 = part * 1024 + hf * 512
                    ld(V(st_.t[:, :, :], st_.key), ew1_d[l, e_, :, c0:c0 + 512].rearrange("(k p) f -> p k f", p=128))
                    S.copy("pool", V(w1_.t[:, :, :], w1_.key), V(st_.t[:, :, :], st_.key))
                    wgl.append(w1_)
                wg_, wl_ = wgl
                for (g0, g1) in groups:
                    n = g1 - g0
                    for ii in range(4):
                        i = 4 * hf + ii
                        psg = PS.bank(); psl = PS.bank()
                        for kc in range(8):
                            S.mm(psg[:, 0:n], V(wg_.t[:, kc, ii * 128:(ii + 1) * 128], wg_.key),
                                 V(hT.t[:, kc, g0:g1], ("hT", "all")), start=(kc == 0), stop=(kc == 7))
                        for kc in range(8):
                            S.mm(psl[:, 0:n], V(wl_.t[:, kc, ii * 128:(ii + 1) * 128], wl_.key),
                                 V(hT.t[:, kc, g0:g1], ("hT", "all")), start=(kc == 0), stop=(kc == 7))
                        x_, s_, l_ = xg[ci % 2], sgm[ci % 2], xl[ci % 2]; ci += 1
                        bg = V(b1T.t[:, e_ * 16 + i:e_ * 16 + i + 1], b1T.key)
                        bl = V(b1T.t[:, e_ * 16 + 8 + i:e_ * 16 + 8 + i + 1], b1T.key)
                        S.ts("dve", V(x_.t[:, 0:n], x_.key), psg[:, 0:n], bg, LIMIT, ALU.add, ALU.min)
                        S.act(V(s_.t[:, 0:n], s_.key), V(x_.t[:, 0:n], x_.key), AF.Sigmoid, scale=SW_ALPHA)
                        S.ts("dve", V(l_.t[:, 0:n], l_.key), psl[:, 0:n], bl, 1.0 - LIMIT, ALU.add, ALU.max)
                        S.tt("pool", V(x_.t[:, 0:n], x_.key), V(x_.t[:, 0:n], x_.key), V(s_.t[:, 0:n], s_.key), ALU.mult)
                        S.stt("dve", V(actb.t[:, i, g0:g1], (actb.key, i)), V(l_.t[:, 0:n], l_.key), 1.0 + LIMIT,
                              V(x_.t[:, 0:n], x_.key), ALU.min, ALU.mult)
            for t in tiles:
                for half in range(2):
                    ps = PS.bank()
                    for fc in range(8):
                        S.mm(ps[:, :], V(actb.t[:, fc, t * 128:(t + 1) * 128], (actb.key, fc)),
                             V(w2_.t[:, fc, half * 512:(half + 1) * 512], w2_.key), start=(fc == 0), stop=(fc == 7))
                    S.stt("dve", A(t, half), ps[:, :], V(gates.t[:, t, e_:e_ + 1], ("gates", t)), A(t, half),
                          ALU.mult, ALU.add)
        sc.close(S)

        sc = Scope(nc)
        g2 = bc_tile(sc, "g2", lnp_d[l, 2]); b2 = bc_tile(sc, "b2", lnp_d[l, 3])
        if l == 0:
            G3 = bc_tile(sc, "G3", modbc_d[1, 1]); B3 = bc_tile(sc, "B3", modbc_d[1, 0])
            tmpb = sc.buf("tmpb", [128, 1024])
            S.tt("dve", tmpb[:, :], b2[:, :], G3[:, :], ALU.mult)
            S.tt("dve", B3[:, :], B3[:, :], tmpb[:, :], ALU.add)
            S.tt("dve", G3[:, :], G3[:, :], g2[:, :], ALU.mult)
            S.ts("dve", g2[:, :], g2[:, :], ALPHA, None, ALU.mult)
            S.ts("dve", b2[:, :], b2[:, :], ALPHA, None, ALU.mult)
            h2 = [sc.buf("h2%d" % i, [128, 1024]) for i in range(2)]
            for t in tiles:
                ln_tile(t)
                h_ = h2[t % 2]
                S.tt("dve", h_[:, :], A(t), G3[:, :], ALU.mult)
                S.tt("pool", h_[:, :], h_[:, :], B3[:, :], ALU.add)
                S.tt("dve", A(t), A(t), g2[:, :], ALU.mult)
                S.tt("pool", A(t), A(t), b2[:, :], ALU.add)
                transpose_tile(h_, t)
                if 1 not in layers and t >= 1:
                    S.store(out_d[p, (t - 1) * 128:t * 128, :], A(t))
        else:
            for t in tiles:
                ln_tile(t)
                S.tt("dve", A(t), A(t), g2[:, :], ALU.mult)
                S.tt("pool", A(t), A(t), b2[:, :], ALU.add)
                S.store(out_d[p, (t - 1) * 128:t * 128, :], A(t))
        sc.close(S)

    for p in range(npass):
        if 0 in layers:
            mix0(p)
            post(0, p)
        if 1 in layers:
            mix1(p)
            post(1, p)
    S.barrier()
    S.flush(final=True)
    return nc


def prep_phaseB(inp, og_b, b, s, shared=None):
    f = np.float32
    xh = np.zeros((2, NTOK, D), f)
    ogh = np.zeros((2, NTOK, 2048), f)
    flag = np.ones((128, 2), f)
    for p in range(2):
        t_lo = s * 2048 + p * 1024 - 128
        if t_lo < 0:
            flag[:, p] = 0.0
            xh[p, 128:] = inp["x"][b, 0:1024]
            ogh[p, 128:] = og_b[0:1024]
        else:
            xh[p] = inp["x"][b, t_lo:t_lo + NTOK]
            ogh[p] = og_b[t_lo:t_lo + NTOK]
    m = {"xh": xh, "ogh": ogh, "flag": flag,
         "cb": np.ascontiguousarray(inp["c"][b].reshape(8, 128).T)}
    if shared is None:
        shared = prep_phaseB_shared(inp)
    m.update(shared)
    return m


def prep_phaseB_shared(inp):
    f = np.float32
    bc = lambda v: np.ascontiguousarray(np.broadcast_to(v[None, :], (128, v.shape[0])))
    pp = lambda v, n: np.ascontiguousarray(v.reshape(n, 128).T)
    lnp = np.stack([np.stack([bc(inp[k][l]) for k in ("ln1_g", "ln1_b", "ln2_g", "ln2_b")]) for l in range(2)])
    return {
        "adaw": np.stack([pkc(inp["ada_w"][l]) for l in range(2)]),
        "adab": np.ascontiguousarray(inp["ada_b"][:, None, :]),
        "outw": pkc(inp["dn_out_w"][0]),
        "pw1w": pkc(inp["cf_pw1_w"][0]),
        "pw1b": pp(inp["cf_pw1_b"][0], 16),
        "dww": np.ascontiguousarray(inp["cf_dw_w"][0].reshape(31, 8, 128).transpose(2, 1, 0)),
        "dwb": pp(inp["cf_dw_b"][0], 8),
        "cfg": pp(inp["cf_ln_g"][0], 8),
        "cfb": pp(inp["cf_ln_b"][0], 8),
        "pw2w": pkc(inp["cf_pw2_w"][0]),
        "pw2b": np.ascontiguousarray(inp["cf_pw2_b"][0][None, :]),
        "lnp": np.ascontiguousarray(lnp.astype(f)),
        "rw": np.stack([pkc(inp["router_w"][l]) for l in range(2)]),
        "rb": np.ascontiguousarray(inp["router_b"][:, None, :]),
        "ew1": np.ascontiguousarray(inp["e_w1"]),
        "eb1": np.ascontiguousarray(inp["e_b1"].reshape(2, 4, 128, 128)),
        "ew2": np.ascontiguousarray(inp["e_w2"]),
        "eb2": np.ascontiguousarray(inp["e_b2"]),
        "cst": consts_np(),
    }


def build_fused():
    nc = bass.Bass("TRN2", target_bir_lowering=False)
    dt = nc.dram_tensor
    I = lambda name, shape: dt(name, shape, F32, kind="ExternalInput").ap()
    fz = declare_B(nc, with_og=False)
    fz["nc"] = nc
    fz["x"] = I("x", [SEQ, D])
    win4 = I("win", [4, 128, 8, 1544])
    cw4 = I("cw", [4, 128, 8, 4])
    alog4 = I("alog", [4, 128, 4])
    dtb4 = I("dtb", [4, 128, 4])
    fz["onw"] = I("onw", [128, 512])
    fz["sel"] = I("sel", [128, 4])
    fz["ogg"] = dt("ogg", [4 * OGROWS, 512], F32).ap()
    S = Sched(nc)
    fz["S"] = S
    fz["PS"] = PsumPool(nc, 4, "p")
    fz["PS"].register(S)
    for hg in range(4):
        fh = dict(fz)
        fh["win"], fh["cw"], fh["alog"], fh["dtb"] = win4[hg], cw4[hg], alog4[hg], dtb4[hg]
        fh["ogpad"] = fz["ogg"][hg * OGROWS:(hg + 1) * OGROWS, :]
        build_phaseA(SEQ, fh)
    build_phaseB(fz=fz)
    return nc


def kernel(**inp):
    inp = {k: np.asarray(v) for k, v in inp.items()}
    nc = build_fused()
    shared = prep_phaseB_shared(inp)
    dummy_og = np.zeros((SEQ, 2048), np.float32)
    mA = {b: [prep_phaseA(inp, b, hg, SEQ) for hg in range(4)] for b in range(2)}
    stk = {b: {k: np.ascontiguousarray(np.stack([mA[b][hg][k] for hg in range(4)])) for k in ("win", "cw", "alog", "dtb")}
           for b in range(2)}
    maps = []
    for i in range(8):
        b, r = i // 4, i % 4
        mB = prep_phaseB(inp, dummy_og, b, r, shared)
        m = {k: v for k, v in mB.items() if k != "ogh"}
        m["x"] = mA[b][0]["x"]
        m["onw"] = mA[b][0]["onw"]
        m.update(stk[b])
        sel = np.zeros((128, 4), np.float32)
        sel[:, r] = 1.0
        m["sel"] = sel
        maps.append(m)
    res = run_bass_kernel_spmd(nc, maps, core_ids=list(range(8)))
    out = np.zeros((2, SEQ, D), np.float32)
    for i in range(8):
        b, s_ = i // 4, i % 4
        o = res.results[i]["out"]
        out[b, s_ * 2048:s_ * 2048 + 1024] = o[0]
        out[b, s_ * 2048 + 1024:s_ * 2048 + 2048] = o[1]
    return out
```

```python
import contextlib
import numpy as np
import concourse.bass as bass
import concourse.mybir as mybir
from concourse.bass_utils import run_bass_kernel_spmd

F32 = mybir.dt.float32
BF16 = mybir.dt.bfloat16
ALU = mybir.AluOpType
AF = mybir.ActivationFunctionType

ENGS = ("pe", "dve", "act", "pool", "sp")
N_DMA_SEMS = 12

D = 1024
SEQ = 8192
DEPTH = 2
NEXP = 32
ALPHA = (2 * DEPTH) ** 0.25
LN_EPS = 1e-5
RMS_EPS = 1e-6
L2_EPS = 1e-6
LIMIT = 7.0
SW_ALPHA = 1.702


class V:
    def __init__(self, ap, key):
        self.ap, self.key = ap, key


class Buf:
    def __init__(self, nc, name, shape, dtype, psum=False):
        if psum:
            self.t = nc.alloc_psum_tensor(name, shape, dtype)
        else:
            self.t = nc.alloc_sbuf_tensor("sb_" + name, shape, dtype)
        self.key = name

    def __getitem__(self, idx):
        return V(self.t[idx], self.key)

    def v(self, idx, sub):
        return V(self.t[idx], (self.key, sub))


class Sched:
    def __init__(self, nc):
        self.nc = nc
        self.ops = []
        self.flushed = 0
        self.cnt = {e: 0 for e in ENGS}
        self.last_w = {}
        self.readers = {}
        self.dma_tot = [0] * (N_DMA_SEMS * 3)
        self.dma_last = [None] * (N_DMA_SEMS * 3)
        self.dma_rr = {"sp": 0, "act": 0, "pool": 0}
        self.opinfo = []
        self.esem = {e: nc.alloc_semaphore("s_" + e) for e in ENGS}
        self.dsem = [nc.alloc_semaphore("d%d" % i) for i in range(N_DMA_SEMS * 3)]
        self.known = {e: {} for e in ENGS}
        self.last_on = {e: None for e in ENGS}
        import os
        self.maxops = int(os.environ.get("KSTOP", "0")) or None
        self.marks = {}
        self.rar_keys = set()

    def mark(self, label):
        self.marks.setdefault(label, len(self.ops))

    def _deps(self, reads, writes):
        d = set()
        for k in reads:
            w = self.last_w.get(k)
            if w is not None:
                d.add(w)
            if k in self.rar_keys:
                for r in self.readers.get(k, ()):
                    d.add(r)
        for k in writes:
            w = self.last_w.get(k)
            if w is not None:
                d.add(w)
            for r in self.readers.get(k, ()):
                d.add(r)
        return d

    def _commit(self, oid, reads, writes):
        for k in reads:
            self.readers.setdefault(k, []).append(oid)
        for k in writes:
            self.last_w[k] = oid
            self.readers[k] = []

    def op(self, eng, fn, reads=(), writes=()):
        if self.maxops is not None and len(self.ops) >= self.maxops:
            return -1
        reads = tuple(reads)
        writes = tuple(writes)
        deps = self._deps(reads, writes)
        oid = len(self.ops)
        self.cnt[eng] += 1
        self.opinfo.append(("c", eng, self.cnt[eng]))
        self.ops.append((eng, fn, deps, None))
        self._commit(oid, reads, writes)
        self.last_on[eng] = oid
        return oid

    def dma(self, fn, reads=(), writes=(), q="sp"):
        if self.maxops is not None and len(self.ops) >= self.maxops:
            return -1
        reads = tuple(reads)
        writes = tuple(writes)
        deps = self._deps(reads, writes)
        base = {"sp": 0, "act": N_DMA_SEMS, "pool": 2 * N_DMA_SEMS}[q]
        slot = base + self.dma_rr[q]
        self.dma_rr[q] = (self.dma_rr[q] + 1) % N_DMA_SEMS
        if self.dma_last[slot] is not None:
            deps.add(self.dma_last[slot])
        oid = len(self.ops)
        self.dma_tot[slot] += 16
        self.opinfo.append(("d", slot, self.dma_tot[slot]))
        self.dma_last[slot] = oid
        self.ops.append((q, fn, deps, slot))
        self._commit(oid, reads, writes)
        return oid

    def barrier(self):
        if self.maxops is not None and len(self.ops) >= self.maxops:
            return
        deps = set()
        for e in ENGS:
            if self.last_on[e] is not None:
                deps.add(self.last_on[e])
        for o in self.dma_last:
            if o is not None:
                deps.add(o)
        for e in ENGS:
            self.ops.append((e, None, set(deps), "bar"))
            self.opinfo.append(("n", None, 0))
        self.last_w = {}
        self.readers = {}

    def flush(self, final=False):
        nc = self.nc
        lo, hi = self.flushed, len(self.ops)
        self.flushed = hi
        per_eng = {e: [] for e in ENGS}
        for oid in range(lo, hi):
            per_eng[self.ops[oid][0]].append(oid)
        final_deps = []
        if final:
            for o in self.dma_last:
                if o is not None:
                    final_deps.append(o)

        def run(engname, engobj):
            known = self.known[engname]

            def wait_for(dlist, is_pe_compute):
                need = {}
                for d in dlist:
                    kind, a, v = self.opinfo[d]
                    if kind == "n":
                        continue
                    if kind == "c":
                        if a == "pe" and is_pe_compute:
                            continue
                        key = ("c", a)
                    else:
                        key = ("d", a)
                    if v > need.get(key, 0):
                        need[key] = v
                for key, v in need.items():
                    if known.get(key, 0) >= v:
                        continue
                    known[key] = v
                    sem = self.esem[key[1]] if key[0] == "c" else self.dsem[key[1]]
                    engobj.wait_ge(sem, v)

            for oid in per_eng[engname]:
                eng, fn, deps, slot = self.ops[oid]
                if slot == "bar":
                    wait_for(deps, False)
                    continue
                wait_for(deps, engname == "pe" and slot is None)
                ins = fn(engobj)
                if slot is None:
                    ins.then_inc(self.esem[engname], 1)
                else:
                    ins.then_inc(self.dsem[slot], 16)
            if final and engname == "sp":
                wait_for(final_deps, False)

        with nc.Block() as block:
            block.tensor(lambda e: run("pe", e))
            block.vector(lambda e: run("dve", e))
            block.scalar(lambda e: run("act", e))
            block.gpsimd(lambda e: run("pool", e))
            block.sync(lambda e: run("sp", e))

    @staticmethod
    def _k(*vs):
        return [v.key for v in vs if isinstance(v, V)]

    @staticmethod
    def _a(x):
        return x.ap if isinstance(x, V) else x

    def mm(self, out, lhsT, rhs, start=True, stop=True):
        return self.op("pe", lambda e: e.matmul(out.ap, lhsT.ap, rhs.ap, start=start, stop=stop),
                       reads=self._k(lhsT, rhs), writes=[out.key])

    def tr(self, out, in_, ident):
        return self.op("pe", lambda e: e.transpose(out.ap, in_.ap, ident.ap),
                       reads=self._k(in_, ident), writes=[out.key])

    def act(self, out, in_, func, bias=0.0, scale=1.0, accum=None, eng="act"):
        a = self._a
        kw = {}
        if accum is not None:
            kw["accum_out"] = accum.ap
        return self.op(eng, lambda e: e.activation(out.ap, in_.ap, func, bias=a(bias), scale=a(scale), **kw),
                       reads=self._k(in_, bias, scale), writes=self._k(out, accum))

    def ts(self, eng, out, in0, s1, s2, op0, op1=None):
        a = self._a
        if op1 is None:
            f = lambda e: e.tensor_scalar(out.ap, in0.ap, a(s1), None, op0)
        else:
            f = lambda e: e.tensor_scalar(out.ap, in0.ap, a(s1), a(s2), op0, op1)
        return self.op(eng, f, reads=self._k(in0, s1, s2), writes=[out.key])

    def tt(self, eng, out, in0, in1, op):
        return self.op(eng, lambda e: e.tensor_tensor(out.ap, in0.ap, in1.ap, op),
                       reads=self._k(in0, in1), writes=[out.key])

    def stt(self, eng, out, in0, s, in1, op0, op1):
        a = self._a
        eng = "dve"
        return self.op(eng, lambda e: e.scalar_tensor_tensor(out.ap, in0.ap, a(s), in1.ap, op0, op1),
                       reads=self._k(in0, s, in1), writes=[out.key])

    def copy(self, eng, out, in_):
        if eng == "act":
            return self.act(out, in_, AF.Copy)
        return self.op(eng, lambda e: e.tensor_copy(out.ap, in_.ap), reads=[in_.key], writes=[out.key])

    def memset(self, eng, out, val):
        return self.op(eng, lambda e: e.memset(out.ap, val), writes=[out.key])

    def load(self, out, src_ap, q="sp"):
        return self.dma(lambda e: e.dma_start(out=out.ap, in_=src_ap), writes=[out.key], q=q)

    def store(self, dst_ap, in_, q="sp", dkey=None):
        w = [dkey] if dkey is not None else []
        return self.dma(lambda e: e.dma_start(out=dst_ap, in_=in_.ap), reads=[in_.key], writes=w, q=q)


class PsumPool:
    def __init__(self, nc, n_big, prefix):
        self.banks = [Buf(nc, "%s_ps%d" % (prefix, b), [128, 512], F32, psum=True) for b in range(8)]
        self.configure(n_big)

    def register(self, S):
        S.rar_keys.update(b.key for b in self.banks)

    def configure(self, n_big):
        self.big = self.banks[:n_big]
        self.small = [(buf, q) for buf in self.banks[n_big:] for q in range(4)]
        self.bi = 0
        self.si = 0

    def bank(self):
        b = self.big[self.bi]
        self.bi = (self.bi + 1) % len(self.big)
        return b

    def quarter(self, w=128):
        nb = len(self.small) // 4
        i = self.si
        self.si = (self.si + 1) % len(self.small)
        buf, _ = self.small[(i % nb) * 4]
        q = (i // nb) % 4
        return V(buf.t[:, q * 128:q * 128 + w], buf.key)


def ada_broadcast(S, nc, PS, condB, ones_row, adaw_ap, adab_ap, col0, ncols, stage, outs, ident_unused=None):
    npieces = ncols // 512
    for pi in range(npieces):
        c0 = col0 + pi * 512
        st = stage
        S.load(V(st.t[:, :, :], st.key), adaw_ap[:, :, c0:c0 + 512])
        brow = outs["brow"]
        S.load(V(brow.t[0:1, 0:512], brow.key), adab_ap[0:1, c0:c0 + 512])
        ps = PS.bank()
        for kc in range(8):
            S.mm(ps[:, :], V(condB.t[:, kc, :], condB.key), V(st.t[:, kc, :], st.key), start=(kc == 0), stop=False)
        S.mm(ps[:, :], V(ones_row.t[0:1, :], ones_row.key), V(brow.t[0:1, 0:512], brow.key), start=False, stop=True)
        dst, add_one = outs["dst"][pi]
        if add_one:
            S.ts("dve", dst, ps[:, :], 1.0, None, ALU.add)
        else:
            S.copy("dve", dst, ps[:, :])


def build_phaseA(n_tok=SEQ, fz=None):
    if fz is None:
        nc = bass.Bass("TRN2", target_bir_lowering=False)
        dt = nc.dram_tensor
        x_d = dt("x", [n_tok, D], F32, kind="ExternalInput").ap()
        cb_d = dt("cb", [128, 8], F32, kind="ExternalInput").ap()
        adaw_d = dt("adaw", [128, 8, 2048], F32, kind="ExternalInput").ap()
        adab_d = dt("adab", [1, 2048], F32, kind="ExternalInput").ap()
        win_d = dt("win", [128, 8, 1544], F32, kind="ExternalInput").ap()
        cw_d = dt("cw", [128, 8, 4], F32, kind="ExternalInput").ap()
        alog_d = dt("alog", [128, 4], F32, kind="ExternalInput").ap()
        dtb_d = dt("dtb", [128, 4], F32, kind="ExternalInput").ap()
        onw_d = dt("onw", [128, 512], F32, kind="ExternalInput").ap()
        cst_d = dt("cst", [128, 4, 128], F32, kind="ExternalInput").ap()
        og_d = dt("og", [n_tok, 512], F32, kind="ExternalOutput").ap()
        S = Sched(nc)
        PS = PsumPool(nc, 2, "a")
        PS.register(S)
    else:
        nc, S, PS = fz["nc"], fz["S"], fz["PS"]
        PS.configure(2)
        x_d, cb_d, cst_d = fz["x"], fz["cb"], fz["cst"]
        adaw_d, adab_d = fz["adaw"][0], fz["adab"][0]
        win_d, cw_d, alog_d, dtb_d, onw_d = fz["win"], fz["cw"], fz["alog"], fz["dtb"], fz["onw"]
        og_d = fz["ogpad"][128:128 + n_tok, :]
    scA = Scope(nc)
    scA.plain = True
    B = lambda name, shape, dtp=F32: scA.buf(name, shape, dtp)

    cst = B("cst", [128, 4, 128])
    ident = V(cst.t[:, 0, :], "cst")
    ones = V(cst.t[:, 1, :], "cst")
    U = V(cst.t[:, 2, :], "cst")
    LS = V(cst.t[:, 3, :], "cst")
    S.load(V(cst.t[:, :, :], "cst"), cst_d)
    win = B("win", [128, 8, 1544], BF16)
    for kc in range(8):
        S.load(V(win.t[:, kc, :], "win"), win_d[:, kc, :], q="pool")
    cw = B("cw", [128, 8, 4])
    S.load(V(cw.t[:, :, :], "cw"), cw_d)
    alog = B("alog", [128, 4]); dtb = B("dtb", [128, 4]); onw = B("onw", [128, 512])
    S.load(alog[:, :], alog_d); S.load(dtb[:, :], dtb_d); S.load(onw[:, :], onw_d)
    negA = B("negA", [128, 4])
    S.act(negA[:, :], alog[:, :], AF.Exp)
    S.ts("dve", negA[:, :], negA[:, :], -1.0, None, ALU.mult)

    cbt = B("cbt", [128, 8])
    S.load(cbt[:, :], cb_d)
    cond = B("cond", [128, 8])
    S.act(cond[:, :], cbt[:, :], AF.Silu)
    condB = B("condB", [128, 8, 128])
    for kc in range(8):
        S.ts("dve", V(condB.t[:, kc, :], "condB"), ones, V(cond.t[:, kc:kc + 1], "cond"), None, ALU.mult)
    h4 = B("h4", [128, 4, 1024])
    sh_bc = B("sh_bc", [128, 1024]); scp_bc = B("scp_bc", [128, 1024])
    brow = B("brow", [1, 512])
    stg = B("adastage", [128, 8, 512])
    outs = {"brow": brow, "dst": [(V(sh_bc.t[:, 0:512], "sh_bc"), False), (V(sh_bc.t[:, 512:1024], "sh_bc"), False),
                                  (V(scp_bc.t[:, 0:512], "scp_bc"), True), (V(scp_bc.t[:, 512:1024], "scp_bc"), True)]}
    ada_broadcast(S, nc, PS, condB, Buf_view(cst, 1), adaw_d, adab_d, 0, 2048, stg, outs)

    xt = [B("xt%d" % i, [128, 1024]) for i in range(2)]
    hT = B("hT", [128, 8, 512], BF16)
    pre = B("pre", [128, 8, 515])
    S.memset("dve", V(pre.t[:, :, :], ("pre", "all")), 0.0)
    cv = [B("cv%d" % i, [128, 512]) for i in range(2)]
    qkvs = B("qkvs", [128, 8, 512])
    rn = B("rn", [128, 512])
    vtok = B("vtok", [128, 512]); ktok = B("ktok", [128, 256]); gz = B("gz", [128, 512])
    og = [B("og%d" % i, [128, 512]) for i in range(2)]
    sm = B("sm", [128, 64])
    Sst = [[B("S%d_%d" % (h, i), [128, 128]) for i in range(2)] for h in range(4)]
    for h in range(4):
        S.memset("pool", Sst[h][0][:, :], 0.0)
    W = {}
    for h in range(4):
        for nm in ("rhsU", "Gs", "e1", "DLs", "e2", "DUi", "P0", "N0", "P1", "N1", "R", "vb", "kbg", "negwT",
                   "vnew", "eg", "qgT", "kd", "QKmT", "junk"):
            W[(h, nm)] = B("w%d_%s" % (h, nm), [128, 128])
        W[(h, "s")] = B("w%d_s" % h, [128, 8])
    S.barrier()

    nblk = n_tok // 512
    og_i = 0
    S.mark("setup_done")
    for blk in range(nblk):
        t0 = blk * 512
        for c in range(4):
            xb = xt[c % 2]
            S.load(xb[:, :], x_d[t0 + c * 128:t0 + (c + 1) * 128, :])
            hv = h4.v((slice(None), c, slice(None)), c)
            S.tt("dve", hv, xb[:, :], scp_bc[:, :], ALU.mult)
            S.tt("pool", hv, hv, sh_bc[:, :], ALU.add)
        S.mark("modulated")
        for kc in range(8):
            ps = PS.bank()
            for c in range(4):
                hv = h4.v((slice(None), c, slice(kc * 128, (kc + 1) * 128)), c)
                S.tr(ps[:, c * 128:(c + 1) * 128], hv, ident)
            S.copy("act" if kc % 2 else "dve", hT.v((slice(None), kc, slice(None)), kc), ps[:, :])
        S.mark("transposed")
        for fc in range(8):
            ps = PS.bank()
            for kc in range(8):
                S.mm(ps[:, :], V(win.t[:, kc, fc * 128:(fc + 1) * 128], "win"),
                     hT.v((slice(None), kc, slice(None)), kc), start=(kc == 0), stop=(kc == 7))
            pk = ("pre", fc) if blk > 0 or True else None
            S.copy("act", V(pre.t[:, fc, 3:515], ("pre", fc)), ps[:, :])
            c_ = cv[fc % 2]
            e2_ = "dve"
            S.ts(e2_, c_[:, :], V(pre.t[:, fc, 0:512], ("pre", fc)), V(cw.t[:, fc, 0:1], "cw"), None, ALU.mult)
            for j in range(1, 4):
                S.stt(e2_, c_[:, :], V(pre.t[:, fc, j:j + 512], ("pre", fc)), V(cw.t[:, fc, j:j + 1], "cw"),
                      c_[:, :], ALU.mult, ALU.add)
            S.act(qkvs.v((slice(None), fc, slice(None)), fc), c_[:, :], AF.Silu)
            S.copy("pool", V(pre.t[:, fc, 0:3], ("pre", fc)), V(pre.t[:, fc, 512:515], ("pre", fc)))
        S.mark("conv_done")
        for fc in range(4):
            qv = qkvs.v((slice(None), fc, slice(None)), fc)
            c_ = cv[fc % 2]
            S.act(c_[:, :], qv, AF.Square)
            ps = PS.bank()
            S.mm(ps[:, :], ones, c_[:, :])
            S.ts("dve", rn[:, :], ps[:, :], L2_EPS, None, ALU.add)
            S.act(rn[:, :], rn[:, :], AF.Sqrt)
            S.op("dve", lambda e: e.reciprocal(rn.t[:, :], rn.t[:, :]), reads=["rn"], writes=["rn"])
            if fc < 2:
                S.stt("dve", qv, qv, 128.0 ** -0.5, rn[:, :], ALU.mult, ALU.mult)
            else:
                S.tt("dve", qv, qv, rn[:, :], ALU.mult)

        S.mark("l2norm_done")
        for c in range(4):
            cs = slice(c * 128, (c + 1) * 128)
            ps = PS.bank()
            for h in range(4):
                S.tr(ps[:, h * 128:(h + 1) * 128], qkvs.v((slice(None), 4 + h, cs), 4 + h), ident)
            S.copy("act", vtok[:, :], ps[:, :])
            ps = PS.bank()
            for qh in range(2):
                S.tr(ps[:, qh * 128:(qh + 1) * 128], qkvs.v((slice(None), 2 + qh, cs), 2 + qh), ident)
            S.copy("dve", ktok[:, :], ps[:, 0:256])
            ps = PS.bank()
            for kc in range(8):
                S.mm(ps[:, :], hT.v((slice(None), kc, cs), kc), V(win.t[:, kc, 1024:1536], "win"),
                     start=(kc == 0), stop=(kc == 7))
            S.act(gz[:, :], ps[:, :], AF.Silu)
            S.tt("pool", gz[:, :], gz[:, :], onw[:, :], ALU.mult)
            pq = PS.quarter(8)
            for kc in range(8):
                S.mm(pq, hT.v((slice(None), kc, cs), kc), V(win.t[:, kc, 1536:1544], "win"),
                     start=(kc == 0), stop=(kc == 7))
            smv = lambda a, b: V(sm.t[:, a:b], "sm")
            S.copy("dve", smv(32, 40), pq)
            S.act(smv(0, 4), smv(32, 36), AF.Sigmoid)
            S.ts("dve", smv(4, 8), smv(0, 4), -1.0, None, ALU.mult)
            S.tt("dve", smv(8, 12), smv(36, 40), dtb[:, :], ALU.add)
            S.act(smv(8, 12), smv(8, 12), AF.Exp)
            S.act(smv(8, 12), smv(8, 12), AF.Ln, bias=1.0)
            S.tt("dve", smv(12, 16), smv(8, 12), negA[:, :], ALU.mult)
            pq2 = PS.quarter(4)
            S.mm(pq2, U, smv(12, 16))
            S.copy("dve", smv(16, 20), pq2)
            S.act(smv(20, 24), smv(16, 20), AF.Exp)
            S.tt("dve", smv(24, 28), smv(0, 4), smv(20, 24), ALU.mult)

            S.mark("chunkprep_done")
            KK = []
            QKT = []
            for qh in range(2):
                kT = qkvs.v((slice(None), 2 + qh, cs), 2 + qh)
                qT = qkvs.v((slice(None), qh, cs), qh)
                p1 = PS.quarter()
                S.mm(p1, kT, kT)
                p2 = PS.quarter()
                S.mm(p2, kT, qT)
                KK.append(p1)
                QKT.append(p2)
            w = lambda h, nm: W[(h, nm)][:, :]
            H4 = range(4)
            gcol = lambda h: V(sm.t[:, 16 + h:17 + h], "sm")
            sv = lambda h, a_, b_: V(W[(h, "s")].t[:, a_:b_], W[(h, "s")].key)
            glast = lambda h: V(W[(h, "Gs")].t[:, 127:128], W[(h, "Gs")].key)
            ktk = lambda h: V(ktok.t[:, (h // 2) * 128:(h // 2 + 1) * 128], "ktok")
            for h in H4:
                S.act(w(h, "rhsU"), U, AF.Copy, scale=V(sm.t[:, 12 + h:13 + h], "sm"))
            pg = {}
            for h in H4:
                pg[h] = PS.quarter()
                S.mm(pg[h], ones, w(h, "rhsU"))
            for h in H4:
                S.copy("act", w(h, "Gs"), pg[h])
            for h in H4:
                S.ts("dve", w(h, "e1"), w(h, "Gs"), gcol(h), 0.0, ALU.subtract, ALU.max)
                S.ts("dve", w(h, "e2"), w(h, "Gs"), gcol(h), 0.0, ALU.subtract, ALU.min)
            for h in H4:
                S.act(w(h, "e1"), w(h, "e1"), AF.Exp, scale=-1.0)
                S.act(w(h, "e2"), w(h, "e2"), AF.Exp)
                S.act(w(h, "eg"), w(h, "Gs"), AF.Exp)
                S.act(sv(h, 0, 1), gcol(h), AF.Exp, bias=glast(h), scale=-1.0)
                S.act(sv(h, 1, 2), glast(h), AF.Exp)
            for h in H4:
                S.tt("pool", w(h, "DLs"), w(h, "e1"), LS, ALU.mult)
                S.tt("pool", w(h, "DUi"), w(h, "e2"), U, ALU.mult)
                S.tt("pool", w(h, "qgT"), qkvs.v((slice(None), h // 2, cs), h // 2), w(h, "eg"), ALU.mult)
            for h in H4:
                S.stt("dve", w(h, "P0"), KK[h // 2], V(sm.t[:, 4 + h:5 + h], "sm"), w(h, "DLs"), ALU.mult, ALU.mult)
            pn = {}
            for h in H4:
                pn[h] = PS.quarter()
                S.tr(pn[h], w(h, "P0"), ident)
            for h in H4:
                S.copy("act", w(h, "N0"), pn[h])
            for h in H4:
                S.act(w(h, "vb"), V(vtok.t[:, h * 128:(h + 1) * 128], "vtok"), AF.Copy, scale=V(sm.t[:, h:h + 1], "sm"))
                S.act(w(h, "kbg"), ktk(h), AF.Copy, scale=V(sm.t[:, 24 + h:25 + h], "sm"))
                S.act(w(h, "kd"), ktk(h), AF.Copy, scale=sv(h, 0, 1))
            for h in H4:
                S.tt("dve", w(h, "R"), w(h, "N0"), ident, ALU.add)
                S.tt("dve", w(h, "QKmT"), QKT[h // 2], w(h, "DUi"), ALU.mult)
            S.mark("stage1_done")
            cur = {h: ("P0", "N0") for h in H4}
            for m in range(1, 7):
                nxt = {h: (("P1", "N1") if cur[h][0] == "P0" else ("P0", "N0")) for h in H4}
                pp, pnn, pr = {}, {}, {}
                for h in H4:
                    pp[h] = PS.quarter()
                    S.mm(pp[h], w(h, cur[h][1]), w(h, cur[h][0]))
                if m < 6:
                    PS.quarter(); PS.quarter()
                    for h in H4:
                        pnn[h] = PS.quarter()
                        S.mm(pnn[h], w(h, cur[h][0]), w(h, cur[h][1]))
                    PS.quarter(); PS.quarter()
                for h in H4:
                    S.copy("act", w(h, nxt[h][0]), pp[h])
                if m < 6:
                    for h in H4:
                        S.copy("dve", w(h, nxt[h][1]), pnn[h])
                for h in H4:
                    pr[h] = PS.quarter()
                    S.mm(pr[h], w(h, nxt[h][0]), w(h, "R"))
                for h in H4:
                    S.tt("dve", w(h, "R"), pr[h], w(h, "R"), ALU.add)
                cur = nxt
            S.mark("stage2_done")
            ogb = og[og_i % 2]
            og_i += 1
            Sc = {h: Sst[h][(blk * 4 + c) % 2] for h in H4}
            Sn = {h: Sst[h][(blk * 4 + c + 1) % 2] for h in H4}
            pw, pv, po, pu = {}, {}, {}, {}
            for h in H4:
                pw[h] = PS.quarter()
                S.mm(pw[h], w(h, "kbg"), w(h, "R"))
            for h in H4:
                S.act(w(h, "negwT"), pw[h], AF.Copy, scale=-1.0)
            for h in H4:
                pv[h] = PS.quarter()
                S.mm(pv[h], w(h, "R"), w(h, "vb"), start=True, stop=False)
                S.mm(pv[h], w(h, "negwT"), Sc[h][:, :], start=False, stop=True)
            for h in H4:
                S.copy("dve", w(h, "vnew"), pv[h])
            for h in H4:
                po[h] = PS.quarter()
                S.mm(po[h], w(h, "qgT"), Sc[h][:, :], start=True, stop=False)
                S.mm(po[h], w(h, "QKmT"), w(h, "vnew"), start=False, stop=True)
            for h in H4:
                pu[h] = PS.quarter()
                S.mm(pu[h], w(h, "kd"), w(h, "vnew"))
            for h in H4:
                S.stt("dve", Sn[h][:, :], Sc[h][:, :], sv(h, 1, 2), pu[h], ALU.mult, ALU.add)
            for h in H4:
                S.copy("dve", w(h, "e1"), po[h])
            for h in H4:
                S.act(w(h, "junk"), w(h, "e1"), AF.Square, accum=sv(h, 2, 3))
            for h in H4:
                S.ts("dve", sv(h, 3, 4), sv(h, 2, 3), 1.0 / 128.0, RMS_EPS, ALU.mult, ALU.add)
            for h in H4:
                S.act(sv(h, 3, 4), sv(h, 3, 4), AF.Sqrt)
            for h in H4:
                S.op("dve", lambda e, h=h: e.reciprocal(W[(h, "s")].t[:, 3:4], W[(h, "s")].t[:, 3:4]),
                     reads=[W[(h, "s")].key], writes=[W[(h, "s")].key])
            for h in H4:
                S.stt("dve", V(ogb.t[:, h * 128:(h + 1) * 128], ogb.key), w(h, "e1"), sv(h, 3, 4),
                      V(gz.t[:, h * 128:(h + 1) * 128], "gz"), ALU.mult, ALU.mult)
            S.store(og_d[t0 + c * 128:t0 + (c + 1) * 128, :], ogb[:, :])
            S.mark("chunk0_done")
    if fz is None:
        S.flush(final=True)
        return nc
    zt = V(W[(0, "junk")].t[:, :], W[(0, "junk")].key)
    S.memset("dve", zt, 0.0)
    for q4 in range(4):
        S.store(fz["ogpad"][0:128, q4 * 128:(q4 + 1) * 128], zt)
    scA.close(S)
    return nc


def Buf_view(buf, idx):
    b = Buf.__new__(Buf)
    b.t = buf.t[:, idx, :]
    b.key = buf.key
    return b


def consts_np():
    i = np.arange(128)
    ident = np.eye(128, dtype=np.float32)
    ones = np.ones((128, 128), np.float32)
    U = (i[:, None] <= i[None, :]).astype(np.float32)
    LS = (i[:, None] > i[None, :]).astype(np.float32)
    return np.ascontiguousarray(np.stack([ident, ones, U, LS], axis=1))


def pkc(w):
    K, F = w.shape
    return np.ascontiguousarray(w.reshape(K // 128, 128, F).transpose(1, 0, 2))


def prep_phaseA(inp, b, hg, n_tok=SEQ):
    f = np.float32
    in_w = inp["dn_in_w"][0]
    cols = np.concatenate([np.arange(256 * hg, 256 * hg + 256), 1024 + np.arange(256 * hg, 256 * hg + 256),
                           2048 + np.arange(512 * hg, 512 * hg + 512), 4096 + np.arange(512 * hg, 512 * hg + 512),
                           6144 + np.arange(4 * hg, 4 * hg + 4), 6160 + np.arange(4 * hg, 4 * hg + 4)])
    ccols = cols[:1024]
    cw = inp["dn_conv_w"][0][:, ccols]
    cw = np.ascontiguousarray(cw.reshape(4, 8, 128).transpose(2, 1, 0))
    hs = slice(4 * hg, 4 * hg + 4)
    return {
        "x": np.ascontiguousarray(inp["x"][b, :n_tok]).astype(f),
        "cb": np.ascontiguousarray(inp["c"][b].reshape(8, 128).T),
        "adaw": pkc(inp["ada_w"][0][:, :2048]),
        "adab": np.ascontiguousarray(inp["ada_b"][0][None, :2048]),
        "win": pkc(in_w[:, cols]),
        "cw": cw,
        "alog": np.ascontiguousarray(np.broadcast_to(inp["dn_A_log"][0][hs][None, :], (128, 4))),
        "dtb": np.ascontiguousarray(np.broadcast_to(inp["dn_dt_bias"][0][hs][None, :], (128, 4))),
        "onw": np.ascontiguousarray(np.broadcast_to(np.tile(inp["dn_onorm_w"][0], 4)[None, :], (128, 512))),
        "cst": consts_np(),
    }


class Scope:
    _n = 0

    def __init__(self, nc):
        self.nc = nc
        self.st = contextlib.ExitStack()
        Scope._n += 1
        self.sfx = "_s%d" % Scope._n
        self.plain = False

    def buf(self, name, shape, dtp=F32):
        b = Buf.__new__(Buf)
        b.t = self.st.enter_context(self.nc.sbuf_tensor("sb_" + name + self.sfx, shape, dtp))
        b.key = name if self.plain else name + self.sfx
        return b

    def close(self, S):
        S.barrier()
        S.flush()
        self.st.close()


NTILE = 9
NTOK = NTILE * 128
OGROWS = SEQ + 128


def declare_B(nc, with_og=True):
    dt = nc.dram_tensor
    I = lambda name, shape: dt(name, shape, F32, kind="ExternalInput").ap()
    d = {}
    d["xh"] = I("xh", [2, NTOK, D])
    if with_og:
        d["ogh"] = I("ogh", [2, NTOK, 2048])
    d["flag"] = I("flag", [128, 2])
    d["cb"] = I("cb", [128, 8])
    d["adaw"] = I("adaw", [2, 128, 8, 6144])
    d["adab"] = I("adab", [2, 1, 6144])
    d["outw"] = I("outw", [128, 16, 1024])
    d["pw1w"] = I("pw1w", [128, 8, 2048])
    d["pw1b"] = I("pw1b", [128, 16])
    d["dww"] = I("dww", [128, 8, 31])
    d["dwb"] = I("dwb", [128, 8])
    d["cfg"] = I("cfg", [128, 8])
    d["cfb"] = I("cfb", [128, 8])
    d["pw2w"] = I("pw2w", [128, 8, 1024])
    d["pw2b"] = I("pw2b", [1, 1024])
    d["lnp"] = I("lnp", [2, 4, 128, 1024])
    d["rw"] = I("rw", [2, 128, 8, 32])
    d["rb"] = I("rb", [2, 1, 32])
    d["ew1"] = I("ew1", [2, NEXP, 1024, 2048])
    d["eb1"] = I("eb1", [2, 4, 128, 128])
    d["ew2"] = I("ew2", [2, NEXP, 1024, 1024])
    d["eb2"] = I("eb2", [2, NEXP, 1024])
    d["cst"] = I("cst", [128, 4, 128])
    d["out"] = dt("out", [2, 1024, D], F32, kind="ExternalOutput").ap()
    d["modbc"] = dt("modbc", [2, 6, 128, 1024], F32).ap()
    return d


def build_phaseB(n_exp=NEXP, layers=(0, 1), npass=2, fz=None):
    if fz is None:
        nc = bass.Bass("TRN2", target_bir_lowering=False)
        d = declare_B(nc)
    else:
        nc = fz["nc"]
        d = fz
    xh_d, flag_d, cb_d, adaw_d, adab_d = d["xh"], d["flag"], d["cb"], d["adaw"], d["adab"]
    og_d = d.get("ogh")
    outw_d, pw1w_d, pw1b_d, dww_d, dwb_d, cfg_d, cfb_d = (d[k] for k in ("outw", "pw1w", "pw1b", "dww", "dwb", "cfg", "cfb"))
    pw2w_d, pw2b_d, lnp_d, rw_d, rb_d = (d[k] for k in ("pw2w", "pw2b", "lnp", "rw", "rb"))
    ew1_d, eb1_d, ew2_d, eb2_d, cst_d, out_d, modbc_d = (d[k] for k in ("ew1", "eb1", "ew2", "eb2", "cst", "out", "modbc"))

    if fz is None:
        S = Sched(nc)
        PS = PsumPool(nc, 6, "b")
        PS.register(S)
    else:
        S, PS = fz["S"], fz["PS"]
        PS.configure(6)
    B = lambda name, shape, dtp=F32: Buf(nc, "b_" + name, shape, dtp)
    cst = B("cst", [128, 4, 128])
    cst.key = "cst"
    ident = V(cst.t[:, 0, :], "cst")
    ones = V(cst.t[:, 1, :], "cst")
    ones_row = V(cst.t[0:1, 1, :], "cst")
    S.load(V(cst.t[:, :, :], "cst"), cst_d)
    flag = B("flag", [128, 2]); flag.key = "flag"
    S.load(flag[:, :], flag_d)
    acc = B("acc", [128, NTILE, 1024]); acc.key = "acc"
    hT = B("hT", [128, 8, NTOK], BF16); hT.key = "hT"
    gates = B("gates", [128, NTILE, 32]); gates.key = "gates"
    gT = B("gT", [32, NTOK]); gT.key = "gT"
    lns = B("lns", [128, NTILE, 16]); lns.key = "lns"
    if fz is not None:
        sel = B("sel", [128, 4]); sel.key = "sel"
        S.load(sel[:, :], fz["sel"])

    A = lambda t, half=None: (acc.v((slice(None), t, slice(None)), t) if half is None else
                              acc.v((slice(None), t, slice(half * 512, half * 512 + 512)), t))

    def ld(out, src, rkey=None, q="sp"):
        r = [rkey] if rkey is not None else []
        return S.dma(lambda e: e.dma_start(out=out.ap, in_=src), reads=r, writes=[out.key], q=q)

    sc = Scope(nc)
    cbt = sc.buf("cbt", [128, 8]); cond = sc.buf("cond", [128, 8]); condB = sc.buf("condB", [128, 8, 128])
    ld(cbt[:, :], cb_d)
    S.act(cond[:, :], cbt[:, :], AF.Silu)
    for kc in range(8):
        S.ts("dve", V(condB.t[:, kc, :], condB.key), ones, V(cond.t[:, kc:kc + 1], cond.key), None, ALU.mult)
    stg = [sc.buf("stg%d" % i, [128, 8, 512]) for i in range(2)]
    brow = [sc.buf("brow%d" % i, [1, 512]) for i in range(2)]
    mtmp = [sc.buf("mtmp%d" % i, [128, 512]) for i in range(2)]
    pi = 0
    for l in range(2):
        for ch in range(6):
            for half in range(2):
                c0 = ch * 1024 + half * 512
                st_, br_, mt_ = stg[pi % 2], brow[pi % 2], mtmp[pi % 2]
                pi += 1
                ld(V(st_.t[:, :, :], st_.key), adaw_d[l, :, :, c0:c0 + 512])
                ld(V(br_.t[0:1, :], br_.key), adab_d[l, 0:1, c0:c0 + 512])
                ps = PS.bank()
                for kc in range(8):
                    S.mm(ps[:, :], V(condB.t[:, kc, :], condB.key), V(st_.t[:, kc, :], st_.key), start=(kc == 0), stop=False)
                S.mm(ps[:, :], ones_row, V(br_.t[0:1, :], br_.key), start=False, stop=True)
                if ch in (1, 2, 4, 5):
                    S.ts("dve", mt_[:, :], ps[:, :], 1.0, None, ALU.add)
                else:
                    S.copy("dve", mt_[:, :], ps[:, :])
                S.store(modbc_d[l, ch, :, half * 512:half * 512 + 512], mt_[:, :], dkey=("modbc", l, ch, half))
    sc.close(S)

    def ln_tile(t):
        sv = lambda a, b: V(lns.t[:, t, a:b], ("lns", t))
        S.op("dve", lambda e: e.bn_stats(lns.t[:, t, 0:6], acc.t[:, t, 0:512]), reads=[("acc", t)], writes=[("lns", t)])
        S.op("dve", lambda e: e.bn_stats(lns.t[:, t, 6:12], acc.t[:, t, 512:1024]), reads=[("acc", t)], writes=[("lns", t)])
        S.op("dve", lambda e: e.bn_aggr(lns.t[:, t, 12:14], lns.t[:, t, 0:12]), reads=[("lns", t)], writes=[("lns", t)])
        S.ts("dve", sv(14, 15), sv(13, 14), LN_EPS, None, ALU.add)
        S.act(sv(14, 15), sv(14, 15), AF.Sqrt)
        S.op("dve", lambda e: e.reciprocal(lns.t[:, t, 14:15], lns.t[:, t, 14:15]), reads=[("lns", t)], writes=[("lns", t)])
        S.ts("dve", A(t), A(t), sv(12, 13), sv(14, 15), ALU.subtract, ALU.mult)

    def transpose_tile(src, t, hTf=None):
        for b2 in range(2):
            ps = PS.bank()
            for k4 in range(4):
                kc = b2 * 4 + k4
                S.tr(ps[:, k4 * 128:(k4 + 1) * 128], V(src.t[:, kc * 128:(kc + 1) * 128], src.key), ident)
            psv = V(ps.t[:, :].rearrange("p (k t) -> p k t", k=4), ps.key)
            if hTf is not None:
                S.copy("act", V(hTf.t[:, b2 * 4:b2 * 4 + 4, :], hTf.key), psv)
                S.copy("pool", V(hT.t[:, b2 * 4:b2 * 4 + 4, t * 128:(t + 1) * 128], ("hT", t)),
                       V(hTf.t[:, b2 * 4:b2 * 4 + 4, :], hTf.key))
            else:
                S.copy("act" if b2 else "dve", V(hT.t[:, b2 * 4:b2 * 4 + 4, t * 128:(t + 1) * 128], ("hT", t)), psv)

    def bc_tile(sc_, name, src, rkey=None):
        b = sc_.buf(name, [128, 1024])
        ld(b[:, :], src, rkey)
        return b

    def mix0(p):
        sc = Scope(nc)
        outw = sc.buf("outw", [128, 16, 1024], BF16)
        for fc in range(16):
            ld(V(outw.t[:, fc, :], outw.key), outw_d[:, fc, :], q="pool")
        gt1p = bc_tile(sc, "gt1p", modbc_d[0, 2])
        ogt = [sc.buf("ogt%d" % i, [128, 2048]) for i in range(2)]
        cand = [sc.buf("cand%d" % i, [128, 2048]) for i in range(2)] if fz is not None else None
        ogT = [sc.buf("ogT%d" % i, [128, 2048], BF16) for i in range(2)]
        xt = [sc.buf("xt%d" % i, [128, 1024]) for i in range(2)]
        t1 = [sc.buf("t1%d" % i, [128, 512]) for i in range(2)]
        for t in range(NTILE):
            o_, oT, x_ = ogt[t % 2], ogT[t % 2], xt[t % 2]
            if fz is None:
                ld(o_[:, :], og_d[p, t * 128:(t + 1) * 128, :])
            else:
                for sp in range(4):
                    cd = cand[sp % 2]
                    r0 = sp * 2048 + p * 1024 + t * 128
                    for hg in range(4):
                        S.dma(lambda e, cd=cd, hg=hg, r0=r0: e.dma_start(
                            out=cd.t[:, hg * 512:(hg + 1) * 512], in_=fz["ogg"][hg * OGROWS + r0:hg * OGROWS + r0 + 128, :]),
                            reads=["ogg"], writes=[cd.key])
                    if sp == 0:
                        S.ts("dve", o_[:, :], cd[:, :], V(sel.t[:, 0:1], "sel"), None, ALU.mult)
                    else:
                        S.stt("dve", o_[:, :], cd[:, :], V(sel.t[:, sp:sp + 1], "sel"), o_[:, :], ALU.mult, ALU.add)
            ld(x_[:, :], xh_d[p, t * 128:(t + 1) * 128, :])
            for b4 in range(4):
                ps = PS.bank()
                for k4 in range(4):
                    fc = b4 * 4 + k4
                    S.tr(ps[:, k4 * 128:(k4 + 1) * 128], V(o_.t[:, fc * 128:(fc + 1) * 128], o_.key), ident)
                S.copy("act" if b4 % 2 else "dve", V(oT.t[:, b4 * 512:(b4 + 1) * 512], oT.key), ps[:, :])
            for half in range(2):
                ps = PS.bank()
                for fc in range(16):
                    S.mm(ps[:, :], V(oT.t[:, fc * 128:(fc + 1) * 128], oT.key),
                         V(outw.t[:, fc, half * 512:(half + 1) * 512], outw.key), start=(fc == 0), stop=(fc == 15))
                tt_ = t1[half]
                S.tt("dve", tt_[:, :], ps[:, :], V(gt1p.t[:, half * 512:(half + 1) * 512], gt1p.key), ALU.mult)
                S.stt("dve", A(t, half), V(x_.t[:, half * 512:(half + 1) * 512], x_.key), ALPHA, tt_[:, :], ALU.mult, ALU.add)
        sc.close(S)

    def mix1(p):
        sc = Scope(nc)
        pw1w = sc.buf("pw1w", [128, 8, 2048], BF16)
        pw2w = sc.buf("pw2w", [128, 8, 1024], BF16)
        for kc in range(8):
            ld(V(pw1w.t[:, kc, :], pw1w.key), pw1w_d[:, kc, :], q="pool")
            ld(V(pw2w.t[:, kc, :], pw2w.key), pw2w_d[:, kc, :], q="pool")
        pw1b = sc.buf("pw1b", [128, 16]); dww = sc.buf("dww", [128, 8, 31]); dwb = sc.buf("dwb", [128, 8])
        cfg = sc.buf("cfg", [128, 8]); cfb = sc.buf("cfb", [128, 8]); pw2b = sc.buf("pw2b", [1, 1024])
        ld(pw1b[:, :], pw1b_d); ld(V(dww.t[:, :, :], dww.key), dww_d); ld(dwb[:, :], dwb_d)
        ld(cfg[:, :], cfg_d); ld(cfb[:, :], cfb_d); ld(V(pw2b.t[0:1, :], pw2b.key), pw2b_d)
        gt1p = bc_tile(sc, "gt1p", modbc_d[1, 2])
        ut = [sc.buf("ut%d" % i, [128, NTOK]) for i in range(2)]
        sg = [sc.buf("sg%d" % i, [128, 512]) for i in range(2)]
        cvT = sc.buf("cvT", [128, 8, 1024])
        sT = sc.buf("sT", [128, 8, 1024], BF16)
        mean = sc.buf("mean", [128, 1024]); rstd = sc.buf("rstd", [128, 1024]); m2 = sc.buf("m2", [128, 512])
        groups = [(0, 128), (128, 640), (640, 1152)]
        gi = 0
        for fc in range(8):
            u_ = ut[fc % 2]
            for (g0, g1) in groups:
                n = g1 - g0
                psa = PS.bank(); psg = PS.bank()
                for kc in range(8):
                    S.mm(psa[:, 0:n], V(pw1w.t[:, kc, fc * 128:(fc + 1) * 128], pw1w.key),
                         V(hT.t[:, kc, g0:g1], ("hT", "all")), start=(kc == 0), stop=(kc == 7))
                for kc in range(8):
                    S.mm(psg[:, 0:n], V(pw1w.t[:, kc, 1024 + fc * 128:1024 + (fc + 1) * 128], pw1w.key),
                         V(hT.t[:, kc, g0:g1], ("hT", "all")), start=(kc == 0), stop=(kc == 7))
                s_ = sg[gi % 2]; gi += 1
                S.act(V(s_.t[:, 0:n], s_.key), psg[:, 0:n], AF.Sigmoid, bias=V(pw1b.t[:, 8 + fc:9 + fc], pw1b.key))
                S.stt("dve", V(u_.t[:, g0:g1], u_.key), psa[:, 0:n], V(pw1b.t[:, fc:fc + 1], pw1b.key),
                      V(s_.t[:, 0:n], s_.key), ALU.add, ALU.mult)
            S.ts("dve", V(u_.t[:, 0:128], u_.key), V(u_.t[:, 0:128], u_.key), V(flag.t[:, p:p + 1], "flag"), None, ALU.mult)
            cv = V(cvT.t[:, fc, :], (cvT.key, fc))
            S.ts("dve", cv, V(u_.t[:, 98:98 + 1024], u_.key), V(dww.t[:, fc, 0:1], dww.key),
                 V(dwb.t[:, fc:fc + 1], dwb.key), ALU.mult, ALU.add)
            for j in range(1, 31):
                S.stt("dve", cv, V(u_.t[:, 98 + j:98 + j + 1024], u_.key), V(dww.t[:, fc, j:j + 1], dww.key), cv,
                      ALU.mult, ALU.add)
        for tg in range(2):
            ts_ = slice(tg * 512, (tg + 1) * 512)
            pss = PS.bank(); psq = PS.bank()
            for fc in range(8):
                cvs = V(cvT.t[:, fc, ts_], (cvT.key, fc))
                S.mm(pss[:, :], ones, cvs, start=(fc == 0), stop=(fc == 7))
                s_ = sg[fc % 2]
                S.act(s_[:, :], cvs, AF.Square)
                S.mm(psq[:, :], ones, s_[:, :], start=(fc == 0), stop=(fc == 7))
            mv = V(mean.t[:, ts_], (mean.key, tg)); rv = V(rstd.t[:, ts_], (rstd.key, tg))
            S.ts("dve", mv, pss[:, :], 1.0 / 1024.0, None, ALU.mult)
            S.tt("pool", m2[:, :], mv, mv, ALU.mult)
            S.stt("dve", rv, psq[:, :], 1.0 / 1024.0, m2[:, :], ALU.mult, ALU.subtract)
            S.ts("dve", rv, rv, LN_EPS, None, ALU.add)
            S.act(rv, rv, AF.Sqrt)
            S.op("dve", lambda e, ts_=ts_: e.reciprocal(rstd.t[:, ts_], rstd.t[:, ts_]), reads=[rv.key], writes=[rv.key])
            for fc in range(8):
                cvs = V(cvT.t[:, fc, ts_], (cvT.key, fc))
                S.tt("dve", cvs, cvs, mv, ALU.subtract)
                S.tt("pool", cvs, cvs, rv, ALU.mult)
                S.act(V(sT.t[:, fc, ts_], (sT.key, fc)), cvs, AF.Silu, bias=V(cfb.t[:, fc:fc + 1], cfb.key),
                      scale=V(cfg.t[:, fc:fc + 1], cfg.key))
        t1 = [sc.buf("t1%d" % i, [128, 512]) for i in range(2)]
        for t in range(1, NTILE):
            m0 = (t - 1) * 128
            for half in range(2):
                ps = PS.bank()
                for fc in range(8):
                    S.mm(ps[:, :], V(sT.t[:, fc, m0:m0 + 128], (sT.key, fc)),
                         V(pw2w.t[:, fc, half * 512:(half + 1) * 512], pw2w.key), start=(fc == 0), stop=False)
                S.mm(ps[:, :], ones_row, V(pw2b.t[0:1, half * 512:(half + 1) * 512], pw2b.key), start=False, stop=True)
                tt_ = t1[half]
                S.tt("dve", tt_[:, :], ps[:, :], V(gt1p.t[:, half * 512:(half + 1) * 512], gt1p.key), ALU.mult)
                S.tt("pool", A(t, half), A(t, half), tt_[:, :], ALU.add)
        sc.close(S)

    def post(l, p):
        tiles = list(range(NTILE)) if l == 0 else list(range(1, NTILE))
        sc = Scope(nc)
        lg = bc_tile(sc, "lg", lnp_d[l, 0]); lb = bc_tile(sc, "lb", lnp_d[l, 1])
        G2 = bc_tile(sc, "G2", modbc_d[l, 4]); B2 = bc_tile(sc, "B2", modbc_d[l, 3])
        S.tt("dve", B2[:, :], B2[:, :], B2[:, :], ALU.bypass) if False else None
        tmpb = sc.buf("tmpb", [128, 1024])
        S.tt("dve", tmpb[:, :], lb[:, :], G2[:, :], ALU.mult)
        S.tt("dve", B2[:, :], B2[:, :], tmpb[:, :], ALU.add)
        S.tt("dve", G2[:, :], G2[:, :], lg[:, :], ALU.mult)
        S.ts("dve", lg[:, :], lg[:, :], ALPHA, None, ALU.mult)
        S.ts("dve", lb[:, :], lb[:, :], ALPHA, None, ALU.mult)
        rw = sc.buf("rw", [128, 8, 32]); rb = sc.buf("rb", [1, 32])
        ld(V(rw.t[:, :, :], rw.key), rw_d[l]); ld(V(rb.t[0:1, :], rb.key), rb_d[l])
        h2 = [sc.buf("h2%d" % i, [128, 1024]) for i in range(2)]
        hTf = [sc.buf("hTf%d" % i, [128, 8, 128]) for i in range(2)]
        rt = sc.buf("rt", [128, NTILE, 128])
        for t in tiles:
            ln_tile(t)
            h_ = h2[t % 2]; hf = hTf[t % 2]
            S.tt("dve", h_[:, :], A(t), G2[:, :], ALU.mult)
            S.tt("pool", h_[:, :], h_[:, :], B2[:, :], ALU.add)
            S.tt("dve", A(t), A(t), lg[:, :], ALU.mult)
            S.tt("pool", A(t), A(t), lb[:, :], ALU.add)
            transpose_tile(h_, t, hf)
            pq = PS.quarter(32)
            for kc in range(8):
                S.mm(pq, V(hf.t[:, kc, :], hf.key), V(rw.t[:, kc, :], rw.key), start=(kc == 0), stop=False)
            S.mm(pq, ones_row, V(rb.t[0:1, :], rb.key), start=False, stop=True)
            r = lambda a, b, t=t: V(rt.t[:, t, a:b], (rt.key, t))
            S.copy("dve", r(0, 32), pq)
            S.op("dve", lambda e, t=t: e.max(rt.t[:, t, 32:40], rt.t[:, t, 0:32]), reads=[(rt.key, t)], writes=[(rt.key, t)])
            S.ts("dve", r(40, 72), r(0, 32), r(35, 36), None, ALU.is_ge)
            S.ts("dve", r(72, 73), r(32, 33), -1.0, None, ALU.mult)
            S.act(r(80, 112), r(0, 32), AF.Exp, bias=r(72, 73))
            S.tt("dve", r(80, 112), r(80, 112), r(40, 72), ALU.mult)
            S.op("dve", lambda e, t=t: e.reduce_sum(rt.t[:, t, 73:74], rt.t[:, t, 80:112], mybir.AxisListType.X),
                 reads=[(rt.key, t)], writes=[(rt.key, t)])
            S.op("dve", lambda e, t=t: e.reciprocal(rt.t[:, t, 74:75], rt.t[:, t, 73:74]), reads=[(rt.key, t)], writes=[(rt.key, t)])
            gv = V(gates.t[:, t, :], ("gates", t))
            S.ts("dve", gv, r(80, 112), r(74, 75), None, ALU.mult)
            pq2 = PS.quarter(128)
            S.tr(V(pq2.ap[0:32, :], pq2.key), gv, ident)
            S.copy("act", V(gT.t[0:32, t * 128:(t + 1) * 128], ("gT", t)), V(pq2.ap[0:32, :], pq2.key))
        sc.close(S)

        sc = Scope(nc)
        gt2p = bc_tile(sc, "gt2p", modbc_d[l, 5])
        b2s = sc.buf("b2s", [32, 1024])
        ld(V(b2s.t[0:32, :], b2s.key), eb2_d[l])
        S.tt("dve", V(b2s.t[0:32, :], b2s.key), V(b2s.t[0:32, :], b2s.key), V(gt2p.t[0:32, :], gt2p.key), ALU.mult)
        b1r = sc.buf("b1r", [128, 4, 128]); b1T = sc.buf("b1T", [128, 512])
        ld(V(b1r.t[:, :, :], b1r.key), eb1_d[l].rearrange("a p f -> p a f"))
        ps = PS.bank()
        for a4 in range(4):
            S.tr(ps[:, a4 * 128:(a4 + 1) * 128], V(b1r.t[:, a4, :], b1r.key), ident)
        S.copy("dve", b1T[:, :], ps[:, :])
        b1v = b1T.t[:, :].rearrange("p (e f) -> p e f", f=16)[:, :, 8:16]
        S.op("dve", lambda e: e.tensor_scalar(b1v, b1v, 1.0, None, ALU.add), reads=[b1T.key], writes=[b1T.key])
        for t in tiles:
            for half in range(2):
                ps = PS.bank()
                S.mm(ps[:, :], V(gT.t[0:32, t * 128:(t + 1) * 128], ("gT", t)),
                     V(b2s.t[0:32, half * 512:(half + 1) * 512], b2s.key))
                S.tt("dve", A(t, half), ps[:, :], A(t, half), ALU.add)
        stg = [sc.buf("stg%d" % i, [128, 8, 512]) for i in range(2)]
        NW1 = 4
        w1b = [sc.buf("w1b%d" % i, [128, 8, 512], BF16) for i in range(NW1)]
        w2b = [sc.buf("w2b%d" % i, [128, 8, 1024], BF16) for i in range(2)]
        actb = sc.buf("actb", [128, 8, NTOK], BF16)
        xg = [sc.buf("xg%d" % i, [128, 512]) for i in range(2)]
        sgm = [sc.buf("sgm%d" % i, [128, 512]) for i in range(2)]
        xl = [sc.buf("xl%d" % i, [128, 512]) for i in range(2)]
        groups = [(0, 128), (128, 640), (640, 1152)] if l == 0 else [(128, 640), (640, 1152)]
        si = 0; wi = 0; ci = 0
        for e_ in range(n_exp):
            w2_ = w2b[e_ % 2]
            for hh in range(2):
                st_ = stg[si % 2]; si += 1
                st4 = st_.t[:, :, :].rearrange("p a f -> p (a f)").rearrange("p (k d) -> p k d", k=4)
                ld(V(st4, st_.key), ew2_d[l, e_, hh * 512:(hh + 1) * 512, :].rearrange("(k p) d -> p k d", p=128))
                for k in range(4):
                    for a in range(2):
                        S.tt("pool", V(w2_.t[:, hh * 4 + k, a * 512:(a + 1) * 512], w2_.key),
                             V(st4[:, k, a * 512:(a + 1) * 512], st_.key), V(gt2p.t[:, a * 512:(a + 1) * 512], gt2p.key), ALU.mult)
            for hf in range(2):
                wgl = []
                for part in range(2):
                    st_ = stg[si % 2]; si += 1
                    w1_ = w1b[wi % NW1]; wi += 1
                    c0 = part * 1024 + hf * 512
                    ld(V(st_.t[:, :, :], st_.key), ew1_d[l, e_, :, c0:c0 + 512].rearrange("(k p) f -> p k f", p=128))
                    S.copy("pool", V(w1_.t[:, :, :], w1_.key), V(st_.t[:, :, :], st_.key))
                    wgl.append(w1_)
                wg_, wl_ = wgl
                for (g0, g1) in groups:
                    n = g1 - g0
                    for ii in range(4):
                        i = 4 * hf + ii
                        psg = PS.bank(); psl = PS.bank()
                        for kc in range(8):
                            S.mm(psg[:, 0:n], V(wg_.t[:, kc, ii * 128:(ii + 1) * 128], wg_.key),
                                 V(hT.t[:, kc, g0:g1], ("hT", "all")), start=(kc == 0), stop=(kc == 7))
                        for kc in range(8):
                            S.mm(psl[:, 0:n], V(wl_.t[:, kc, ii * 128:(ii + 1) * 128], wl_.key),
                                 V(hT.t[:, kc, g0:g1], ("hT", "all")), start=(kc == 0), stop=(kc == 7))
                        x_, s_, l_ = xg[ci % 2], sgm[ci % 2], xl[ci % 2]; ci += 1
                        bg = V(b1T.t[:, e_ * 16 + i:e_ * 16 + i + 1], b1T.key)
                        bl = V(b1T.t[:, e_ * 16 + 8 + i:e_ * 16 + 8 + i + 1], b1T.key)
                        S.ts("dve", V(x_.t[:, 0:n], x_.key), psg[:, 0:n], bg, LIMIT, ALU.add, ALU.min)
                        S.act(V(s_.t[:, 0:n], s_.key), V(x_.t[:, 0:n], x_.key), AF.Sigmoid, scale=SW_ALPHA)
                        S.ts("dve", V(l_.t[:, 0:n], l_.key), psl[:, 0:n], bl, 1.0 - LIMIT, ALU.add, ALU.max)
                        S.tt("pool", V(x_.t[:, 0:n], x_.key), V(x_.t[:, 0:n], x_.key), V(s_.t[:, 0:n], s_.key), ALU.mult)
                        S.stt("dve", V(actb.t[:, i, g0:g1], (actb.key, i)), V(l_.t[:, 0:n], l_.key), 1.0 + LIMIT,
                              V(x_.t[:, 0:n], x_.key), ALU.min, ALU.mult)
            for t in tiles:
                for half in range(2):
                    ps = PS.bank()
                    for fc in range(8):
                        S.mm(ps[:, :], V(actb.t[:, fc, t * 128:(t + 1) * 128], (actb.key, fc)),
                             V(w2_.t[:, fc, half * 512:(half + 1) * 512], w2_.key), start=(fc == 0), stop=(fc == 7))
                    S.stt("dve", A(t, half), ps[:, :], V(gates.t[:, t, e_:e_ + 1], ("gates", t)), A(t, half),
                          ALU.mult, ALU.add)
        sc.close(S)

        sc = Scope(nc)
        g2 = bc_tile(sc, "g2", lnp_d[l, 2]); b2 = bc_tile(sc, "b2", lnp_d[l, 3])
        if l == 0:
            G3 = bc_tile(sc, "G3", modbc_d[1, 1]); B3 = bc_tile(sc, "B3", modbc_d[1, 0])
            tmpb = sc.buf("tmpb", [128, 1024])
            S.tt("dve", tmpb[:, :], b2[:, :], G3[:, :], ALU.mult)
            S.tt("dve", B3[:, :], B3[:, :], tmpb[:, :], ALU.add)
            S.tt("dve", G3[:, :], G3[:, :], g2[:, :], ALU.mult)
            S.ts("dve", g2[:, :], g2[:, :], ALPHA, None, ALU.mult)
            S.ts("dve", b2[:, :], b2[:, :], ALPHA, None, ALU.mult)
            h2 = [sc.buf("h2%d" % i, [128, 1024]) for i in range(2)]
            for t in tiles:
                ln_tile(t)
                h_ = h2[t % 2]
                S.tt("dve", h_[:, :], A(t), G3[:, :], ALU.mult)
                S.tt("pool", h_[:, :], h_[:, :], B3[:, :], ALU.add)
                S.tt("dve", A(t), A(t), g2[:, :], ALU.mult)
                S.tt("pool", A(t), A(t), b2[:, :], ALU.add)
                transpose_tile(h_, t)
                if 1 not in layers and t >= 1:
                    S.store(out_d[p, (t - 1) * 128:t * 128, :], A(t))
        else:
            for t in tiles:
                ln_tile(t)
                S.tt("dve", A(t), A(t), g2[:, :], ALU.mult)
                S.tt("pool", A(t), A(t), b2[:, :], ALU.add)
                S.store(out_d[p, (t - 1) * 128:t * 128, :], A(t))
        sc.close(S)

    for p in range(npass):
        if 0 in layers:
            mix0(p)
            post(0, p)
        if 1 in layers:
            mix1(p)
            post(1, p)
    S.barrier()
    S.flush(final=True)
    return nc


def prep_phaseB(inp, og_b, b, s, shared=None):
    f = np.float32
    xh = np.zeros((2, NTOK, D), f)
    ogh = np.zeros((2, NTOK, 2048), f)
    flag = np.ones((128, 2), f)
    for p in range(2):
        t_lo = s * 2048 + p * 1024 - 128
        if t_lo < 0:
            flag[:, p] = 0.0
            xh[p, 128:] = inp["x"][b, 0:1024]
            ogh[p, 128:] = og_b[0:1024]
        else:
            xh[p] = inp["x"][b, t_lo:t_lo + NTOK]
            ogh[p] = og_b[t_lo:t_lo + NTOK]
    m = {"xh": xh, "ogh": ogh, "flag": flag,
         "cb": np.ascontiguousarray(inp["c"][b].reshape(8, 128).T)}
    if shared is None:
        shared = prep_phaseB_shared(inp)
    m.update(shared)
    return m


def prep_phaseB_shared(inp):
    f = np.float32
    bc = lambda v: np.ascontiguousarray(np.broadcast_to(v[None, :], (128, v.shape[0])))
    pp = lambda v, n: np.ascontiguousarray(v.reshape(n, 128).T)
    lnp = np.stack([np.stack([bc(inp[k][l]) for k in ("ln1_g", "ln1_b", "ln2_g", "ln2_b")]) for l in range(2)])
    return {
        "adaw": np.stack([pkc(inp["ada_w"][l]) for l in range(2)]),
        "adab": np.ascontiguousarray(inp["ada_b"][:, None, :]),
        "outw": pkc(inp["dn_out_w"][0]),
        "pw1w": pkc(inp["cf_pw1_w"][0]),
        "pw1b": pp(inp["cf_pw1_b"][0], 16),
        "dww": np.ascontiguousarray(inp["cf_dw_w"][0].reshape(31, 8, 128).transpose(2, 1, 0)),
        "dwb": pp(inp["cf_dw_b"][0], 8),
        "cfg": pp(inp["cf_ln_g"][0], 8),
        "cfb": pp(inp["cf_ln_b"][0], 8),
        "pw2w": pkc(inp["cf_pw2_w"][0]),
        "pw2b": np.ascontiguousarray(inp["cf_pw2_b"][0][None, :]),
        "lnp": np.ascontiguousarray(lnp.astype(f)),
        "rw": np.stack([pkc(inp["router_w"][l]) for l in range(2)]),
        "rb": np.ascontiguousarray(inp["router_b"][:, None, :]),
        "ew1": np.ascontiguousarray(inp["e_w1"]),
        "eb1": np.ascontiguousarray(inp["e_b1"].reshape(2, 4, 128, 128)),
        "ew2": np.ascontiguousarray(inp["e_w2"]),
        "eb2": np.ascontiguousarray(inp["e_b2"]),
        "cst": consts_np(),
    }


def build_fused():
    nc = bass.Bass("TRN2", target_bir_lowering=False)
    dt = nc.dram_tensor
    I = lambda name, shape: dt(name, shape, F32, kind="ExternalInput").ap()
    fz = declare_B(nc, with_og=False)
    fz["nc"] = nc
    fz["x"] = I("x", [SEQ, D])
    win4 = I("win", [4, 128, 8, 1544])
    cw4 = I("cw", [4, 128, 8, 4])
    alog4 = I("alog", [4, 128, 4])
    dtb4 = I("dtb", [4, 128, 4])
    fz["onw"] = I("onw", [128, 512])
    fz["sel"] = I("sel", [128, 4])
    fz["ogg"] = dt("ogg", [4 * OGROWS, 512], F32).ap()
    S = Sched(nc)
    fz["S"] = S
    fz["PS"] = PsumPool(nc, 4, "p")
    fz["PS"].register(S)
    for hg in range(4):
        fh = dict(fz)
        fh["win"], fh["cw"], fh["alog"], fh["dtb"] = win4[hg], cw4[hg], alog4[hg], dtb4[hg]
        fh["ogpad"] = fz["ogg"][hg * OGROWS:(hg + 1) * OGROWS, :]
        build_phaseA(SEQ, fh)
    build_phaseB(fz=fz)
    return nc


def kernel(**inp):
    inp = {k: np.asarray(v) for k, v in inp.items()}
    nc = build_fused()
    shared = prep_phaseB_shared(inp)
    dummy_og = np.zeros((SEQ, 2048), np.float32)
    mA = {b: [prep_phaseA(inp, b, hg, SEQ) for hg in range(4)] for b in range(2)}
    stk = {b: {k: np.ascontiguousarray(np.stack([mA[b][hg][k] for hg in range(4)])) for k in ("win", "cw", "alog", "dtb")}
           for b in range(2)}
    maps = []
    for i in range(8):
        b, r = i // 4, i % 4
        mB = prep_phaseB(inp, dummy_og, b, r, shared)
        m = {k: v for k, v in mB.items() if k != "ogh"}
        m["x"] = mA[b][0]["x"]
        m["onw"] = mA[b][0]["onw"]
        m.update(stk[b])
        sel = np.zeros((128, 4), np.float32)
        sel[:, r] = 1.0
        m["sel"] = sel
        maps.append(m)
    res = run_bass_kernel_spmd(nc, maps, core_ids=list(range(8)))
    out = np.zeros((2, SEQ, D), np.float32)
    for i in range(8):
        b, s_ = i // 4, i % 4
        o = res.results[i]["out"]
        out[b, s_ * 2048:s_ * 2048 + 1024] = o[0]
        out[b, s_ * 2048 + 1024:s_ * 2048 + 2048] = o[1]
    return out
```
